# Optimizing a Trainium2 kernel written in Bass

```python
import math
import jax, jax.numpy as jnp
from jax import lax
import numpy as np

D_MODEL = 1024
BATCH = 8
SEQ = 2048
DEPTH = 2

MIX_WIDTH = D_MODEL
HEAD_DIM = 64
A_WIDTH = MIX_WIDTH // 2
B_WIDTH = MIX_WIDTH - A_WIDTH
A_GROUPS = A_WIDTH // HEAD_DIM
B_HEADS = B_WIDTH // HEAD_DIM
IN_COLS = 2 * A_WIDTH + 3 * B_WIDTH
CHUNK = 128
DILATED_CONFIGS = ((128, 1), (512, 4), (2048, 16))
ATTN_BLOCK = 128
REL_BUCKETS = 32
REL_MAX_EXACT = REL_BUCKETS // 2
REL_MAX_DISTANCE = 2048
N_EXPERTS = 16
N_EXPERT_GROUPS = 4
EXPERTS_PER_GROUP = N_EXPERTS // N_EXPERT_GROUPS
TOP_K = 2
D_EXPERT = D_MODEL // 2
N_MOD = 6
EPS = 1e-6
NEG_INF = -1e30

kernel_name = "hybrid_gmlp_dilated_attn_grouped_moe"


def rms_norm(x, g):
    xf = x.astype(jnp.float32)
    y = xf * lax.rsqrt(jnp.mean(xf * xf, axis=-1, keepdims=True) + EPS)
    return (y * g.astype(jnp.float32)).astype(x.dtype)


def layer_norm(x, g, b):
    xf = x.astype(jnp.float32)
    mu = jnp.mean(xf, axis=-1, keepdims=True)
    xc = xf - mu
    y = xc * lax.rsqrt(jnp.mean(xc * xc, axis=-1, keepdims=True) + EPS)
    return (y * g.astype(jnp.float32) + b.astype(jnp.float32)).astype(x.dtype)


def t5_bucket_np(dist):
    dist = np.maximum(dist, 0)
    ratio = np.log(np.maximum(dist, 1) / REL_MAX_EXACT) / np.log(REL_MAX_DISTANCE / REL_MAX_EXACT)
    large = REL_MAX_EXACT + np.floor(ratio * (REL_BUCKETS - REL_MAX_EXACT)).astype(np.int64)
    large = np.minimum(large, REL_BUCKETS - 1)
    return np.where(dist < REL_MAX_EXACT, dist, large).astype(np.int32)


def chunked_spatial_gating(u, v, ln_g, ln_b, w_s, b_s):
    B, S, _ = u.shape
    v = layer_norm(v, ln_g, ln_b)
    vc = v.reshape(B, S // CHUNK, CHUNK, A_GROUPS, HEAD_DIM)
    causal = jnp.tril(jnp.ones((CHUNK, CHUNK), dtype=w_s.dtype))
    s = jnp.einsum('gts,bnsge->bntge', w_s * causal, vc) + b_s.T[None, None, :, :, None]
    return u * s.reshape(B, S, A_WIDTH)


def dilated_window_attention(q, k, v, rel_bias, window, dilation):
    B, S, H, Dh = q.shape
    d = dilation
    L = S // d
    span = window // d
    blk = ATTN_BLOCK
    nb = -(-L // blk)
    Lp = nb * blk

    def gather(t):
        t = t.reshape(B, L, d, H, Dh).transpose(0, 2, 1, 3, 4)
        return jnp.pad(t, ((0, 0), (0, 0), (0, Lp - L), (0, 0), (0, 0)))

    def band(t):
        tb = t.reshape(B, d, nb, blk, H, Dh)
        prev = jnp.pad(tb, ((0, 0), (0, 0), (1, 0), (0, 0), (0, 0), (0, 0)))[:, :, :-1]
        return jnp.concatenate([prev, tb], axis=3)

    qb = gather(q).reshape(B, d, nb, blk, H, Dh).astype(jnp.float32)
    kb = band(gather(k)).astype(jnp.float32)
    vb = band(gather(v)).astype(jnp.float32)
    scores = jnp.einsum('brnqhe,brnkhe->brnhqk', qb, kb)

    qi = np.arange(blk)[:, None]
    kj = np.arange(2 * blk)[None, :]
    rel = qi + blk - kj
    bucket = t5_bucket_np(np.clip(rel, 0, span) * d)
    bias = jnp.transpose(rel_bias.astype(jnp.float32)[bucket], (2, 0, 1))
    key_pos = np.arange(nb)[:, None, None] * blk + kj[None] - blk
    valid = ((rel >= 0) & (rel <= span))[None] & (key_pos >= 0)
    scores = jnp.where(valid[None, None, :, None], scores + bias, NEG_INF)

    lse = jax.nn.logsumexp(scores, axis=-1)
    p = jnp.exp(scores - lse[..., None])
    o = jnp.einsum('brnhqk,brnkhe->brnqhe', p, vb)
    o = o.reshape(B, d, Lp, H, Dh)[:, :, :L].transpose(0, 2, 1, 3, 4).reshape(B, S, H, Dh)
    lse = lse.transpose(0, 1, 2, 4, 3).reshape(B, d, Lp, H)[:, :, :L]
    lse = lse.transpose(0, 2, 1, 3).reshape(B, S, H)
    return o, lse


def dilated_mixture_attention(q, k, v, rel_bias):
    B, S, _ = q.shape
    q = q.reshape(B, S, B_HEADS, HEAD_DIM) * (HEAD_DIM ** -0.5)
    k = k.reshape(B, S, B_HEADS, HEAD_DIM)
    v = v.reshape(B, S, B_HEADS, HEAD_DIM)
    outs, lses = [], []
    for window, dilation in DILATED_CONFIGS:
        o, lse = dilated_window_attention(q, k, v, rel_bias, window, dilation)
        outs.append(o)
        lses.append(lse)
    w = jax.nn.softmax(jnp.stack(lses, axis=0), axis=0)
    o = jnp.sum(w[..., None] * jnp.stack(outs, axis=0), axis=0)
    return o.reshape(B, S, B_WIDTH).astype(v.dtype)


def grouped_top2_moe(h, router_w, router_b, w_gate, w_up, w_down):
    B, S, D = h.shape
    t = h.reshape(-1, D)
    probs = jax.nn.softmax((t @ router_w).astype(jnp.float32), axis=-1)
    sel = probs + router_b.astype(jnp.float32)
    grp = sel.reshape(-1, N_EXPERT_GROUPS, EXPERTS_PER_GROUP)
    grp_score = jnp.sum(lax.top_k(grp, TOP_K)[0], axis=-1)
    best = jnp.argmax(grp_score, axis=-1)
    in_group = (jnp.arange(N_EXPERTS) // EXPERTS_PER_GROUP)[None, :] == best[:, None]
    _, idx = lax.top_k(jnp.where(in_group, sel, NEG_INF), TOP_K)
    g = jnp.take_along_axis(probs, idx, axis=-1)
    g = g / jnp.sum(g, axis=-1, keepdims=True)
    gates = jnp.sum(jax.nn.one_hot(idx, N_EXPERTS, dtype=jnp.float32) * g[..., None], axis=1)
    hg = jnp.einsum('td,edf->tef', t, w_gate)
    hu = jnp.einsum('td,edf->tef', t, w_up)
    act = jax.nn.silu(hg) * hu * gates[:, :, None].astype(hu.dtype)
    y = jnp.einsum('tef,efd->td', act, w_down)
    return y.reshape(B, S, D)


def setup_inputs(seed: int = 0) -> dict:
    key = jax.random.key(seed)
    ks = jax.random.split(key, 24)
    f32 = jnp.float32
    nrm = lambda k, shape, s: (jax.random.normal(k, shape, f32) * s)
    return {
        "x": nrm(ks[0], (BATCH, SEQ, D_MODEL), 1.0),
        "c": nrm(ks[1], (BATCH, D_MODEL), 1.0),
        "rel_bias": nrm(ks[2], (REL_BUCKETS, B_HEADS), 0.5),
        "router_w": nrm(ks[3], (D_MODEL, N_EXPERTS), D_MODEL ** -0.5),
        "router_b": nrm(ks[4], (N_EXPERTS,), 0.01),
        "mod_w": nrm(ks[5], (DEPTH, D_MODEL, N_MOD * D_MODEL), 0.5 * D_MODEL ** -0.5),
        "mod_b": nrm(ks[6], (DEPTH, N_MOD * D_MODEL), 0.02),
        "norm1_g": 1.0 + nrm(ks[7], (DEPTH, D_MODEL), 0.05),
        "w_in": nrm(ks[8], (DEPTH, D_MODEL, IN_COLS), D_MODEL ** -0.5),
        "gmlp_ln_g": 1.0 + nrm(ks[9], (DEPTH, A_WIDTH), 0.05),
        "gmlp_ln_b": nrm(ks[10], (DEPTH, A_WIDTH), 0.02),
        "gmlp_ws": nrm(ks[11], (DEPTH, A_GROUPS, CHUNK, CHUNK), CHUNK ** -0.5),
        "gmlp_bs": 1.0 + nrm(ks[12], (DEPTH, A_GROUPS, CHUNK), 0.1),
        "out_norm_a_g": 1.0 + nrm(ks[13], (DEPTH, A_WIDTH), 0.05),
        "out_norm_b_g": 1.0 + nrm(ks[14], (DEPTH, B_WIDTH), 0.05),
        "w_out": nrm(ks[15], (DEPTH, MIX_WIDTH, D_MODEL), MIX_WIDTH ** -0.5),
        "norm2_g": 1.0 + nrm(ks[16], (DEPTH, D_MODEL), 0.05),
        "moe_w_gate": nrm(ks[17], (DEPTH, N_EXPERTS, D_MODEL, D_EXPERT), D_MODEL ** -0.5),
        "moe_w_up": nrm(ks[18], (DEPTH, N_EXPERTS, D_MODEL, D_EXPERT), D_MODEL ** -0.5),
        "moe_w_down": nrm(ks[19], (DEPTH, N_EXPERTS, D_EXPERT, D_MODEL), D_EXPERT ** -0.5),
        "final_g": 1.0 + nrm(ks[20], (D_MODEL,), 0.05),
    }


def reference(x, c, rel_bias, router_w, router_b, mod_w, mod_b, norm1_g, w_in,
              gmlp_ln_g, gmlp_ln_b, gmlp_ws, gmlp_bs, out_norm_a_g, out_norm_b_g,
              w_out, norm2_g, moe_w_gate, moe_w_up, moe_w_down, final_g):
    split_at = [A_WIDTH, 2 * A_WIDTH, 2 * A_WIDTH + B_WIDTH, 2 * A_WIDTH + 2 * B_WIDTH]
    c_act = jax.nn.silu(c)
    for l in range(DEPTH):
        mod = (c_act @ mod_w[l] + mod_b[l])[:, None, :]
        sh1, sc1, g1, sh2, sc2, g2 = jnp.split(mod, N_MOD, axis=-1)

        h = rms_norm(x, norm1_g[l]) * (1.0 + sc1) + sh1
        proj = h @ w_in[l]
        u, va, q, k, vb = jnp.split(proj, split_at, axis=-1)
        out_a = chunked_spatial_gating(jax.nn.gelu(u, approximate=False),
                                       jax.nn.gelu(va, approximate=False),
                                       gmlp_ln_g[l], gmlp_ln_b[l], gmlp_ws[l], gmlp_bs[l])
        out_b = dilated_mixture_attention(q, k, vb, rel_bias)
        mixed = jnp.concatenate([rms_norm(out_a, out_norm_a_g[l]),
                                 rms_norm(out_b, out_norm_b_g[l])], axis=-1) @ w_out[l]
        x = x + g1 * mixed

        h = rms_norm(x, norm2_g[l]) * (1.0 + sc2) + sh2
        x = x + g2 * grouped_top2_moe(h, router_w, router_b,
                                      moe_w_gate[l], moe_w_up[l], moe_w_down[l])
    return rms_norm(x, final_g)
```

```python
import contextlib
import os
import numpy as np
import concourse.bass as bass
import concourse.mybir as mybir
from concourse.bass_utils import run_bass_kernel_spmd

F32 = mybir.dt.float32
BF16 = mybir.dt.bfloat16
ALU = mybir.AluOpType
AF = mybir.ActivationFunctionType
AX = mybir.AxisListType
ENGS = ("pe", "act", "dve", "pool", "sp")
EPS = 1e-6
NEG = -30000.0
CONFIGS = ((128, 1), (512, 4), (2048, 16))


class Prog:
    def __init__(self, nc):
        self.nc = nc
        self.ins = []
        self.last_w = {}
        self.readers = {}
        self.stream_cnt = {}
        self.stream_last = {}

    def add(self, eng, fn, reads=(), writes=(), dma=None):
        i = len(self.ins)
        deps = {}

        def dep(j):
            if j is None:
                return
            pj = self.ins[j]
            deps[j] = self.stream_cnt[pj["dma"]] if pj["dma"] is not None else None

        for r in reads:
            dep(self.last_w.get(r))
        for w in writes:
            dep(self.last_w.get(w))
            for j in self.readers.get(w, ()):
                dep(j)
        pruned = {}
        for j, c in deps.items():
            pj = self.ins[j]
            if pj["dma"] is None and pj["eng"] == eng:
                if eng == "pe":
                    continue
                if not any(self.last_w.get(r) == j for r in reads):
                    continue
            pruned[j] = c
            if pj["dma"] is None:
                pj["needs_inc"] = True
        rec = dict(eng=eng, fn=fn, deps=pruned, dma=dma, needs_inc=False, val=None)
        if dma is not None:
            self.stream_cnt[dma] = self.stream_cnt.get(dma, 0) + 1
            self.stream_last[dma] = i
        self.ins.append(rec)
        for r in reads:
            self.readers.setdefault(r, []).append(i)
        for w in writes:
            self.last_w[w] = i
            self.readers[w] = []
        return i

    def pe(self, fn, reads=(), writes=()):
        return self.add("pe", fn, reads, writes)

    def act(self, fn, reads=(), writes=()):
        return self.add("act", fn, reads, writes)

    def dve(self, fn, reads=(), writes=()):
        return self.add("dve", fn, reads, writes)

    def pool(self, fn, reads=(), writes=()):
        return self.add("pool", fn, reads, writes)

    def dma(self, q, stream, fn, reads=(), writes=()):
        return self.add(q, fn, reads, writes, dma=stream)

    def barrier(self):
        last = {}
        for idx, rec in enumerate(self.ins):
            if rec["dma"] is None and not rec.get("bar"):
                last[rec["eng"]] = idx
        for e in ENGS:
            deps = {}
            for e2, j in last.items():
                if e2 == e and e == "pe":
                    continue
                deps[j] = None
                self.ins[j]["needs_inc"] = True
            for s, c in self.stream_cnt.items():
                deps[self.stream_last[s]] = c
            self.ins.append(dict(eng=e, fn=None, deps=deps, dma=None, needs_inc=False, val=None, bar=True))
        self.last_w = {}
        self.readers = {}

    def emit(self, final_wait_streams=()):
        nc = self.nc
        streams = sorted(self.stream_cnt.keys())
        with contextlib.ExitStack() as es:
            esem = {e: es.enter_context(nc.semaphore("s_" + e)) for e in ENGS}
            ssem = {s: es.enter_context(nc.semaphore("d_" + str(s))) for s in streams}
            cnt = {e: 0 for e in ENGS}
            for rec in self.ins:
                if rec["dma"] is None and rec["needs_inc"]:
                    cnt[rec["eng"]] += 1
                    rec["val"] = cnt[rec["eng"]]
            per_eng = {e: [] for e in ENGS}
            for rec in self.ins:
                per_eng[rec["eng"]].append(rec)
            ins = self.ins
            block = es.enter_context(nc.Block())

            def run(ename, eng):
                waited = {}
                for rec in per_eng[ename]:
                    for j, c in rec["deps"].items():
                        pj = ins[j]
                        if pj["dma"] is not None:
                            sem, v, key = ssem[pj["dma"]], c * 16, ("d", pj["dma"])
                        else:
                            sem, v, key = esem[pj["eng"]], pj["val"], ("e", pj["eng"])
                        if waited.get(key, 0) >= v:
                            continue
                        waited[key] = v
                        eng.wait_ge(sem, v)
                    if rec["fn"] is None:
                        continue
                    bi = rec["fn"](eng)
                    if rec["dma"] is not None:
                        bi.then_inc(ssem[rec["dma"]], 16)
                    elif rec["needs_inc"]:
                        bi.then_inc(esem[ename], 1)
                if ename == "sp":
                    for s in final_wait_streams:
                        eng.wait_ge(ssem[s], self.stream_cnt[s] * 16)

            block.tensor(lambda e: run("pe", e))
            block.scalar(lambda e: run("act", e))
            block.vector(lambda e: run("dve", e))
            block.gpsimd(lambda e: run("pool", e))
            block.sync(lambda e: run("sp", e))


class Arena:
    def __init__(self, t, words):
        self.t = t
        self.words = words
        self.off = 0

    def reset(self):
        self.off = 0

    def _take(self, nwords):
        nwords = (nwords + 7) // 8 * 8
        a = self.off
        self.off += nwords
        assert self.off <= self.words, ("arena overflow", self.off, self.words)
        return a

    def f32(self, shape):
        n = int(np.prod(shape[1:]))
        a = self._take(n)
        ap = self.t[0:shape[0], a:a + n]
        return self._shape(ap, shape)

    def bf16(self, shape):
        n = int(np.prod(shape[1:]))
        a = self._take((n + 1) // 2)
        ap = self.t[0:shape[0], a:a + (n + 1) // 2].bitcast(BF16)[:, 0:n]
        return self._shape(ap, shape)

    @staticmethod
    def _shape(ap, shape):
        if len(shape) == 2:
            return ap
        if len(shape) == 3:
            return ap.rearrange("p (a b) -> p a b", a=shape[1])
        if len(shape) == 4:
            return ap.rearrange("p (a b c) -> p a b c", a=shape[1], b=shape[2])
        raise ValueError(shape)


NSB = 4
ARENA_WORDS = 25 * 1024


def build(stop=None, n_layers=2):
    nc = bass.Bass("TRN2", target_bir_lowering=False)
    din = lambda name, shape: nc.dram_tensor(name, shape, F32, kind="ExternalInput").ap()
    x_d = din("x", [2048, 1024])
    cT_d = din("cT", [128, 8])
    relb_d = din("rel_bias", [32, 8])
    rw_d = din("router_w", [1024, 16])
    rb_d = din("router_b", [1, 16])
    modw_d = din("mod_w", [2, 1024, 6144])
    modb_d = din("mod_b", [2, 6144])
    n1g_d = din("norm1_g", [2, 1024])
    win_d = din("w_in", [2, 1024, 2560])
    lng_d = din("gmlp_ln_g", [2, 512])
    lnb_d = din("gmlp_ln_b", [2, 512])
    ws_d = din("gmlp_ws", [2, 8, 128, 128])
    bs_d = din("gmlp_bs", [2, 8, 128])
    gab_d = din("gab", [2, 128, 8])
    wout_d = din("w_out", [2, 1024, 1024])
    n2g_d = din("norm2_g", [2, 1024])
    wg_d = din("moe_w_gate", [2, 16, 1024, 512])
    wu_d = din("moe_w_up", [2, 16, 1024, 512])
    wd_d = din("moe_w_down", [2, 16, 512, 1024])
    fg_d = din("final_g", [1, 1024])
    identf_d = din("identf", [128, 128])
    btab_d = din("btab", [3, 33, 384])
    ind_d = din("ind", [8, 512])
    tril_d = din("trilm", [128, 128])
    jmat_d = din("jmat", [128, 128])
    out_d = nc.dram_tensor("out", [2048, 1024], F32, kind="ExternalOutput").ap()
    modscr = nc.dram_tensor("modscr", [2, 6144], F32, kind="Internal").ap()
    gscr_h = nc.dram_tensor("gscr", [3, 8, 384], F32, kind="Internal")
    gscr = gscr_h.ap()
    texp = nc.dram_tensor("texp", [3, 8, 128, 256], F32, kind="Internal").ap()
    obT_d = nc.dram_tensor("obT_d", [128, 4, 2048], BF16, kind="Internal").ap()
    dbg = stop is not None
    if dbg:
        dbg_x = nc.dram_tensor("dbg_x", [2048, 1024], F32, kind="ExternalOutput").ap()
        dbg_hT = nc.dram_tensor("dbg_hT", [128, 8, 2048], BF16, kind="ExternalOutput").ap()
        dbg_g = nc.dram_tensor("dbg_g", [128, 256], F32, kind="ExternalOutput").ap()
        dbg_obT = nc.dram_tensor("dbg_obT", [128, 4, 2048], BF16, kind="ExternalOutput").ap()

    P = Prog(nc)
    es = contextlib.ExitStack()
    with es:
        sb = lambda name, shape, dt: es.enter_context(nc.sbuf_tensor(name, shape, dt))
        x = sb("x_sb", [128, 16, 1024], F32)
        hT = sb("hT", [128, 8, 2048], BF16)
        identf = sb("identf_sb", [128, 128], F32)
        identb = sb("identb_sb", [128, 128], BF16)
        trilm = sb("trilm_sb", [128, 128], F32)
        ind = sb("ind_sb", [8, 512], F32)
        ones_bf = sb("ones_bf", [1, 128], BF16)
        eps_t = sb("eps_t", [128, 1], F32)
        nhalf = sb("nhalf_t", [128, 1], F32)
        gates = sb("gates_sb", [128, 16, 16], F32)
        rbb = sb("rbb_sb", [128, 16, 16], F32)
        rw = sb("rw_sb", [128, 8, 16], F32)
        statA = sb("statA", [128, 64], F32)
        cact = sb("cact_sb", [128, 8], BF16)
        arena_t = sb("arena", [128, ARENA_WORDS], F32)
        PS = es.enter_context(nc.psum_tensor("ps", [128, 4096], F32))
        ar = Arena(arena_t, ARENA_WORDS)
        ar2 = Arena(arena_t, ARENA_WORDS)

        def bank(b, n=512, parts=128):
            return PS[0:parts, b * 512:b * 512 + n]

        P.dma("sp", "c0", lambda e: e.dma_start(out=identf[:], in_=identf_d), writes=["identf"])
        P.dma("sp", "c0", lambda e: e.dma_start(out=trilm[:], in_=tril_d), writes=["trilm"])
        P.dma("sp", "c0", lambda e: e.dma_start(out=ind[:], in_=ind_d), writes=["ind"])
        P.dma("sp", "c0", lambda e: e.dma_start(out=rw[:], in_=rw_d.rearrange("(k p) e -> p k e", p=128)), writes=["rw"])
        P.dma("sp", "c0", lambda e: e.dma_start(
            out=rbb[:], in_=bass.AP(rb_d.tensor, 0, [[0, 128], [0, 16], [1, 16]])), writes=["rbb"])
        P.dve(lambda e: e.tensor_copy(out=identb[:], in_=identf[:]), reads=["identf"], writes=["identb"])
        P.dve(lambda e: e.memset(ones_bf[:], 1.0), writes=["ones_bf"])
        P.dve(lambda e: e.memset(eps_t[:], EPS), writes=["eps"])
        P.dve(lambda e: e.memset(nhalf[:], -0.5), writes=["nhalf"])

        ar.reset()
        cT = ar.f32([128, 8])
        stageR = [ar.bf16([128, 3072]) for _ in range(3)]
        modbrR = ar.f32([1, 3072])
        mrowR = ar.f32([1, 3072])
        rb33 = ar.f32([33, 8])
        btab = ar.f32([33, 3, 384])
        grow = ar.f32([8, 3, 384])
        P.dma("sp", "c0", lambda e: e.dma_start(out=cT, in_=cT_d), writes=["cT"])
        P.act(lambda e: e.activation(out=cact[:], in_=cT, func=AF.Silu), reads=["cT"], writes=["cact"])
        def mod_chunk(l, j, pb, stage_, modbr_, mrow_, bk, part=3, extra_w=()):
            if part & 1:
                P.dma("pool", "mw%d" % pb, lambda e: e.dma_start(
                    out=stage_, in_=modw_d[l, :, j * 512:(j + 1) * 512].rearrange("(k p) n -> p k n", p=128)),
                    writes=[("stage", pb)] + list(extra_w))
                P.dma("sp", "mb%d" % pb, lambda e: e.dma_start(out=modbr_, in_=modb_d[l:l + 1, j * 512:(j + 1) * 512]),
                      writes=[("modbr", pb)] + list(extra_w))
            if not (part & 2):
                return
            for k in range(8):
                P.pe(lambda e, k=k: e.matmul(bank(bk, 512, 1), lhsT=cact[:, k:k + 1], rhs=stage_[:, k, :],
                                             start=(k == 0), stop=(k == 7)),
                     reads=["cact", ("stage", pb)], writes=[("ps", bk)])
            P.dve(lambda e: e.tensor_tensor(out=mrow_, in0=bank(bk, 512, 1), in1=modbr_, op=ALU.add),
                  reads=[("ps", bk), ("modbr", pb)], writes=[("mrow", pb)])
            P.dma("sp", "ms%d" % pb, lambda e: e.dma_start(out=modscr[l:l + 1, j * 512:(j + 1) * 512], in_=mrow_),
                  reads=[("mrow", pb)], writes=[("modscr", l)])

        for i in range(16):
            P.dma("act", "x", lambda e, i=i: e.dma_start(out=x[:, i, :], in_=x_d[i * 128:(i + 1) * 128, :]),
                  writes=[("x", i)])
        P.dve(lambda e: e.memset(rb33, 1.0), writes=["rb33"])
        P.dma("sp", "c1", lambda e: e.dma_start(out=rb33[0:32, :], in_=relb_d), writes=["rb33"])
        P.dma("sp", "c1", lambda e: e.dma_start(out=btab, in_=btab_d.rearrange("d r j -> r d j")), writes=["btab"])
        jmat = ar.f32([128, 128])
        thk = [ar.f32([128, 8, 256]) for _ in range(3)]
        tfx = [ar.f32([128, 8, 256]) for _ in range(2)]
        P.dma("sp", "c1", lambda e: e.dma_start(out=jmat, in_=jmat_d), writes=["jmat"])
        for d in range(3):
            P.pe(lambda e, d=d: e.matmul(bank(6 + d % 2, 384, 8), lhsT=rb33[:, :], rhs=btab[:, d, :], start=True, stop=True),
                 reads=["rb33", "btab"], writes=[("ps", 6 + d % 2)])
            P.dve(lambda e, d=d: e.tensor_copy(out=grow[:, d, :], in_=bank(6 + d % 2, 384, 8)),
                  reads=[("ps", 6 + d % 2)], writes=["grow"])
        P.dma("act", "c2", lambda e: e.dma_start(out=gscr.rearrange("d h j -> h d j"), in_=grow),
              reads=["grow"], writes=["gscr"])
        for d in range(3):
            P.dma("act", "tk%d" % d, lambda e, d=d: e.dma_start(
                out=thk[d], in_=bass.AP(gscr_h, d * 8 * 384, [[1, 128], [384, 8], [1, 256]])),
                reads=["gscr"], writes=[("thk", d)])

        def texp_flip():
            nj = 0
            for d in range(3):
                pb = d % 2
                for j in range(4):
                    bk = 6 + nj % 2
                    nj += 1
                    P.pe(lambda e, d=d, j=j, bk=bk: e.matmul(bank(bk), lhsT=jmat, rhs=thk[d][:, 2 * j:2 * j + 2, :].rearrange("p a b -> p (a b)"),
                                                             start=True, stop=True),
                         reads=["jmat", ("thk", d)], writes=[("ps", bk)])
                    P.dve(lambda e, pb=pb, j=j, bk=bk: e.tensor_copy(out=tfx[pb][:, 2 * j:2 * j + 2, :].rearrange("p a b -> p (a b)"), in_=bank(bk)),
                          reads=[("ps", bk)], writes=[("tfx", pb)])
                P.dma("act", "tx%d" % pb, lambda e, d=d, pb=pb: e.dma_start(out=texp[d].rearrange("h p q -> p h q"), in_=tfx[pb]),
                      reads=[("tfx", pb)], writes=["texp"])

        nblk = 0
        for hh in range(2):
            P.dma("sp", "mb0", lambda e, hh=hh: e.dma_start(out=modbrR, in_=modb_d[0:1, hh * 3072:(hh + 1) * 3072]),
                  writes=["modbrR"])
            for k in range(8):
                sb_ = nblk % 3
                nblk += 1
                P.dma("pool", "mr%d" % sb_, lambda e, hh=hh, k=k, sb_=sb_: e.dma_start(
                    out=stageR[sb_], in_=modw_d[0, k * 128:(k + 1) * 128, hh * 3072:(hh + 1) * 3072]),
                    writes=[("stageR", sb_)])
                for j in range(6):
                    P.pe(lambda e, k=k, j=j, sb_=sb_: e.matmul(bank(j, 512, 1), lhsT=cact[:, k:k + 1], rhs=stageR[sb_][:, j * 512:(j + 1) * 512],
                                                               start=(k == 0), stop=(k == 7)),
                         reads=["cact", ("stageR", sb_)], writes=[("ps", j)])
            for j in range(6):
                P.dve(lambda e, j=j: e.tensor_tensor(out=mrowR[:, j * 512:(j + 1) * 512], in0=bank(j, 512, 1),
                                                     in1=modbrR[:, j * 512:(j + 1) * 512], op=ALU.add),
                      reads=[("ps", j), "modbrR"], writes=["mrowR"])
            P.dma("sp", "ms0", lambda e, hh=hh: e.dma_start(out=modscr[0:1, hh * 3072:(hh + 1) * 3072], in_=mrowR),
                  reads=["mrowR"], writes=[("modscr", 0)])
            if hh == 0:
                texp_flip()

        ssqN = statA[:, 16:32]

        def norm_phase(l, which, pre=None, post=None, have_ssq=False):
            P.barrier()
            ar.reset()
            if pre is not None:
                pre()
            gmod = ar.f32([128, 1024])
            ngb = ar.f32([128, 1024])
            shb = ar.f32([128, 1024])
            h32 = [ar.f32([128, 1024]) for _ in range(2)]
            junk = ar.bf16([128, 1024])
            ssq = ssqN
            rstd = ar.f32([128, 16])
            xh32 = [ar.f32([128, 8, 128]) for _ in range(2)] if which == 2 else None
            off_sh, off_sc = (0, 1024) if which == 1 else (3072, 4096)
            ng_d = n1g_d if which == 1 else n2g_d
            P.dma("sp", "n0", lambda e: e.dma_start(out=gmod, in_=modscr[l, off_sc:off_sc + 1024].partition_broadcast(128)),
                  writes=["gmod"])
            P.dma("sp", "n0", lambda e: e.dma_start(out=ngb, in_=ng_d[l, :].partition_broadcast(128)), writes=["ngb"])
            P.dma("sp", "n0", lambda e: e.dma_start(out=shb, in_=modscr[l, off_sh:off_sh + 1024].partition_broadcast(128)),
                  writes=["shb"])
            P.dve(lambda e: e.scalar_tensor_tensor(out=gmod, in0=gmod, scalar=1.0, in1=ngb, op0=ALU.add, op1=ALU.mult),
                  reads=["gmod", "ngb"], writes=["gmod"])
            if not have_ssq:
                for i in range(16):
                    P.act(lambda e, i=i: e.activation(out=junk, in_=x[:, i, :], func=AF.Square, accum_out=ssq[:, i:i + 1]),
                          reads=[("x", i)], writes=["junk", ("ssq", i)])
            P.act(lambda e: e.activation(out=rstd, in_=ssq, func=AF.Sqrt, bias=eps_t[:, 0:1], scale=1.0 / 1024),
                  reads=[("ssq", i) for i in range(16)] + ["eps"], writes=["rstd0"])
            P.dve(lambda e: e.reciprocal(out=rstd, in_=rstd), reads=["rstd0"], writes=["rstd"])
            for i in range(16):
                hb = h32[i % 2]
                pp = i % 2
                P.dve(lambda e, i=i, hb=hb: e.scalar_tensor_tensor(out=hb, in0=x[:, i, :], scalar=rstd[:, i:i + 1], in1=gmod,
                                                                    op0=ALU.mult, op1=ALU.mult),
                      reads=[("x", i), "rstd", "gmod"], writes=[("h32", pp)])
                P.dve(lambda e, hb=hb: e.tensor_tensor(out=hb, in0=hb, in1=shb, op=ALU.add),
                      reads=[("h32", pp), "shb"], writes=[("h32", pp)])
                for c in range(8):
                    P.pe(lambda e, c=c, hb=hb, pp=pp: e.transpose(out=PS[:, pp * 1024 + c * 128:pp * 1024 + (c + 1) * 128],
                                                                  in_=hb[:, c * 128:(c + 1) * 128], identity=identf[:]),
                         reads=[("h32", pp), "identf"], writes=[("ps", 2 * pp + c // 4)])
                if which == 1:
                    P.act(lambda e, i=i, pp=pp: e.activation(out=hT[:, :, i * 128:(i + 1) * 128],
                                                             in_=PS[:, pp * 1024:(pp + 1) * 1024].rearrange("p (c t) -> p c t", c=8),
                                                             func=AF.Copy),
                          reads=[("ps", 2 * pp), ("ps", 2 * pp + 1)], writes=[("hT", i)])
                if which == 2:
                    xb = xh32[pp]
                    P.dve(lambda e, pp=pp, xb=xb: e.tensor_copy(out=xb, in_=PS[:, pp * 1024:(pp + 1) * 1024].rearrange("p (c t) -> p c t", c=8)),
                          reads=[("ps", 2 * pp), ("ps", 2 * pp + 1)], writes=[("xh32", pp)])
                    P.act(lambda e, i=i, xb=xb: e.activation(out=hT[:, :, i * 128:(i + 1) * 128], in_=xb, func=AF.Copy),
                          reads=[("xh32", pp)], writes=[("hT", i)])
                    for c in range(8):
                        P.pe(lambda e, i=i, c=c, xb=xb: e.matmul(PS[:, 4 * 512 + i * 16:4 * 512 + (i + 1) * 16], lhsT=xb[:, c, :],
                                                                 rhs=rw[:, c, :], start=(c == 0), stop=(c == 7)),
                             reads=[("xh32", pp), "rw"], writes=[("ps", 4)])

        def norm_phase_post(post):
            if post is not None:
                post()

        def router_phase():
            T = lambda: ar.f32([128, 16, 16])
            L, E_, pr, sel, t1, t2, t3, selm = T(), T(), T(), T(), T(), T(), T(), T()
            mx = ar.f32([128, 16]); sm = ar.f32([128, 16])
            g4 = ar.f32([128, 16, 4]); p6 = [ar.f32([128, 16, 4]) for _ in range(6)]
            gmx = ar.f32([128, 16]); gm = ar.f32([128, 16, 4])
            m1 = ar.f32([128, 16]); m2 = ar.f32([128, 16])
            bc = lambda a, n: a.unsqueeze(2).to_broadcast([128, 16, n])
            v = lambda e: e
            P.dve(lambda e: e.tensor_copy(out=L, in_=PS[:, 2048:2048 + 256].rearrange("p (a b) -> p a b", a=16)),
                  reads=[("ps", 4)], writes=["rt"])
            seq = []
            seq.append(lambda e: e.tensor_reduce(out=mx, in_=L, axis=AX.X, op=ALU.max))
            seq.append(lambda e: e.tensor_tensor(out=t1, in0=L, in1=bc(mx, 16), op=ALU.subtract))
            for f in seq:
                P.dve(f, reads=["rt"], writes=["rt"])
            P.act(lambda e: e.activation(out=E_, in_=t1, func=AF.Exp), reads=["rt"], writes=["rt"])
            seq = []
            seq.append(lambda e: e.tensor_reduce(out=sm, in_=E_, axis=AX.X, op=ALU.add))
            seq.append(lambda e: e.reciprocal(out=sm, in_=sm))
            seq.append(lambda e: e.tensor_tensor(out=pr, in0=E_, in1=bc(sm, 16), op=ALU.mult))
            seq.append(lambda e: e.tensor_tensor(out=sel, in0=pr, in1=rbb[:], op=ALU.add))
            s4 = sel.rearrange("p a (g k) -> p a g k", k=4)
            pairs = [(0, 1), (0, 2), (0, 3), (1, 2), (1, 3), (2, 3)]
            for q, (a, b) in enumerate(pairs):
                seq.append(lambda e, q=q, a=a, b=b: e.tensor_tensor(out=p6[q], in0=s4[:, :, :, a], in1=s4[:, :, :, b], op=ALU.add))
            seq.append(lambda e: e.tensor_tensor(out=g4, in0=p6[0], in1=p6[1], op=ALU.max))
            for q in range(2, 6):
                seq.append(lambda e, q=q: e.tensor_tensor(out=g4, in0=g4, in1=p6[q], op=ALU.max))
            seq.append(lambda e: e.tensor_reduce(out=gmx, in_=g4, axis=AX.X, op=ALU.max))
            seq.append(lambda e: e.tensor_tensor(out=gm, in0=g4, in1=bc(gmx, 4), op=ALU.is_ge))
            gm16 = gm.unsqueeze(3).to_broadcast([128, 16, 4, 4])
            sm4 = selm.rearrange("p a (g k) -> p a g k", k=4)
            seq.append(lambda e: e.scalar_tensor_tensor(out=sm4, in0=s4, scalar=100.0, in1=gm16, op0=ALU.add, op1=ALU.mult))
            seq.append(lambda e: e.tensor_scalar(out=selm, in0=selm, scalar1=-100.0, scalar2=None, op0=ALU.add))
            seq.append(lambda e: e.tensor_reduce(out=m1, in_=selm, axis=AX.X, op=ALU.max))
            seq.append(lambda e: e.tensor_tensor(out=t2, in0=selm, in1=bc(m1, 16), op=ALU.is_ge))
            seq.append(lambda e: e.scalar_tensor_tensor(out=t3, in0=t2, scalar=-1000.0, in1=selm, op0=ALU.mult, op1=ALU.add))
            seq.append(lambda e: e.tensor_reduce(out=m2, in_=t3, axis=AX.X, op=ALU.max))
            seq.append(lambda e: e.tensor_tensor(out=t2, in0=selm, in1=bc(m2, 16), op=ALU.is_ge))
            seq.append(lambda e: e.tensor_tensor(out=t3, in0=t2, in1=pr, op=ALU.mult))
            seq.append(lambda e: e.tensor_reduce(out=sm, in_=t3, axis=AX.X, op=ALU.add))
            seq.append(lambda e: e.reciprocal(out=sm, in_=sm))
            seq.append(lambda e: e.tensor_tensor(out=gates[:], in0=t3, in1=bc(sm, 16), op=ALU.mult))
            for f in seq:
                P.dve(f, reads=["rt", "rbb"], writes=["rt", "gates"])

        def dump_and_finish():
            for i in range(16):
                P.dma("sp", "dbg", lambda e, i=i: e.dma_start(out=dbg_x[i * 128:(i + 1) * 128, :], in_=x[:, i, :]),
                      reads=[("x", i)])
            P.dma("sp", "dbg", lambda e: e.dma_start(out=dbg_hT, in_=hT[:]), reads=[("hT", i) for i in range(16)])
            P.dma("sp", "dbg", lambda e: e.dma_start(out=dbg_g, in_=gates[:].rearrange("p a b -> p (a b)")), reads=["gates"])
            P.emit(final_wait_streams=["dbg"])

        def layer(l):
            ar2.reset()
            vball = ar2.bf16([128, 16, 512])
            gub = ar2.bf16([128, 16, 512])
            w_uva = ar2.bf16([128, 8, 1024])
            w_oa = ar2.bf16([128, 4, 1024])
            WT = ar2.bf16([128, 8, 128])
            oaT = [ar2.bf16([128, 4, 128]) for _ in range(2)]
            g1b = ar2.f32([128, 1024])
            Wld = ar2.f32([128, 8, 128])
            lngb = ar2.f32([128, 512]); lnbb = ar2.f32([128, 512])
            gv = [ar2.f32([128, 512]) for _ in range(2)]
            vn = [ar2.f32([128, 512]) for _ in range(2)]
            oa = [ar2.f32([128, 512]) for _ in range(2)]
            junkA = ar2.bf16([128, 512])
            bsT = ar2.f32([8, 128])
            gabf = ar2.f32([128, 8])
            st6 = ar2.f32([128, 2, 6]); mv = ar2.f32([128, 2, 2]); vpe = ar2.f32([128, 2]); rsv = ar2.f32([128, 2])
            ssqa = ar2.f32([128, 16]); rsa = ar2.f32([128, 16])
            assert vball is not None

            def A_pre():
                for h2 in range(2):
                    P.dma("pool", "wA", lambda e, h2=h2: e.dma_start(
                        out=w_uva[:, :, h2 * 512:(h2 + 1) * 512],
                        in_=win_d[l, :, h2 * 512:(h2 + 1) * 512].rearrange("(k p) n -> p k n", p=128)), writes=[("w_uva", h2)])
                P.dma("pool", "wA", lambda e: e.dma_start(out=w_oa, in_=wout_d[l, 0:512, :].rearrange("(k p) n -> p k n", p=128)),
                      writes=["w_oa"])
                P.dma("sp", "a0", lambda e: e.dma_start(out=g1b, in_=modscr[l, 2048:3072].partition_broadcast(128)), writes=["g1b"])
                P.dma("sp", "a0", lambda e: e.dma_start(out=gabf, in_=gab_d[l]), writes=["gabf"])
                P.dma("sp", "a0", lambda e: e.dma_start(out=Wld, in_=ws_d[l].rearrange("g t s -> t g s")), writes=["Wld"])
                P.dma("sp", "a0", lambda e: e.dma_start(out=bsT, in_=bs_d[l]), writes=["bsT"])
                P.dma("sp", "a0", lambda e: e.dma_start(out=lngb, in_=lng_d[l, :].partition_broadcast(128)), writes=["lngb"])
                P.dma("sp", "a0", lambda e: e.dma_start(out=lnbb, in_=lnb_d[l, :].partition_broadcast(128)), writes=["lnbb"])

            def A_post():
                for c in range(4):
                    P.dve(lambda e, c=c: e.scalar_tensor_tensor(out=w_oa[:, c, :], in0=w_oa[:, c, :], scalar=gabf[:, c:c + 1], in1=g1b,
                                                                 op0=ALU.mult, op1=ALU.mult),
                          reads=["w_oa", "gabf", "g1b"], writes=["w_oa"])
                for g in range(8):
                    P.pe(lambda e, g=g: e.transpose(out=PS[:, 3072 + g * 128:3072 + (g + 1) * 128], in_=Wld[:, g, :], identity=identf[:]),
                         reads=["Wld", "identf"], writes=[("ps", 6 + g // 4)])
                for g in range(8):
                    P.dve(lambda e, g=g: e.tensor_tensor(out=WT[:, g, :], in0=PS[:, 3072 + g * 128:3072 + (g + 1) * 128], in1=trilm[:], op=ALU.mult),
                          reads=[("ps", 6 + g // 4), "trilm"], writes=["WT"])


            norm_phase(l, 1, pre=A_pre, have_ssq=(l > 0))
            assert ar.off <= 8 * 1024, ar.off
            A_post()
            if stop == ("N1", l):
                P.barrier(); dump_and_finish(); return True

            P.barrier()
            def A1(i):
                pp = i % 2
                for k in range(8):
                    P.pe(lambda e, k=k: e.matmul(bank(pp), lhsT=hT[:, k, i * 128:(i + 1) * 128], rhs=w_uva[:, k, 0:512],
                                                 start=(k == 0), stop=(k == 7)),
                         reads=[("hT", i), ("w_uva", 0)], writes=[("ps", pp)])
                for k in range(8):
                    P.pe(lambda e, k=k: e.matmul(bank(2 + pp), lhsT=hT[:, k, i * 128:(i + 1) * 128], rhs=w_uva[:, k, 512:1024],
                                                 start=(k == 0), stop=(k == 7)),
                         reads=[("hT", i), ("w_uva", 1)], writes=[("ps", 2 + pp)])
                P.act(lambda e: e.activation(out=gub[:, i, :], in_=bank(pp), func=AF.Gelu), reads=[("ps", pp)], writes=[("gub", i)])
                P.act(lambda e: e.activation(out=gv[pp], in_=bank(2 + pp), func=AF.Gelu), reads=[("ps", 2 + pp)], writes=[("gv", pp)])
                P.dve(lambda e: e.bn_stats(out=st6[:, pp, :], in_=gv[pp]), reads=[("gv", pp)], writes=[("st6", pp)])
                P.dve(lambda e: e.bn_aggr(out=mv[:, pp, :], in_=st6[:, pp, :]), reads=[("st6", pp)], writes=[("mv", pp)])
                P.dve(lambda e: e.tensor_scalar(out=vpe[:, pp:pp + 1], in0=mv[:, pp, 1:2], scalar1=EPS, scalar2=None, op0=ALU.add),
                      reads=[("mv", pp)], writes=[("vpe", pp)])
                P.pool(lambda e: e.tensor_tensor(out=rsv[:, pp:pp + 1], in0=vpe[:, pp:pp + 1], in1=nhalf[:, 0:1], op=ALU.pow),
                       reads=[("vpe", pp), "nhalf"], writes=[("rsv", pp)])
                P.dve(lambda e: e.tensor_scalar(out=vn[pp], in0=gv[pp], scalar1=mv[:, pp, 0:1], scalar2=rsv[:, pp:pp + 1],
                                                op0=ALU.subtract, op1=ALU.mult),
                      reads=[("gv", pp), ("mv", pp), ("rsv", pp)], writes=[("vn", pp)])
                P.dve(lambda e: e.tensor_tensor(out=vn[pp], in0=vn[pp], in1=lngb, op=ALU.mult),
                      reads=[("vn", pp), "lngb"], writes=[("vn", pp)])
                P.dve(lambda e: e.tensor_tensor(out=vball[:, i, :], in0=vn[pp], in1=lnbb, op=ALU.add),
                      reads=[("vn", pp), "lnbb"], writes=[("vball", i)])

            def A2(i):
                pp = i % 2
                P.pe(lambda e: e.matmul(bank(pp), lhsT=bsT[:, :], rhs=ind[:, :], start=True, stop=False),
                     reads=["bsT", "ind"], writes=[("ps", pp)])
                for g in range(8):
                    P.pe(lambda e, g=g: e.matmul(PS[:, pp * 512 + g * 64:pp * 512 + (g + 1) * 64], lhsT=WT[:, g, :],
                                                 rhs=vball[:, i, g * 64:(g + 1) * 64], start=False, stop=(g == 7)),
                         reads=["WT", ("vball", i)], writes=[("ps", pp)])
                P.dve(lambda e: e.tensor_tensor(out=oa[pp], in0=bank(pp), in1=gub[:, i, :], op=ALU.mult),
                      reads=[("ps", pp), ("gub", i)], writes=[("oa", pp)])
                P.act(lambda e: e.activation(out=junkA, in_=oa[pp], func=AF.Square, accum_out=ssqa[:, i:i + 1]),
                      reads=[("oa", pp)], writes=["junkA", ("ssqa", i)])
                P.dve(lambda e: e.tensor_scalar(out=rsa[:, i:i + 1], in0=ssqa[:, i:i + 1], scalar1=1.0 / 512, scalar2=EPS,
                                                op0=ALU.mult, op1=ALU.add),
                      reads=[("ssqa", i)], writes=[("rsa0", i)])
                P.pool(lambda e: e.tensor_tensor(out=rsa[:, i:i + 1], in0=rsa[:, i:i + 1], in1=nhalf[:, 0:1], op=ALU.pow),
                       reads=[("rsa0", i), "nhalf"], writes=[("rsa", i)])

            def A2b(i):
                pp = i % 2
                for c in range(4):
                    P.pe(lambda e, c=c: e.transpose(out=PS[:, (2 + pp) * 512 + c * 128:(2 + pp) * 512 + (c + 1) * 128],
                                                    in_=oa[pp][:, c * 128:(c + 1) * 128], identity=identf[:]),
                         reads=[("oa", pp), "identf"], writes=[("ps", 2 + pp)])
                P.act(lambda e: e.activation(out=oaT[pp], in_=bank(2 + pp).rearrange("p (c t) -> p c t", c=4), func=AF.Copy),
                      reads=[("ps", 2 + pp)], writes=[("oaT", pp)])

            def A3(i):
                pp = i % 2
                for hf in range(2):
                    bk = 4 + 2 * pp + hf
                    for c in range(4):
                        P.pe(lambda e, c=c, hf=hf, bk=bk: e.matmul(bank(bk), lhsT=oaT[pp][:, c, :], rhs=w_oa[:, c, hf * 512:(hf + 1) * 512],
                                                                   start=(c == 0), stop=(c == 3)),
                             reads=[("oaT", pp), "w_oa"], writes=[("ps", bk)])
                    P.dve(lambda e, hf=hf, bk=bk: e.scalar_tensor_tensor(out=x[:, i, hf * 512:(hf + 1) * 512], in0=bank(bk),
                                                                         scalar=rsa[:, i:i + 1], in1=x[:, i, hf * 512:(hf + 1) * 512],
                                                                         op0=ALU.mult, op1=ALU.add),
                          reads=[("ps", bk), ("rsa", i), ("x", i)], writes=[("x", i)])

            for i in range(16):
                A1(i)
            for s in range(18):
                if s < 16:
                    A2(s)
                if 0 <= s - 1 < 16:
                    A2b(s - 1)
                if 0 <= s - 2 < 16:
                    A3(s - 2)
            if stop == ("A", l):
                P.barrier(); dump_and_finish(); return True

            P.barrier()
            ar.reset()
            ost = [ar.bf16([128, 1024]) for _ in range(2)]
            wq = ar.bf16([128, 8, 128]); wk = ar.bf16([128, 8, 128]); wv = ar.bf16([128, 8, 128])
            QTd = [[ar.bf16([128, 2048]) for _ in range(3)] for _ in range(2)]
            KTd = [ar.bf16([128, 2048]) for _ in range(3)]
            VT = ar.bf16([128, 2048])
            Vb = [ar.bf16([128, 16, 2, 65]) for _ in range(3)]
            Pt = [ar.bf16([128, 512]) for _ in range(NSB)]
            obtok = ar.bf16([128, 16, 128])
            Tt = [[ar.f32([128, 512]) for _ in range(3)] for _ in range(2)]
            E32 = [ar.f32([128, 512]) for _ in range(NSB)]
            accS = [ar.f32([65, 512]) for _ in range(2)]
            rden = ar.f32([128, 4])
            ssqb = ar.f32([128, 16, 4]); rsb = statA[:, 0:16]
            junkB = ar.bf16([128, 128])
            b_end = ar.off
            for d in range(3):
                P.dve(lambda e, d=d: e.memset(Vb[d][:, :, :, 64:65], 1.0), writes=[("Vb", d)])
            for di in range(3):
                P.dve(lambda e, di=di: e.memset(QTd[0][di][64:128, :], 0.0), writes=["qz0"])
                P.dve(lambda e, di=di: e.memset(QTd[1][di][0:64, :], 0.0), writes=["qz1"])
            def B_proj(hp):
                for nm, wt, col in (("wq", wq, 1024), ("wk", wk, 1536), ("wv", wv, 2048)):
                    P.dma("pool", "wB", lambda e, wt=wt, col=col: e.dma_start(
                        out=wt, in_=win_d[l, :, col + hp * 128:col + (hp + 1) * 128].rearrange("(k p) n -> p k n", p=128)),
                        writes=[nm])
                nbk = 0
                for tb in range(4):
                    for nm, wt in (("wq", wq), ("wk", wk), ("wv", wv)):
                        bk = 6 + (nbk % 2)
                        nbk += 1
                        for k in range(8):
                            P.pe(lambda e, k=k, wt=wt, bk=bk, tb=tb: e.matmul(bank(bk), lhsT=wt[:, k, :], rhs=hT[:, k, tb * 512:(tb + 1) * 512],
                                                                              start=(k == 0), stop=(k == 7)),
                                 reads=[nm], writes=[("ps", bk)])
                        if nm == "wk":
                            P.act(lambda e, bk=bk, tb=tb: e.activation(out=KTd[0][:, tb * 512:(tb + 1) * 512], in_=bank(bk), func=AF.Copy),
                                  reads=[("ps", bk)], writes=[("wkT", tb)])
                        elif nm == "wv":
                            P.dve(lambda e, bk=bk, tb=tb: e.tensor_copy(out=VT[:, tb * 512:(tb + 1) * 512], in_=bank(bk)),
                                  reads=[("ps", bk)], writes=[("VT", tb)])
                        else:
                            for hh in range(2):
                                P.act(lambda e, bk=bk, tb=tb, hh=hh: e.activation(
                                    out=QTd[hh][0][64 * hh:64 * hh + 64, tb * 512:(tb + 1) * 512],
                                    in_=PS[64 * hh:64 * hh + 64, bk * 512:(bk + 1) * 512], func=AF.Copy, scale=0.125),
                                    reads=[("ps", bk)], writes=[("wqT", tb)])
                def deint(t, d):
                    return t.rearrange("p (r i) -> p r i", r=d), None
                for di in (1, 2):
                    d = CONFIGS[di][1]
                    P.pool(lambda e, di=di, d=d: e.tensor_copy(out=KTd[di].rearrange("p (r i) -> p r i", r=d),
                                                               in_=KTd[0].rearrange("p (i r) -> p r i", r=d)),
                           reads=[("wkT", j) for j in range(4)], writes=[("wkTd", di)])
                    P.pool(lambda e, di=di, d=d: e.tensor_copy(out=QTd[0][di][0:64, :].rearrange("p (r i) -> p r i", r=d),
                                                               in_=QTd[0][0][0:64, :].rearrange("p (i r) -> p r i", r=d)),
                           reads=[("wqT", j) for j in range(4)], writes=[("wqTd", 0, di)])
                for di in (1, 2):
                    d = CONFIGS[di][1]
                    P.pool(lambda e, di=di, d=d: e.tensor_copy(out=QTd[1][di][64:128, :].rearrange("p (r i) -> p r i", r=d),
                                                               in_=QTd[1][0][64:128, :].rearrange("p (i r) -> p r i", r=d)),
                           reads=[("wqT", j) for j in range(4)], writes=[("wqTd", 1, di)])
                for di, (win, d) in enumerate(CONFIGS):
                    nb = 16 // d
                    for q8 in range(2):
                        bk = 6 + (nbk % 2)
                        nbk += 1
                        pbf = bank(bk).bitcast(BF16)
                        for t8 in range(8):
                            tile = q8 * 8 + t8
                            r, m = tile // nb, tile % nb
                            t0 = r + d * 128 * m
                            P.pe(lambda e, t0=t0, d=d, pbf=pbf, t8=t8: e.transpose(
                                out=pbf[:, t8 * 128:(t8 + 1) * 128], in_=VT[:, t0:t0 + d * 127 + 1:d], identity=identb[:]),
                                reads=[("VT", j) for j in range(4)] + ["identb"], writes=[("ps", bk)])
                        P.act(lambda e, di=di, q8=q8, pbf=pbf: e.activation(
                            out=Vb[di][:, q8 * 8:(q8 + 1) * 8, :, 0:64],
                            in_=pbf.rearrange("p (a h e) -> p a h e", a=8, h=2), func=AF.Copy),
                            reads=[("ps", bk)], writes=[("Vb", di)])

            def B_head(hp, hl, base):
                h = 2 * hp + hl
                hpar = h % 2
                p0 = 64 * hl
                for di in range(3):
                    P.dma("sp", "tt%d" % hpar, lambda e, di=di: e.dma_start(
                        out=Tt[hpar][di].rearrange("p (a b) -> p a b", a=2),
                        in_=bass.AP(texp.tensor, (di * 8 + h) * 128 * 256, [[256, 128], [0, 2], [1, 256]])),
                        writes=[("Tt", hpar, di)])
                started = [False] * 4
                steps = []
                for di, (win, d) in enumerate(CONFIGS):
                    nb = 16 // d
                    if nb >= 2:
                        for r in range(d):
                            for m in range(0, nb, 2):
                                steps.append((di, d, nb, [(r, m, 256, 0), (r, m + 1, 256 if m + 2 < nb else 128, 256)]))
                    else:
                        for r in range(0, d, 2):
                            steps.append((di, d, nb, [(r, 0, 128, 0), (r + 1, 0, 128, 256)]))

                def region(ap512, subs):
                    if subs[0][2] == 256:
                        return ap512[:, 0:256 + subs[1][2]]
                    return ap512.rearrange("p (a b) -> p a b", a=2)[:, :, 0:128]

                def S_step(st, sidx):
                    di, d, nb, subs = st
                    sb_ = sidx % NSB
                    bk = 4 + sb_
                    for (r, m, width, coff) in subs:
                        bs0 = r * (2048 // d) + 128 * m
                        P.pe(lambda e, bs0=bs0, width=width, coff=coff: e.matmul(
                            PS[:, bk * 512 + coff:bk * 512 + coff + width], lhsT=KTd[di][:, bs0:bs0 + 128],
                            rhs=QTd[hl][di][:, bs0:bs0 + width], start=True, stop=True),
                            reads=[("wkT", j) for j in range(4)] + [("wqT", j) for j in range(4)] +
                            ([("wkTd", di), ("wqTd", hl, di)] if di > 0 else []), writes=[("ps", bk)])
                    P.dve(lambda e: e.tensor_tensor(out=region(E32[sb_], subs), in0=region(bank(bk), subs),
                                                    in1=region(Tt[hpar][di], subs), op=ALU.add),
                          reads=[("ps", bk), ("Tt", hpar, di)], writes=[("E32", sb_)])
                    P.act(lambda e: e.activation(out=region(Pt[sb_], subs), in_=region(E32[sb_], subs), func=AF.Exp),
                          reads=[("E32", sb_)], writes=[("Pt", sb_)])

                def PV_step(st, sidx):
                    di, d, nb, subs = st
                    sb_ = sidx % NSB
                    for (r, m, width, coff) in subs:
                        tile = r * nb + m
                        lhs = Vb[di][:, tile, hl, :]
                        for qb in ((m, m + 1) if m + 1 < nb else (m,)):
                            c0 = coff + (qb - m) * 128
                            if d < 16:
                                if d == 1:
                                    bk, cs, step = qb // 4, (qb % 4) * 128, 1
                                else:
                                    bk, cs, step = qb, r, 4
                                pieces = [(bk, cs, step, 128, c0)]
                            else:
                                pieces = [(b4, r, 16, 32, c0 + 32 * b4) for b4 in range(4)]
                            for (bk, cs, step, cnt, pc) in pieces:
                                st_flag = not started[bk]
                                started[bk] = True
                                P.pe(lambda e, bk=bk, cs=cs, step=step, cnt=cnt, pc=pc, st_flag=st_flag, lhs=lhs: e.matmul(
                                    PS[0:65, bk * 512 + cs:bk * 512 + cs + step * (cnt - 1) + 1:step], lhsT=lhs,
                                    rhs=Pt[sb_][:, pc:pc + cnt], start=st_flag, stop=False, skip_group_check=True),
                                    reads=[("Vb", di), ("Pt", sb_)], writes=[("ps", bk)])

                SK = NSB - 1
                for j in range(len(steps) + SK):
                    if j < len(steps):
                        S_step(steps[j], base + j)
                    if j - SK >= 0:
                        PV_step(steps[j - SK], base + j - SK)
                if stop == ("Bs", l):
                    return len(steps)
                for b4 in range(4):
                    ab = accS[b4 % 2]
                    P.act(lambda e, b4=b4, ab=ab: e.activation(out=ab, in_=bank(b4, 512, 65), func=AF.Copy),
                          reads=[("ps", b4)], writes=[("accS", b4 % 2)])
                    bk = 6 + (b4 % 2)
                    for j in range(4):
                        P.pe(lambda e, j=j, ab=ab, bk=bk: e.transpose(out=PS[:, bk * 512 + j * 65:bk * 512 + (j + 1) * 65],
                                                                      in_=ab[:, j * 128:(j + 1) * 128], identity=identf[0:65, 0:65]),
                             reads=[("accS", b4 % 2), "identf"], writes=[("ps", bk)])
                    pv = bank(bk, 260).rearrange("p (j e) -> p j e", j=4)
                    P.dve(lambda e, pv=pv: e.reciprocal(out=rden.unsqueeze(2), in_=pv[:, :, 64:65]),
                          reads=[("ps", bk)], writes=["rden"])
                    P.dve(lambda e, pv=pv, b4=b4: e.tensor_tensor(out=obtok[:, b4 * 4:(b4 + 1) * 4, p0:p0 + 64], in0=pv[:, :, 0:64],
                                                                  in1=rden.unsqueeze(2).to_broadcast([128, 4, 64]), op=ALU.mult),
                          reads=[("ps", bk), "rden"], writes=[("obtok", hl)])
                return len(steps)

            def B_pair_end(hp):
                for i in range(16):
                    P.act(lambda e, i=i: e.activation(out=junkB, in_=obtok[:, i, :], func=AF.Square, accum_out=ssqb[:, i, hp:hp + 1]),
                          reads=[("obtok", 0), ("obtok", 1)], writes=["junkB", ("ssqb", hp)])
                for i8 in range(2):
                    bk = 6 + i8
                    pbf = bank(bk).bitcast(BF16)
                    for j in range(8):
                        i = i8 * 8 + j
                        P.pe(lambda e, i=i, j=j, pbf=pbf: e.transpose(out=pbf[:, j * 128:(j + 1) * 128], in_=obtok[:, i, :], identity=identb[:]),
                             reads=[("obtok", 0), ("obtok", 1), "identb"], writes=[("ps", bk)])
                    P.act(lambda e, i8=i8, pbf=pbf: e.activation(out=ost[i8], in_=pbf, func=AF.Copy),
                          reads=[("ps", bk)], writes=[("ost", i8)])
                    P.dma("sp", "ob", lambda e, i8=i8: e.dma_start(out=obT_d[:, hp, i8 * 1024:(i8 + 1) * 1024], in_=ost[i8]),
                          reads=[("ost", i8)], writes=["obT_d"])

            sidx = 0
            for hp_ in range(4):
                B_proj(hp_)
                if stop == ("Bp", l):
                    P.barrier(); dump_and_finish(); return True
                for hl_ in range(2):
                    sidx += B_head(hp_, hl_, sidx)
                    if stop in (("Bs", l), ("Bh", l)):
                        P.barrier(); dump_and_finish(); return True
                B_pair_end(hp_)
                if stop == ("Be", l):
                    P.barrier(); dump_and_finish(); return True
            P.dve(lambda e: e.tensor_reduce(out=rsb, in_=ssqb, axis=AX.X, op=ALU.add), reads=[("ssqb", j) for j in range(4)], writes=["rsb0"])
            P.dve(lambda e: e.tensor_scalar(out=rsb, in0=rsb, scalar1=1.0 / 512, scalar2=EPS, op0=ALU.mult, op1=ALU.add),
                  reads=["rsb0"], writes=["rsb1"])
            P.pool(lambda e: e.tensor_tensor(out=rsb, in0=rsb, in1=nhalf[:, 0:1].to_broadcast([128, 16]), op=ALU.pow),
                   reads=["rsb1", "nhalf"], writes=["rsb"])
            if stop == ("B1", l):
                P.barrier()
                P.dma("sp", "dbg", lambda e: e.dma_start(out=dbg_obT, in_=obT_d))
                P.barrier(); dump_and_finish(); return True
            P.barrier()
            ar.reset()
            obT = ar.bf16([128, 4, 2048])
            P.dma("sp", "a0", lambda e: e.dma_start(out=obT, in_=obT_d), writes=["obT"])
            w_ob = ar.bf16([128, 4, 1024])
            junkb = ar.bf16([128, 1024])
            g1b2 = ar.f32([128, 1024])
            gabf2 = ar.f32([128, 8])
            P.dma("pool", "wA", lambda e: e.dma_start(out=w_ob, in_=wout_d[l, 512:1024, :].rearrange("(k p) n -> p k n", p=128)),
                  writes=["w_ob"])
            P.dma("sp", "a0", lambda e: e.dma_start(out=g1b2, in_=modscr[l, 2048:3072].partition_broadcast(128)), writes=["g1b2"])
            P.dma("sp", "a0", lambda e: e.dma_start(out=gabf2, in_=gab_d[l]), writes=["gabf2"])
            for c in range(4):
                P.dve(lambda e, c=c: e.scalar_tensor_tensor(out=w_ob[:, c, :], in0=w_ob[:, c, :], scalar=gabf2[:, 4 + c:5 + c], in1=g1b2,
                                                             op0=ALU.mult, op1=ALU.mult),
                      reads=["w_ob", "gabf2", "g1b2"], writes=["w_ob"])
            for i in range(16):
                for hf in range(2):
                    bk = 2 * (i % 2) + hf
                    for c in range(4):
                        P.pe(lambda e, c=c, hf=hf, bk=bk, i=i: e.matmul(bank(bk), lhsT=obT[:, c, i * 128:(i + 1) * 128],
                                                                        rhs=w_ob[:, c, hf * 512:(hf + 1) * 512], start=(c == 0), stop=(c == 3)),
                             reads=["w_ob", "obT"], writes=[("ps", bk)])
                    P.dve(lambda e, hf=hf, bk=bk, i=i: e.scalar_tensor_tensor(out=x[:, i, hf * 512:(hf + 1) * 512], in0=bank(bk),
                                                                              scalar=rsb[:, i:i + 1], in1=x[:, i, hf * 512:(hf + 1) * 512],
                                                                              op0=ALU.mult, op1=ALU.add),
                          reads=[("ps", bk), ("x", i)], writes=[("x", i)])
                P.act(lambda e, i=i: e.activation(out=junkb, in_=x[:, i, :], func=AF.Square, accum_out=ssqN[:, i:i + 1]),
                      reads=[("x", i)], writes=["junkb", ("ssq", i)])
            if stop == ("B", l):
                P.barrier(); dump_and_finish(); return True

            ar2.reset()
            sg = [ar2.bf16([128, 512]) for _ in range(2)]
            actT = [ar2.bf16([128, 4, 512]) for _ in range(2)]
            junkM = ar2.bf16([128, 1024])
            if l == 0:
                stageM = [ar2.bf16([128, 8, 512]) for _ in range(2)]
                modbrM = [ar2.f32([1, 512]) for _ in range(2)]
                mrowM = [ar2.f32([1, 512]) for _ in range(2)]
            assert ar2.off <= 11 * 1024, ar2.off
            ar2.off = 11 * 1024
            wgb = [ar2.bf16([128, 8, 512]) for _ in range(2)]
            wub = [ar2.bf16([128, 8, 512]) for _ in range(2)]
            wdb = [ar2.bf16([128, 4, 1024]) for _ in range(2)]
            g2b = ar2.f32([128, 1024])

            def load_expert(ex):
                pb = ex % 2
                for kk in range(2):
                    P.dma("pool", "wg%d" % pb, lambda e, kk=kk: e.dma_start(
                        out=wgb[pb][:, kk * 4:(kk + 1) * 4, :],
                        in_=wg_d[l, ex, kk * 512:(kk + 1) * 512, :].rearrange("(k p) n -> p k n", p=128)), writes=[("wg", pb)])
                    P.dma("pool", "wu%d" % pb, lambda e, kk=kk: e.dma_start(
                        out=wub[pb][:, kk * 4:(kk + 1) * 4, :],
                        in_=wu_d[l, ex, kk * 512:(kk + 1) * 512, :].rearrange("(k p) n -> p k n", p=128)), writes=[("wu", pb)])
                    P.dma("pool", "wd%d" % pb, lambda e, kk=kk: e.dma_start(
                        out=wdb[pb][:, kk * 2:(kk + 1) * 2, :],
                        in_=wd_d[l, ex, kk * 256:(kk + 1) * 256, :].rearrange("(k p) n -> p k n", p=128)), writes=[("wd", pb)])
                for c in range(4):
                    P.pool(lambda e, c=c: e.tensor_tensor(out=wdb[pb][:, c, :], in0=wdb[pb][:, c, :], in1=g2b, op=ALU.mult),
                           reads=[("wd", pb), "g2b"], writes=[("wd", pb)])

            def M_pre():
                P.dma("sp", "m0", lambda e: e.dma_start(out=g2b, in_=modscr[l, 5120:6144].partition_broadcast(128)), writes=["g2b"])
                load_expert(0)
                load_expert(1)

            norm_phase(l, 2, pre=M_pre, have_ssq=True)
            if stop == ("N2a", l):
                P.barrier(); dump_and_finish(); return True
            P.barrier()

            def GU(ex, tb, n):
                pb = ex % 2
                ab = n % 2
                for fc in range(4):
                    q = fc % 2
                    for k in range(8):
                        P.pe(lambda e, k=k, fc=fc, q=q: e.matmul(bank(q), lhsT=wgb[pb][:, k, fc * 128:(fc + 1) * 128],
                                                                 rhs=hT[:, k, tb * 512:(tb + 1) * 512], start=(k == 0), stop=(k == 7)),
                             reads=[("wg", pb)], writes=[("ps", q)])
                    for k in range(8):
                        P.pe(lambda e, k=k, fc=fc, q=q: e.matmul(bank(2 + q), lhsT=wub[pb][:, k, fc * 128:(fc + 1) * 128],
                                                                 rhs=hT[:, k, tb * 512:(tb + 1) * 512], start=(k == 0), stop=(k == 7)),
                             reads=[("wu", pb)], writes=[("ps", 2 + q)])
                    P.act(lambda e, q=q: e.activation(out=sg[q], in_=bank(q), func=AF.Silu), reads=[("ps", q)], writes=[("sg", q)])
                    P.dve(lambda e, q=q, fc=fc: e.tensor_tensor(out=actT[ab][:, fc, :], in0=bank(2 + q), in1=sg[q], op=ALU.mult),
                          reads=[("ps", 2 + q), ("sg", q)], writes=[("actT", ab)])

            def DN(ex, tb, n):
                pb = ex % 2
                ab = n % 2
                for tt in range(4):
                    i = tb * 4 + tt
                    for hf in range(2):
                        bk = 4 + ((tt * 2 + hf) % 4)
                        for fc in range(4):
                            P.pe(lambda e, fc=fc, hf=hf, tt=tt, bk=bk: e.matmul(bank(bk), lhsT=actT[ab][:, fc, tt * 128:(tt + 1) * 128],
                                                                                rhs=wdb[pb][:, fc, hf * 512:(hf + 1) * 512],
                                                                                start=(fc == 0), stop=(fc == 3)),
                                 reads=[("actT", ab), ("wd", pb)], writes=[("ps", bk)])
                        P.dve(lambda e, hf=hf, i=i, bk=bk: e.scalar_tensor_tensor(out=x[:, i, hf * 512:(hf + 1) * 512], in0=bank(bk),
                                                                                  scalar=gates[:, i, ex:ex + 1], in1=x[:, i, hf * 512:(hf + 1) * 512],
                                                                                  op0=ALU.mult, op1=ALU.add),
                              reads=[("ps", bk), "gates", ("x", i)], writes=[("x", i)])
                    if ex == 15:
                        P.act(lambda e, i=i: e.activation(out=junkM, in_=x[:, i, :], func=AF.Square, accum_out=ssqN[:, i:i + 1]),
                              reads=[("x", i)], writes=["junkM", ("ssq", i)])

            seqs = [(ex, tb) for ex in range(16) for tb in range(4)]
            for n in range(len(seqs) + 1):
                if n < len(seqs):
                    GU(seqs[n][0], seqs[n][1], n)
                if n == 0:
                    router_phase()
                    assert ar.off <= 11 * 1024, ar.off
                    if stop == ("N2", l):
                        P.barrier(); dump_and_finish(); return True
                if n >= 1:
                    ex, tb = seqs[n - 1]
                    DN(ex, tb, n - 1)
                    if tb == 3 and ex + 2 < 16:
                        load_expert(ex + 2)
                    if tb == 3 and l == 0:
                        if 1 <= ex <= 12:
                            j = ex - 1
                            mod_chunk(1, j, j % 2, stageM[j % 2], modbrM[j % 2], mrowM[j % 2], 7, part=2)
                        if ex < 12:
                            mod_chunk(1, ex, ex % 2, stageM[ex % 2], modbrM[ex % 2], mrowM[ex % 2], 7, part=1,
                                      extra_w=["rt"] if ex < 2 else ())
            if stop == ("M", l):
                P.barrier(); dump_and_finish(); return True

            return False

        for l_ in range(n_layers):
            if layer(l_):
                return nc

        P.barrier()
        ar.reset()
        fgb = ar.f32([128, 1024])
        ob = [ar.f32([128, 1024]) for _ in range(3)]
        junk = ar.bf16([128, 1024])
        ssq = ssqN; rstd = ar.f32([128, 16])
        P.dma("sp", "n0", lambda e: e.dma_start(out=fgb, in_=fg_d[0, :].partition_broadcast(128)), writes=["fgb"])
        P.act(lambda e: e.activation(out=rstd, in_=ssq, func=AF.Sqrt, bias=eps_t[:, 0:1], scale=1.0 / 1024),
              reads=[("ssq", i) for i in range(16)] + ["eps"], writes=["rstd0"])
        P.dve(lambda e: e.reciprocal(out=rstd, in_=rstd), reads=["rstd0"], writes=["rstd"])
        for i in range(16):
            o = ob[i % 3]
            P.dve(lambda e, i=i, o=o: e.scalar_tensor_tensor(out=o, in0=x[:, i, :], scalar=rstd[:, i:i + 1], in1=fgb,
                                                             op0=ALU.mult, op1=ALU.mult),
                  reads=[("x", i), "rstd", "fgb"], writes=[("ob", i % 3)])
            P.dma("sp", "out", lambda e, i=i, o=o: e.dma_start(out=out_d[i * 128:(i + 1) * 128, :], in_=o),
                  reads=[("ob", i % 3)], writes=[("out", i)])
        if dbg:
            P.barrier(); dump_and_finish(); return nc
        P.emit(final_wait_streams=["out"])
    return nc


def t5_bucket_np(dist):
    dist = np.maximum(dist, 0)
    ratio = np.log(np.maximum(dist, 1) / 16) / np.log(2048 / 16)
    large = 16 + np.floor(ratio * 16).astype(np.int64)
    large = np.minimum(large, 31)
    return np.where(dist < 16, dist, large).astype(np.int32)


def host_consts():
    btab = np.zeros((3, 33, 384), np.float32)
    for di, (win, d) in enumerate(CONFIGS):
        for j in range(384):
            rel = j - 127
            if 0 <= rel <= 128:
                btab[di, int(t5_bucket_np(np.array(rel * d))), j] = 1.0
            else:
                btab[di, 32, j] = NEG
    ind = np.zeros((8, 512), np.float32)
    for g in range(8):
        ind[g, g * 64:(g + 1) * 64] = 1.0
    trilm = np.triu(np.ones((128, 128), np.float32))
    return dict(identf=np.eye(128, dtype=np.float32), btab=btab, ind=ind, trilm=trilm,
                jmat=np.ascontiguousarray(np.eye(128, dtype=np.float32)[::-1]))


def make_in_maps(inputs, cores):
    f = lambda a: np.ascontiguousarray(np.asarray(a, dtype=np.float32))
    shared = dict(
        rel_bias=f(inputs["rel_bias"]), router_w=f(inputs["router_w"]), router_b=f(inputs["router_b"]).reshape(1, 16),
        mod_w=f(inputs["mod_w"]), mod_b=f(inputs["mod_b"]), norm1_g=f(inputs["norm1_g"]), w_in=f(inputs["w_in"]),
        gmlp_ln_g=f(inputs["gmlp_ln_g"]), gmlp_ln_b=f(inputs["gmlp_ln_b"]), gmlp_ws=f(inputs["gmlp_ws"]),
        gmlp_bs=f(inputs["gmlp_bs"]), w_out=f(inputs["w_out"]), norm2_g=f(inputs["norm2_g"]),
        moe_w_gate=f(inputs["moe_w_gate"]), moe_w_up=f(inputs["moe_w_up"]), moe_w_down=f(inputs["moe_w_down"]),
        final_g=f(inputs["final_g"]).reshape(1, 1024),
    )
    gab = np.concatenate([f(inputs["out_norm_a_g"]), f(inputs["out_norm_b_g"])], axis=1)
    shared["gab"] = np.ascontiguousarray(gab.reshape(2, 8, 128).transpose(0, 2, 1))
    shared.update(host_consts())
    x = f(inputs["x"]); c = f(inputs["c"])
    maps = []
    for b in cores:
        m = dict(shared)
        m["x"] = np.ascontiguousarray(x[b])
        m["cT"] = np.ascontiguousarray(c[b].reshape(8, 128).T)
        maps.append(m)
    return maps


def kernel(**inputs):
    nc = build()
    maps = make_in_maps(inputs, list(range(8)))
    res = run_bass_kernel_spmd(nc, maps, core_ids=list(range(8)))
    return np.stack([np.asarray(r["out"], dtype=np.float32) for r in res.results], axis=0)
```

```python
import contextlib
import os
import numpy as np
import concourse.bass as bass
import concourse.mybir as mybir
from concourse.bass_utils import run_bass_kernel_spmd

F32 = mybir.dt.float32
BF16 = mybir.dt.bfloat16
ALU = mybir.AluOpType
AF = mybir.ActivationFunctionType
AX = mybir.AxisListType
ENGS = ("pe", "act", "dve", "pool", "sp")
EPS = 1e-6
NEG = -30000.0
CONFIGS = ((128, 1), (512, 4), (2048, 16))


class Prog:
    def __init__(self, nc):
        self.nc = nc
        self.ins = []
        self.last_w = {}
        self.readers = {}
        self.stream_cnt = {}
        self.stream_last = {}

    def add(self, eng, fn, reads=(), writes=(), dma=None):
        i = len(self.ins)
        deps = {}

        def dep(j):
            if j is None:
                return
            pj = self.ins[j]
            deps[j] = self.stream_cnt[pj["dma"]] if pj["dma"] is not None else None

        for r in reads:
            dep(self.last_w.get(r))
        for w in writes:
            dep(self.last_w.get(w))
            for j in self.readers.get(w, ()):
                dep(j)
        pruned = {}
        for j, c in deps.items():
            pj = self.ins[j]
            if pj["dma"] is None and pj["eng"] == eng:
                if eng == "pe":
                    continue
                if not any(self.last_w.get(r) == j for r in reads):
                    continue
            pruned[j] = c
            if pj["dma"] is None:
                pj["needs_inc"] = True
        rec = dict(eng=eng, fn=fn, deps=pruned, dma=dma, needs_inc=False, val=None)
        if dma is not None:
            self.stream_cnt[dma] = self.stream_cnt.get(dma, 0) + 1
            self.stream_last[dma] = i
        self.ins.append(rec)
        for r in reads:
            self.readers.setdefault(r, []).append(i)
        for w in writes:
            self.last_w[w] = i
            self.readers[w] = []
        return i

    def pe(self, fn, reads=(), writes=()):
        return self.add("pe", fn, reads, writes)

    def act(self, fn, reads=(), writes=()):
        return self.add("act", fn, reads, writes)

    def dve(self, fn, reads=(), writes=()):
        return self.add("dve", fn, reads, writes)

    def pool(self, fn, reads=(), writes=()):
        return self.add("pool", fn, reads, writes)

    def dma(self, q, stream, fn, reads=(), writes=()):
        return self.add(q, fn, reads, writes, dma=stream)

    def barrier(self):
        last = {}
        for idx, rec in enumerate(self.ins):
            if rec["dma"] is None and not rec.get("bar"):
                last[rec["eng"]] = idx
        for e in ENGS:
            deps = {}
            for e2, j in last.items():
                if e2 == e and e == "pe":
                    continue
                deps[j] = None
                self.ins[j]["needs_inc"] = True
            for s, c in self.stream_cnt.items():
                deps[self.stream_last[s]] = c
            self.ins.append(dict(eng=e, fn=None, deps=deps, dma=None, needs_inc=False, val=None, bar=True))
        self.last_w = {}
        self.readers = {}

    def emit(self, final_wait_streams=()):
        nc = self.nc
        streams = sorted(self.stream_cnt.keys())
        with contextlib.ExitStack() as es:
            esem = {e: es.enter_context(nc.semaphore("s_" + e)) for e in ENGS}
            ssem = {s: es.enter_context(nc.semaphore("d_" + str(s))) for s in streams}
            cnt = {e: 0 for e in ENGS}
            for rec in self.ins:
                if rec["dma"] is None and rec["needs_inc"]:
                    cnt[rec["eng"]] += 1
                    rec["val"] = cnt[rec["eng"]]
            per_eng = {e: [] for e in ENGS}
            for rec in self.ins:
                per_eng[rec["eng"]].append(rec)
            ins = self.ins
            block = es.enter_context(nc.Block())

            def run(ename, eng):
                waited = {}
                for rec in per_eng[ename]:
                    for j, c in rec["deps"].items():
                        pj = ins[j]
                        if pj["dma"] is not None:
                            sem, v, key = ssem[pj["dma"]], c * 16, ("d", pj["dma"])
                        else:
                            sem, v, key = esem[pj["eng"]], pj["val"], ("e", pj["eng"])
                        if waited.get(key, 0) >= v:
                            continue
                        waited[key] = v
                        eng.wait_ge(sem, v)
                    if rec["fn"] is None:
                        continue
                    bi = rec["fn"](eng)
                    if rec["dma"] is not None:
                        bi.then_inc(ssem[rec["dma"]], 16)
                    elif rec["needs_inc"]:
                        bi.then_inc(esem[ename], 1)
                if ename == "sp":
                    for s in final_wait_streams:
                        eng.wait_ge(ssem[s], self.stream_cnt[s] * 16)

            block.tensor(lambda e: run("pe", e))
            block.scalar(lambda e: run("act", e))
            block.vector(lambda e: run("dve", e))
            block.gpsimd(lambda e: run("pool", e))
            block.sync(lambda e: run("sp", e))


class Arena:
    def __init__(self, t, words):
        self.t = t
        self.words = words
        self.off = 0

    def reset(self):
        self.off = 0

    def _take(self, nwords):
        nwords = (nwords + 7) // 8 * 8
        a = self.off
        self.off += nwords
        assert self.off <= self.words, ("arena overflow", self.off, self.words)
        return a

    def f32(self, shape):
        n = int(np.prod(shape[1:]))
        a = self._take(n)
        ap = self.t[0:shape[0], a:a + n]
        return self._shape(ap, shape)

    def bf16(self, shape):
        n = int(np.prod(shape[1:]))
        a = self._take((n + 1) // 2)
        ap = self.t[0:shape[0], a:a + (n + 1) // 2].bitcast(BF16)[:, 0:n]
        return self._shape(ap, shape)

    @staticmethod
    def _shape(ap, shape):
        if len(shape) == 2:
            return ap
        if len(shape) == 3:
            return ap.rearrange("p (a b) -> p a b", a=shape[1])
        if len(shape) == 4:
            return ap.rearrange("p (a b c) -> p a b c", a=shape[1], b=shape[2])
        raise ValueError(shape)


NSB = 4
ARENA_WORDS = 25 * 1024


def build(stop=None, n_layers=2):
    nc = bass.Bass("TRN2", target_bir_lowering=False)
    din = lambda name, shape: nc.dram_tensor(name, shape, F32, kind="ExternalInput").ap()
    x_d = din("x", [2048, 1024])
    cT_d = din("cT", [128, 8])
    relb_d = din("rel_bias", [32, 8])
    rw_d = din("router_w", [1024, 16])
    rb_d = din("router_b", [1, 16])
    modw_d = din("mod_w", [2, 1024, 6144])
    modb_d = din("mod_b", [2, 6144])
    n1g_d = din("norm1_g", [2, 1024])
    win_d = din("w_in", [2, 1024, 2560])
    lng_d = din("gmlp_ln_g", [2, 512])
    lnb_d = din("gmlp_ln_b", [2, 512])
    ws_d = din("gmlp_ws", [2, 8, 128, 128])
    bs_d = din("gmlp_bs", [2, 8, 128])
    gab_d = din("gab", [2, 128, 8])
    wout_d = din("w_out", [2, 1024, 1024])
    n2g_d = din("norm2_g", [2, 1024])
    wg_d = din("moe_w_gate", [2, 16, 1024, 512])
    wu_d = din("moe_w_up", [2, 16, 1024, 512])
    wd_d = din("moe_w_down", [2, 16, 512, 1024])
    fg_d = din("final_g", [1, 1024])
    identf_d = din("identf", [128, 128])
    btab_d = din("btab", [3, 33, 384])
    ind_d = din("ind", [8, 512])
    tril_d = din("trilm", [128, 128])
    jmat_d = din("jmat", [128, 128])
    out_d = nc.dram_tensor("out", [2048, 1024], F32, kind="ExternalOutput").ap()
    modscr = nc.dram_tensor("modscr", [2, 6144], F32, kind="Internal").ap()
    gscr_h = nc.dram_tensor("gscr", [3, 8, 384], F32, kind="Internal")
    gscr = gscr_h.ap()
    texp = nc.dram_tensor("texp", [3, 8, 128, 256], F32, kind="Internal").ap()
    obT_d = nc.dram_tensor("obT_d", [128, 4, 2048], BF16, kind="Internal").ap()
    dbg = stop is not None
    if dbg:
        dbg_x = nc.dram_tensor("dbg_x", [2048, 1024], F32, kind="ExternalOutput").ap()
        dbg_hT = nc.dram_tensor("dbg_hT", [128, 8, 2048], BF16, kind="ExternalOutput").ap()
        dbg_g = nc.dram_tensor("dbg_g", [128, 256], F32, kind="ExternalOutput").ap()
        dbg_obT = nc.dram_tensor("dbg_obT", [128, 4, 2048], BF16, kind="ExternalOutput").ap()

    P = Prog(nc)
    es = contextlib.ExitStack()
    with es:
        sb = lambda name, shape, dt: es.enter_context(nc.sbuf_tensor(name, shape, dt))
        x = sb("x_sb", [128, 16, 1024], F32)
        hT = sb("hT", [128, 8, 2048], BF16)
        identf = sb("identf_sb", [128, 128], F32)
        identb = sb("identb_sb", [128, 128], BF16)
        trilm = sb("trilm_sb", [128, 128], F32)
        ind = sb("ind_sb", [8, 512], F32)
        ones_bf = sb("ones_bf", [1, 128], BF16)
        eps_t = sb("eps_t", [128, 1], F32)
        nhalf = sb("nhalf_t", [128, 1], F32)
        gates = sb("gates_sb", [128, 16, 16], F32)
        rbb = sb("rbb_sb", [128, 16, 16], F32)
        rw = sb("rw_sb", [128, 8, 16], F32)
        statA = sb("statA", [128, 64], F32)
        cact = sb("cact_sb", [128, 8], BF16)
        arena_t = sb("arena", [128, ARENA_WORDS], F32)
        PS = es.enter_context(nc.psum_tensor("ps", [128, 4096], F32))
        ar = Arena(arena_t, ARENA_WORDS)
        ar2 = Arena(arena_t, ARENA_WORDS)

        def bank(b, n=512, parts=128):
            return PS[0:parts, b * 512:b * 512 + n]

        P.dma("sp", "c0", lambda e: e.dma_start(out=identf[:], in_=identf_d), writes=["identf"])
        P.dma("sp", "c0", lambda e: e.dma_start(out=trilm[:], in_=tril_d), writes=["trilm"])
        P.dma("sp", "c0", lambda e: e.dma_start(out=ind[:], in_=ind_d), writes=["ind"])
        P.dma("sp", "c0", lambda e: e.dma_start(out=rw[:], in_=rw_d.rearrange("(k p) e -> p k e", p=128)), writes=["rw"])
        P.dma("sp", "c0", lambda e: e.dma_start(
            out=rbb[:], in_=bass.AP(rb_d.tensor, 0, [[0, 128], [0, 16], [1, 16]])), writes=["rbb"])
        P.dve(lambda e: e.tensor_copy(out=identb[:], in_=identf[:]), reads=["identf"], writes=["identb"])
        P.dve(lambda e: e.memset(ones_bf[:], 1.0), writes=["ones_bf"])
        P.dve(lambda e: e.memset(eps_t[:], EPS), writes=["eps"])
        P.dve(lambda e: e.memset(nhalf[:], -0.5), writes=["nhalf"])

        ar.reset()
        cT = ar.f32([128, 8])
        stageR = [ar.bf16([128, 3072]) for _ in range(3)]
        modbrR = ar.f32([1, 3072])
        mrowR = ar.f32([1, 3072])
        rb33 = ar.f32([33, 8])
        btab = ar.f32([33, 3, 384])
        grow = ar.f32([8, 3, 384])
        P.dma("sp", "c0", lambda e: e.dma_start(out=cT, in_=cT_d), writes=["cT"])
        P.act(lambda e: e.activation(out=cact[:], in_=cT, func=AF.Silu), reads=["cT"], writes=["cact"])
        def mod_chunk(l, j, pb, stage_, modbr_, mrow_, bk, part=3, extra_w=()):
            if part & 1:
                P.dma("pool", "mw%d" % pb, lambda e: e.dma_start(
                    out=stage_, in_=modw_d[l, :, j * 512:(j + 1) * 512].rearrange("(k p) n -> p k n", p=128)),
                    writes=[("stage", pb)] + list(extra_w))
                P.dma("sp", "mb%d" % pb, lambda e: e.dma_start(out=modbr_, in_=modb_d[l:l + 1, j * 512:(j + 1) * 512]),
                      writes=[("modbr", pb)] + list(extra_w))
            if not (part & 2):
                return
            for k in range(8):
                P.pe(lambda e, k=k: e.matmul(bank(bk, 512, 1), lhsT=cact[:, k:k + 1], rhs=stage_[:, k, :],
                                             start=(k == 0), stop=(k == 7)),
                     reads=["cact", ("stage", pb)], writes=[("ps", bk)])
            P.dve(lambda e: e.tensor_tensor(out=mrow_, in0=bank(bk, 512, 1), in1=modbr_, op=ALU.add),
                  reads=[("ps", bk), ("modbr", pb)], writes=[("mrow", pb)])
            P.dma("sp", "ms%d" % pb, lambda e: e.dma_start(out=modscr[l:l + 1, j * 512:(j + 1) * 512], in_=mrow_),
                  reads=[("mrow", pb)], writes=[("modscr", l)])

        for i in range(16):
            P.dma("act", "x", lambda e, i=i: e.dma_start(out=x[:, i, :], in_=x_d[i * 128:(i + 1) * 128, :]),
                  writes=[("x", i)])
        P.dve(lambda e: e.memset(rb33, 1.0), writes=["rb33"])
        P.dma("sp", "c1", lambda e: e.dma_start(out=rb33[0:32, :], in_=relb_d), writes=["rb33"])
        P.dma("sp", "c1", lambda e: e.dma_start(out=btab, in_=btab_d.rearrange("d r j -> r d j")), writes=["btab"])
        jmat = ar.f32([128, 128])
        thk = [ar.f32([128, 8, 256]) for _ in range(3)]
        tfx = [ar.f32([128, 8, 256]) for _ in range(2)]
        P.dma("sp", "c1", lambda e: e.dma_start(out=jmat, in_=jmat_d), writes=["jmat"])
        for d in range(3):
            P.pe(lambda e, d=d: e.matmul(bank(6 + d % 2, 384, 8), lhsT=rb33[:, :], rhs=btab[:, d, :], start=True, stop=True),
                 reads=["rb33", "btab"], writes=[("ps", 6 + d % 2)])
            P.dve(lambda e, d=d: e.tensor_copy(out=grow[:, d, :], in_=bank(6 + d % 2, 384, 8)),
                  reads=[("ps", 6 + d % 2)], writes=["grow"])
        P.dma("act", "c2", lambda e: e.dma_start(out=gscr.rearrange("d h j -> h d j"), in_=grow),
              reads=["grow"], writes=["gscr"])
        for d in range(3):
            P.dma("act", "tk%d" % d, lambda e, d=d: e.dma_start(
                out=thk[d], in_=bass.AP(gscr_h, d * 8 * 384, [[1, 128], [384, 8], [1, 256]])),
                reads=["gscr"], writes=[("thk", d)])

        def texp_flip():
            nj = 0
            for d in range(3):
                pb = d % 2
                for j in range(4):
                    bk = 6 + nj % 2
                    nj += 1
                    P.pe(lambda e, d=d, j=j, bk=bk: e.matmul(bank(bk), lhsT=jmat, rhs=thk[d][:, 2 * j:2 * j + 2, :].rearrange("p a b -> p (a b)"),
                                                             start=True, stop=True),
                         reads=["jmat", ("thk", d)], writes=[("ps", bk)])
                    P.dve(lambda e, pb=pb, j=j, bk=bk: e.tensor_copy(out=tfx[pb][:, 2 * j:2 * j + 2, :].rearrange("p a b -> p (a b)"), in_=bank(bk)),
                          reads=[("ps", bk)], writes=[("tfx", pb)])
                P.dma("act", "tx%d" % pb, lambda e, d=d, pb=pb: e.dma_start(out=texp[d].rearrange("h p q -> p h q"), in_=tfx[pb]),
                      reads=[("tfx", pb)], writes=["texp"])

        nblk = 0
        for hh in range(2):
            P.dma("sp", "mb0", lambda e, hh=hh: e.dma_start(out=modbrR, in_=modb_d[0:1, hh * 3072:(hh + 1) * 3072]),
                  writes=["modbrR"])
            for k in range(8):
                sb_ = nblk % 3
                nblk += 1
                P.dma("pool", "mr%d" % sb_, lambda e, hh=hh, k=k, sb_=sb_: e.dma_start(
                    out=stageR[sb_], in_=modw_d[0, k * 128:(k + 1) * 128, hh * 3072:(hh + 1) * 3072]),
                    writes=[("stageR", sb_)])
                for j in range(6):
                    P.pe(lambda e, k=k, j=j, sb_=sb_: e.matmul(bank(j, 512, 1), lhsT=cact[:, k:k + 1], rhs=stageR[sb_][:, j * 512:(j + 1) * 512],
                                                               start=(k == 0), stop=(k == 7)),
                         reads=["cact", ("stageR", sb_)], writes=[("ps", j)])
            for j in range(6):
                P.dve(lambda e, j=j: e.tensor_tensor(out=mrowR[:, j * 512:(j + 1) * 512], in0=bank(j, 512, 1),
                                                     in1=modbrR[:, j * 512:(j + 1) * 512], op=ALU.add),
                      reads=[("ps", j), "modbrR"], writes=["mrowR"])
            P.dma("sp", "ms0", lambda e, hh=hh: e.dma_start(out=modscr[0:1, hh * 3072:(hh + 1) * 3072], in_=mrowR),
                  reads=["mrowR"], writes=[("modscr", 0)])
            if hh == 0:
                texp_flip()

        ssqN = statA[:, 16:32]

        def norm_phase(l, which, pre=None, post=None, have_ssq=False):
            P.barrier()
            ar.reset()
            if pre is not None:
                pre()
            gmod = ar.f32([128, 1024])
            ngb = ar.f32([128, 1024])
            shb = ar.f32([128, 1024])
            h32 = [ar.f32([128, 1024]) for _ in range(2)]
            junk = ar.bf16([128, 1024])
            ssq = ssqN
            rstd = ar.f32([128, 16])
            xh32 = [ar.f32([128, 8, 128]) for _ in range(2)] if which == 2 else None
            off_sh, off_sc = (0, 1024) if which == 1 else (3072, 4096)
            ng_d = n1g_d if which == 1 else n2g_d
            P.dma("sp", "n0", lambda e: e.dma_start(out=gmod, in_=modscr[l, off_sc:off_sc + 1024].partition_broadcast(128)),
                  writes=["gmod"])
            P.dma("sp", "n0", lambda e: e.dma_start(out=ngb, in_=ng_d[l, :].partition_broadcast(128)), writes=["ngb"])
            P.dma("sp", "n0", lambda e: e.dma_start(out=shb, in_=modscr[l, off_sh:off_sh + 1024].partition_broadcast(128)),
                  writes=["shb"])
            P.dve(lambda e: e.scalar_tensor_tensor(out=gmod, in0=gmod, scalar=1.0, in1=ngb, op0=ALU.add, op1=ALU.mult),
                  reads=["gmod", "ngb"], writes=["gmod"])
            if not have_ssq:
                for i in range(16):
                    P.act(lambda e, i=i: e.activation(out=junk, in_=x[:, i, :], func=AF.Square, accum_out=ssq[:, i:i + 1]),
                          reads=[("x", i)], writes=["junk", ("ssq", i)])
            P.act(lambda e: e.activation(out=rstd, in_=ssq, func=AF.Sqrt, bias=eps_t[:, 0:1], scale=1.0 / 1024),
                  reads=[("ssq", i) for i in range(16)] + ["eps"], writes=["rstd0"])
            P.dve(lambda e: e.reciprocal(out=rstd, in_=rstd), reads=["rstd0"], writes=["rstd"])
            for i in range(16):
                hb = h32[i % 2]
                pp = i % 2
                P.dve(lambda e, i=i, hb=hb: e.scalar_tensor_tensor(out=hb, in0=x[:, i, :], scalar=rstd[:, i:i + 1], in1=gmod,
                                                                    op0=ALU.mult, op1=ALU.mult),
                      reads=[("x", i), "rstd", "gmod"], writes=[("h32", pp)])
                P.dve(lambda e, hb=hb: e.tensor_tensor(out=hb, in0=hb, in1=shb, op=ALU.add),
                      reads=[("h32", pp), "shb"], writes=[("h32", pp)])
                for c in range(8):
                    P.pe(lambda e, c=c, hb=hb, pp=pp: e.transpose(out=PS[:, pp * 1024 + c * 128:pp * 1024 + (c + 1) * 128],
                                                                  in_=hb[:, c * 128:(c + 1) * 128], identity=identf[:]),
                         reads=[("h32", pp), "identf"], writes=[("ps", 2 * pp + c // 4)])
                if which == 1:
                    P.act(lambda e, i=i, pp=pp: e.activation(out=hT[:, :, i * 128:(i + 1) * 128],
                                                             in_=PS[:, pp * 1024:(pp + 1) * 1024].rearrange("p (c t) -> p c t", c=8),
                                                             func=AF.Copy),
                          reads=[("ps", 2 * pp), ("ps", 2 * pp + 1)], writes=[("hT", i)])
                if which == 2:
                    xb = xh32[pp]
                    P.dve(lambda e, pp=pp, xb=xb: e.tensor_copy(out=xb, in_=PS[:, pp * 1024:(pp + 1) * 1024].rearrange("p (c t) -> p c t", c=8)),
                          reads=[("ps", 2 * pp), ("ps", 2 * pp + 1)], writes=[("xh32", pp)])
                    P.act(lambda e, i=i, xb=xb: e.activation(out=hT[:, :, i * 128:(i + 1) * 128], in_=xb, func=AF.Copy),
                          reads=[("xh32", pp)], writes=[("hT", i)])
                    for c in range(8):
                        P.pe(lambda e, i=i, c=c, xb=xb: e.matmul(PS[:, 4 * 512 + i * 16:4 * 512 + (i + 1) * 16], lhsT=xb[:, c, :],
                                                                 rhs=rw[:, c, :], start=(c == 0), stop=(c == 7)),
                             reads=[("xh32", pp), "rw"], writes=[("ps", 4)])

        def norm_phase_post(post):
            if post is not None:
                post()

        def router_phase():
            T = lambda: ar.f32([128, 16, 16])
            L, E_, pr, sel, t1, t2, t3, selm = T(), T(), T(), T(), T(), T(), T(), T()
            mx = ar.f32([128, 16]); sm = ar.f32([128, 16])
            g4 = ar.f32([128, 16, 4]); p6 = [ar.f32([128, 16, 4]) for _ in range(6)]
            gmx = ar.f32([128, 16]); gm = ar.f32([128, 16, 4])
            m1 = ar.f32([128, 16]); m2 = ar.f32([128, 16])
            bc = lambda a, n: a.unsqueeze(2).to_broadcast([128, 16, n])
            v = lambda e: e
            P.dve(lambda e: e.tensor_copy(out=L, in_=PS[:, 2048:2048 + 256].rearrange("p (a b) -> p a b", a=16)),
                  reads=[("ps", 4)], writes=["rt"])
            seq = []
            seq.append(lambda e: e.tensor_reduce(out=mx, in_=L, axis=AX.X, op=ALU.max))
            seq.append(lambda e: e.tensor_tensor(out=t1, in0=L, in1=bc(mx, 16), op=ALU.subtract))
            for f in seq:
                P.dve(f, reads=["rt"], writes=["rt"])
            P.act(lambda e: e.activation(out=E_, in_=t1, func=AF.Exp), reads=["rt"], writes=["rt"])
            seq = []
            seq.append(lambda e: e.tensor_reduce(out=sm, in_=E_, axis=AX.X, op=ALU.add))
            seq.append(lambda e: e.reciprocal(out=sm, in_=sm))
            seq.append(lambda e: e.tensor_tensor(out=pr, in0=E_, in1=bc(sm, 16), op=ALU.mult))
            seq.append(lambda e: e.tensor_tensor(out=sel, in0=pr, in1=rbb[:], op=ALU.add))
            s4 = sel.rearrange("p a (g k) -> p a g k", k=4)
            pairs = [(0, 1), (0, 2), (0, 3), (1, 2), (1, 3), (2, 3)]
            for q, (a, b) in enumerate(pairs):
                seq.append(lambda e, q=q, a=a, b=b: e.tensor_tensor(out=p6[q], in0=s4[:, :, :, a], in1=s4[:, :, :, b], op=ALU.add))
            seq.append(lambda e: e.tensor_tensor(out=g4, in0=p6[0], in1=p6[1], op=ALU.max))
            for q in range(2, 6):
                seq.append(lambda e, q=q: e.tensor_tensor(out=g4, in0=g4, in1=p6[q], op=ALU.max))
            seq.append(lambda e: e.tensor_reduce(out=gmx, in_=g4, axis=AX.X, op=ALU.max))
            seq.append(lambda e: e.tensor_tensor(out=gm, in0=g4, in1=bc(gmx, 4), op=ALU.is_ge))
            gm16 = gm.unsqueeze(3).to_broadcast([128, 16, 4, 4])
            sm4 = selm.rearrange("p a (g k) -> p a g k", k=4)
            seq.append(lambda e: e.scalar_tensor_tensor(out=sm4, in0=s4, scalar=100.0, in1=gm16, op0=ALU.add, op1=ALU.mult))
            seq.append(lambda e: e.tensor_scalar(out=selm, in0=selm, scalar1=-100.0, scalar2=None, op0=ALU.add))
            seq.append(lambda e: e.tensor_reduce(out=m1, in_=selm, axis=AX.X, op=ALU.max))
            seq.append(lambda e: e.tensor_tensor(out=t2, in0=selm, in1=bc(m1, 16), op=ALU.is_ge))
            seq.append(lambda e: e.scalar_tensor_tensor(out=t3, in0=t2, scalar=-1000.0, in1=selm, op0=ALU.mult, op1=ALU.add))
            seq.append(lambda e: e.tensor_reduce(out=m2, in_=t3, axis=AX.X, op=ALU.max))
            seq.append(lambda e: e.tensor_tensor(out=t2, in0=selm, in1=bc(m2, 16), op=ALU.is_ge))
            seq.append(lambda e: e.tensor_tensor(out=t3, in0=t2, in1=pr, op=ALU.mult))
            seq.append(lambda e: e.tensor_reduce(out=sm, in_=t3, axis=AX.X, op=ALU.add))
            seq.append(lambda e: e.reciprocal(out=sm, in_=sm))
            seq.append(lambda e: e.tensor_tensor(out=gates[:], in0=t3, in1=bc(sm, 16), op=ALU.mult))
            for f in seq:
                P.dve(f, reads=["rt", "rbb"], writes=["rt", "gates"])

        def dump_and_finish():
            for i in range(16):
                P.dma("sp", "dbg", lambda e, i=i: e.dma_start(out=dbg_x[i * 128:(i + 1) * 128, :], in_=x[:, i, :]),
                      reads=[("x", i)])
            P.dma("sp", "dbg", lambda e: e.dma_start(out=dbg_hT, in_=hT[:]), reads=[("hT", i) for i in range(16)])
            P.dma("sp", "dbg", lambda e: e.dma_start(out=dbg_g, in_=gates[:].rearrange("p a b -> p (a b)")), reads=["gates"])
            P.emit(final_wait_streams=["dbg"])

        def layer(l):
            ar2.reset()
            vball = ar2.bf16([128, 16, 512])
            gub = ar2.bf16([128, 16, 512])
            w_uva = ar2.bf16([128, 8, 1024])
            w_oa = ar2.bf16([128, 4, 1024])
            WT = ar2.bf16([128, 8, 128])
            oaT = [ar2.bf16([128, 4, 128]) for _ in range(2)]
            g1b = ar2.f32([128, 1024])
            Wld = ar2.f32([128, 8, 128])
            lngb = ar2.f32([128, 512]); lnbb = ar2.f32([128, 512])
            gv = [ar2.f32([128, 512]) for _ in range(2)]
            vn = [ar2.f32([128, 512]) for _ in range(2)]
            oa = [ar2.f32([128, 512]) for _ in range(2)]
            junkA = ar2.bf16([128, 512])
            bsT = ar2.f32([8, 128])
            gabf = ar2.f32([128, 8])
            st6 = ar2.f32([128, 2, 6]); mv = ar2.f32([128, 2, 2]); vpe = ar2.f32([128, 2]); rsv = ar2.f32([128, 2])
            ssqa = ar2.f32([128, 16]); rsa = ar2.f32([128, 16])
            assert vball is not None

            def A_pre():
                for h2 in range(2):
                    P.dma("pool", "wA", lambda e, h2=h2: e.dma_start(
                        out=w_uva[:, :, h2 * 512:(h2 + 1) * 512],
                        in_=win_d[l, :, h2 * 512:(h2 + 1) * 512].rearrange("(k p) n -> p k n", p=128)), writes=[("w_uva", h2)])
                P.dma("pool", "wA", lambda e: e.dma_start(out=w_oa, in_=wout_d[l, 0:512, :].rearrange("(k p) n -> p k n", p=128)),
                      writes=["w_oa"])
                P.dma("sp", "a0", lambda e: e.dma_start(out=g1b, in_=modscr[l, 2048:3072].partition_broadcast(128)), writes=["g1b"])
                P.dma("sp", "a0", lambda e: e.dma_start(out=gabf, in_=gab_d[l]), writes=["gabf"])
                P.dma("sp", "a0", lambda e: e.dma_start(out=Wld, in_=ws_d[l].rearrange("g t s -> t g s")), writes=["Wld"])
                P.dma("sp", "a0", lambda e: e.dma_start(out=bsT, in_=bs_d[l]), writes=["bsT"])
                P.dma("sp", "a0", lambda e: e.dma_start(out=lngb, in_=lng_d[l, :].partition_broadcast(128)), writes=["lngb"])
                P.dma("sp", "a0", lambda e: e.dma_start(out=lnbb, in_=lnb_d[l, :].partition_broadcast(128)), writes=["lnbb"])

            def A_post():
                for c in range(4):
                    P.dve(lambda e, c=c: e.scalar_tensor_tensor(out=w_oa[:, c, :], in0=w_oa[:, c, :], scalar=gabf[:, c:c + 1], in1=g1b,
                                                                 op0=ALU.mult, op1=ALU.mult),
                          reads=["w_oa", "gabf", "g1b"], writes=["w_oa"])
                for g in range(8):
                    P.pe(lambda e, g=g: e.transpose(out=PS[:, 3072 + g * 128:3072 + (g + 1) * 128], in_=Wld[:, g, :], identity=identf[:]),
                         reads=["Wld", "identf"], writes=[("ps", 6 + g // 4)])
                for g in range(8):
                    P.dve(lambda e, g=g: e.tensor_tensor(out=WT[:, g, :], in0=PS[:, 3072 + g * 128:3072 + (g + 1) * 128], in1=trilm[:], op=ALU.mult),
                          reads=[("ps", 6 + g // 4), "trilm"], writes=["WT"])


            norm_phase(l, 1, pre=A_pre, have_ssq=(l > 0))
            assert ar.off <= 8 * 1024, ar.off
            A_post()
            if stop == ("N1", l):
                P.barrier(); dump_and_finish(); return True

            P.barrier()
            def A1(i):
                pp = i % 2
                for k in range(8):
                    P.pe(lambda e, k=k: e.matmul(bank(pp), lhsT=hT[:, k, i * 128:(i + 1) * 128], rhs=w_uva[:, k, 0:512],
                                                 start=(k == 0), stop=(k == 7)),
                         reads=[("hT", i), ("w_uva", 0)], writes=[("ps", pp)])
                for k in range(8):
                    P.pe(lambda e, k=k: e.matmul(bank(2 + pp), lhsT=hT[:, k, i * 128:(i + 1) * 128], rhs=w_uva[:, k, 512:1024],
                                                 start=(k == 0), stop=(k == 7)),
                         reads=[("hT", i), ("w_uva", 1)], writes=[("ps", 2 + pp)])
                P.act(lambda e: e.activation(out=gub[:, i, :], in_=bank(pp), func=AF.Gelu), reads=[("ps", pp)], writes=[("gub", i)])
                P.act(lambda e: e.activation(out=gv[pp], in_=bank(2 + pp), func=AF.Gelu), reads=[("ps", 2 + pp)], writes=[("gv", pp)])
                P.dve(lambda e: e.bn_stats(out=st6[:, pp, :], in_=gv[pp]), reads=[("gv", pp)], writes=[("st6", pp)])
                P.dve(lambda e: e.bn_aggr(out=mv[:, pp, :], in_=st6[:, pp, :]), reads=[("st6", pp)], writes=[("mv", pp)])
                P.dve(lambda e: e.tensor_scalar(out=vpe[:, pp:pp + 1], in0=mv[:, pp, 1:2], scalar1=EPS, scalar2=None, op0=ALU.add),
                      reads=[("mv", pp)], writes=[("vpe", pp)])
                P.pool(lambda e: e.tensor_tensor(out=rsv[:, pp:pp + 1], in0=vpe[:, pp:pp + 1], in1=nhalf[:, 0:1], op=ALU.pow),
                       reads=[("vpe", pp), "nhalf"], writes=[("rsv", pp)])
                P.dve(lambda e: e.tensor_scalar(out=vn[pp], in0=gv[pp], scalar1=mv[:, pp, 0:1], scalar2=rsv[:, pp:pp + 1],
                                                op0=ALU.subtract, op1=ALU.mult),
                      reads=[("gv", pp), ("mv", pp), ("rsv", pp)], writes=[("vn", pp)])
                P.dve(lambda e: e.tensor_tensor(out=vn[pp], in0=vn[pp], in1=lngb, op=ALU.mult),
                      reads=[("vn", pp), "lngb"], writes=[("vn", pp)])
                P.dve(lambda e: e.tensor_tensor(out=vball[:, i, :], in0=vn[pp], in1=lnbb, op=ALU.add),
                      reads=[("vn", pp), "lnbb"], writes=[("vball", i)])

            def A2(i):
                pp = i % 2
                P.pe(lambda e: e.matmul(bank(pp), lhsT=bsT[:, :], rhs=ind[:, :], start=True, stop=False),
                     reads=["bsT", "ind"], writes=[("ps", pp)])
                for g in range(8):
                    P.pe(lambda e, g=g: e.matmul(PS[:, pp * 512 + g * 64:pp * 512 + (g + 1) * 64], lhsT=WT[:, g, :],
                                                 rhs=vball[:, i, g * 64:(g + 1) * 64], start=False, stop=(g == 7)),
                         reads=["WT", ("vball", i)], writes=[("ps", pp)])
                P.dve(lambda e: e.tensor_tensor(out=oa[pp], in0=bank(pp), in1=gub[:, i, :], op=ALU.mult),
                      reads=[("ps", pp), ("gub", i)], writes=[("oa", pp)])
                P.act(lambda e: e.activation(out=junkA, in_=oa[pp], func=AF.Square, accum_out=ssqa[:, i:i + 1]),
                      reads=[("oa", pp)], writes=["junkA", ("ssqa", i)])
                P.dve(lambda e: e.tensor_scalar(out=rsa[:, i:i + 1], in0=ssqa[:, i:i + 1], scalar1=1.0 / 512, scalar2=EPS,
                                                op0=ALU.mult, op1=ALU.add),
                      reads=[("ssqa", i)], writes=[("rsa0", i)])
                P.pool(lambda e: e.tensor_tensor(out=rsa[:, i:i + 1], in0=rsa[:, i:i + 1], in1=nhalf[:, 0:1], op=ALU.pow),
                       reads=[("rsa0", i), "nhalf"], writes=[("rsa", i)])

            def A2b(i):
                pp = i % 2
                for c in range(4):
                    P.pe(lambda e, c=c: e.transpose(out=PS[:, (2 + pp) * 512 + c * 128:(2 + pp) * 512 + (c + 1) * 128],
                                                    in_=oa[pp][:, c * 128:(c + 1) * 128], identity=identf[:]),
                         reads=[("oa", pp), "identf"], writes=[("ps", 2 + pp)])
                P.act(lambda e: e.activation(out=oaT[pp], in_=bank(2 + pp).rearrange("p (c t) -> p c t", c=4), func=AF.Copy),
                      reads=[("ps", 2 + pp)], writes=[("oaT", pp)])

            def A3(i):
                pp = i % 2
                for hf in range(2):
                    bk = 4 + 2 * pp + hf
                    for c in range(4):
                        P.pe(lambda e, c=c, hf=hf, bk=bk: e.matmul(bank(bk), lhsT=oaT[pp][:, c, :], rhs=w_oa[:, c, hf * 512:(hf + 1) * 512],
                                                                   start=(c == 0), stop=(c == 3)),
                             reads=[("oaT", pp), "w_oa"], writes=[("ps", bk)])
                    P.dve(lambda e, hf=hf, bk=bk: e.scalar_tensor_tensor(out=x[:, i, hf * 512:(hf + 1) * 512], in0=bank(bk),
                                                                         scalar=rsa[:, i:i + 1], in1=x[:, i, hf * 512:(hf + 1) * 512],
                                                                         op0=ALU.mult, op1=ALU.add),
                          reads=[("ps", bk), ("rsa", i), ("x", i)], writes=[("x", i)])

            for i in range(16):
                A1(i)
            for s in range(18):
                if s < 16:
                    A2(s)
                if 0 <= s - 1 < 16:
                    A2b(s - 1)
                if 0 <= s - 2 < 16:
                    A3(s - 2)
            if stop == ("A", l):
                P.barrier(); dump_and_finish(); return True

            P.barrier()
            ar.reset()
            ost = [ar.bf16([128, 1024]) for _ in range(2)]
            wq = ar.bf16([128, 8, 128]); wk = ar.bf16([128, 8, 128]); wv = ar.bf16([128, 8, 128])
            QTd = [[ar.bf16([128, 2048]) for _ in range(3)] for _ in range(2)]
            KTd = [ar.bf16([128, 2048]) for _ in range(3)]
            VT = ar.bf16([128, 2048])
            Vb = [ar.bf16([128, 16, 2, 65]) for _ in range(3)]
            Pt = [ar.bf16([128, 512]) for _ in range(NSB)]
            obtok = ar.bf16([128, 16, 128])
            Tt = [[ar.f32([128, 512]) for _ in range(3)] for _ in range(2)]
            E32 = [ar.f32([128, 512]) for _ in range(NSB)]
            accS = [ar.f32([65, 512]) for _ in range(2)]
            rden = ar.f32([128, 4])
            ssqb = ar.f32([128, 16, 4]); rsb = statA[:, 0:16]
            junkB = ar.bf16([128, 128])
            b_end = ar.off
            for d in range(3):
                P.dve(lambda e, d=d: e.memset(Vb[d][:, :, :, 64:65], 1.0), writes=[("Vb", d)])
            for di in range(3):
                P.dve(lambda e, di=di: e.memset(QTd[0][di][64:128, :], 0.0), writes=["qz0"])
                P.dve(lambda e, di=di: e.memset(QTd[1][di][0:64, :], 0.0), writes=["qz1"])
            def B_proj(hp):
                for nm, wt, col in (("wq", wq, 1024), ("wk", wk, 1536), ("wv", wv, 2048)):
                    P.dma("pool", "wB", lambda e, wt=wt, col=col: e.dma_start(
                        out=wt, in_=win_d[l, :, col + hp * 128:col + (hp + 1) * 128].rearrange("(k p) n -> p k n", p=128)),
                        writes=[nm])
                nbk = 0
                for tb in range(4):
                    for nm, wt in (("wq", wq), ("wk", wk), ("wv", wv)):
                        bk = 6 + (nbk % 2)
                        nbk += 1
                        for k in range(8):
                            P.pe(lambda e, k=k, wt=wt, bk=bk, tb=tb: e.matmul(bank(bk), lhsT=wt[:, k, :], rhs=hT[:, k, tb * 512:(tb + 1) * 512],
                                                                              start=(k == 0), stop=(k == 7)),
                                 reads=[nm], writes=[("ps", bk)])
                        if nm == "wk":
                            P.act(lambda e, bk=bk, tb=tb: e.activation(out=KTd[0][:, tb * 512:(tb + 1) * 512], in_=bank(bk), func=AF.Copy),
                                  reads=[("ps", bk)], writes=[("wkT", tb)])
                        elif nm == "wv":
                            P.dve(lambda e, bk=bk, tb=tb: e.tensor_copy(out=VT[:, tb * 512:(tb + 1) * 512], in_=bank(bk)),
                                  reads=[("ps", bk)], writes=[("VT", tb)])
                        else:
                            for hh in range(2):
                                P.act(lambda e, bk=bk, tb=tb, hh=hh: e.activation(
                                    out=QTd[hh][0][64 * hh:64 * hh + 64, tb * 512:(tb + 1) * 512],
                                    in_=PS[64 * hh:64 * hh + 64, bk * 512:(bk + 1) * 512], func=AF.Copy, scale=0.125),
                                    reads=[("ps", bk)], writes=[("wqT", tb)])
                def deint(t, d):
                    return t.rearrange("p (r i) -> p r i", r=d), None
                for di in (1, 2):
                    d = CONFIGS[di][1]
                    P.pool(lambda e, di=di, d=d: e.tensor_copy(out=KTd[di].rearrange("p (r i) -> p r i", r=d),
                                                               in_=KTd[0].rearrange("p (i r) -> p r i", r=d)),
                           reads=[("wkT", j) for j in range(4)], writes=[("wkTd", di)])
                    P.dve(lambda e, di=di, d=d: e.tensor_copy(out=QTd[0][di][0:64, :].rearrange("p (r i) -> p r i", r=d),
                                                              in_=QTd[0][0][0:64, :].rearrange("p (i r) -> p r i", r=d)),
                          reads=[("wqT", j) for j in range(4)], writes=[("wqTd", 0, di)])
                for di in (1, 2):
                    d = CONFIGS[di][1]
                    P.dve(lambda e, di=di, d=d: e.tensor_copy(out=QTd[1][di][64:128, :].rearrange("p (r i) -> p r i", r=d),
                                                              in_=QTd[1][0][64:128, :].rearrange("p (i r) -> p r i", r=d)),
                          reads=[("wqT", j) for j in range(4)], writes=[("wqTd", 1, di)])
                for di, (win, d) in enumerate(CONFIGS):
                    nb = 16 // d
                    for q8 in range(2):
                        bk = 6 + (nbk % 2)
                        nbk += 1
                        pbf = bank(bk).bitcast(BF16)
                        for t8 in range(8):
                            tile = q8 * 8 + t8
                            r, m = tile // nb, tile % nb
                            t0 = r + d * 128 * m
                            P.pe(lambda e, t0=t0, d=d, pbf=pbf, t8=t8: e.transpose(
                                out=pbf[:, t8 * 128:(t8 + 1) * 128], in_=VT[:, t0:t0 + d * 127 + 1:d], identity=identb[:]),
                                reads=[("VT", j) for j in range(4)] + ["identb"], writes=[("ps", bk)])
                        P.act(lambda e, di=di, q8=q8, pbf=pbf: e.activation(
                            out=Vb[di][:, q8 * 8:(q8 + 1) * 8, :, 0:64],
                            in_=pbf.rearrange("p (a h e) -> p a h e", a=8, h=2), func=AF.Copy),
                            reads=[("ps", bk)], writes=[("Vb", di)])

            def B_head(hp, hl, base):
                h = 2 * hp + hl
                hpar = h % 2
                p0 = 64 * hl
                for di in range(3):
                    P.dma("sp", "tt%d" % hpar, lambda e, di=di: e.dma_start(
                        out=Tt[hpar][di].rearrange("p (a b) -> p a b", a=2),
                        in_=bass.AP(texp.tensor, (di * 8 + h) * 128 * 256, [[256, 128], [0, 2], [1, 256]])),
                        writes=[("Tt", hpar, di)])
                started = [False] * 4
                steps = []
                for di, (win, d) in enumerate(CONFIGS):
                    nb = 16 // d
                    if nb >= 2:
                        for r in range(d):
                            for m in range(0, nb, 2):
                                steps.append((di, d, nb, [(r, m, 256, 0), (r, m + 1, 256 if m + 2 < nb else 128, 256)]))
                    else:
                        for r in range(0, d, 2):
                            steps.append((di, d, nb, [(r, 0, 128, 0), (r + 1, 0, 128, 256)]))

                def region(ap512, subs):
                    if subs[0][2] == 256:
                        return ap512[:, 0:256 + subs[1][2]]
                    return ap512.rearrange("p (a b) -> p a b", a=2)[:, :, 0:128]

                def S_step(st, sidx):
                    di, d, nb, subs = st
                    sb_ = sidx % NSB
                    bk = 4 + sb_
                    for (r, m, width, coff) in subs:
                        bs0 = r * (2048 // d) + 128 * m
                        P.pe(lambda e, bs0=bs0, width=width, coff=coff: e.matmul(
                            PS[:, bk * 512 + coff:bk * 512 + coff + width], lhsT=KTd[di][:, bs0:bs0 + 128],
                            rhs=QTd[hl][di][:, bs0:bs0 + width], start=True, stop=True),
                            reads=[("wkT", j) for j in range(4)] + [("wqT", j) for j in range(4)] +
                            ([("wkTd", di), ("wqTd", hl, di)] if di > 0 else []), writes=[("ps", bk)])
                    P.dve(lambda e: e.tensor_tensor(out=region(E32[sb_], subs), in0=region(bank(bk), subs),
                                                    in1=region(Tt[hpar][di], subs), op=ALU.add),
                          reads=[("ps", bk), ("Tt", hpar, di)], writes=[("E32", sb_)])
                    P.act(lambda e: e.activation(out=region(Pt[sb_], subs), in_=region(E32[sb_], subs), func=AF.Exp),
                          reads=[("E32", sb_)], writes=[("Pt", sb_)])

                def PV_step(st, sidx):
                    di, d, nb, subs = st
                    sb_ = sidx % NSB
                    for (r, m, width, coff) in subs:
                        tile = r * nb + m
                        lhs = Vb[di][:, tile, hl, :]
                        for qb in ((m, m + 1) if m + 1 < nb else (m,)):
                            c0 = coff + (qb - m) * 128
                            if d < 16:
                                if d == 1:
                                    bk, cs, step = qb // 4, (qb % 4) * 128, 1
                                else:
                                    bk, cs, step = qb, r, 4
                                pieces = [(bk, cs, step, 128, c0)]
                            else:
                                pieces = [(b4, r, 16, 32, c0 + 32 * b4) for b4 in range(4)]
                            for (bk, cs, step, cnt, pc) in pieces:
                                st_flag = not started[bk]
                                started[bk] = True
                                P.pe(lambda e, bk=bk, cs=cs, step=step, cnt=cnt, pc=pc, st_flag=st_flag, lhs=lhs: e.matmul(
                                    PS[0:65, bk * 512 + cs:bk * 512 + cs + step * (cnt - 1) + 1:step], lhsT=lhs,
                                    rhs=Pt[sb_][:, pc:pc + cnt], start=st_flag, stop=False, skip_group_check=True),
                                    reads=[("Vb", di), ("Pt", sb_)], writes=[("ps", bk)])

                SK = NSB - 1
                for j in range(len(steps) + SK):
                    if j < len(steps):
                        S_step(steps[j], base + j)
                    if j - SK >= 0:
                        PV_step(steps[j - SK], base + j - SK)
                if stop == ("Bs", l):
                    return len(steps)
                for b4 in range(4):
                    ab = accS[b4 % 2]
                    P.act(lambda e, b4=b4, ab=ab: e.activation(out=ab, in_=bank(b4, 512, 65), func=AF.Copy),
                          reads=[("ps", b4)], writes=[("accS", b4 % 2)])
                    bk = 6 + (b4 % 2)
                    for j in range(4):
                        P.pe(lambda e, j=j, ab=ab, bk=bk: e.transpose(out=PS[:, bk * 512 + j * 65:bk * 512 + (j + 1) * 65],
                                                                      in_=ab[:, j * 128:(j + 1) * 128], identity=identf[0:65, 0:65]),
                             reads=[("accS", b4 % 2), "identf"], writes=[("ps", bk)])
                    pv = bank(bk, 260).rearrange("p (j e) -> p j e", j=4)
                    P.dve(lambda e, pv=pv: e.reciprocal(out=rden.unsqueeze(2), in_=pv[:, :, 64:65]),
                          reads=[("ps", bk)], writes=["rden"])
                    P.dve(lambda e, pv=pv, b4=b4: e.tensor_tensor(out=obtok[:, b4 * 4:(b4 + 1) * 4, p0:p0 + 64], in0=pv[:, :, 0:64],
                                                                  in1=rden.unsqueeze(2).to_broadcast([128, 4, 64]), op=ALU.mult),
                          reads=[("ps", bk), "rden"], writes=[("obtok", hl)])
                return len(steps)

            def B_pair_end(hp):
                for i in range(16):
                    P.act(lambda e, i=i: e.activation(out=junkB, in_=obtok[:, i, :], func=AF.Square, accum_out=ssqb[:, i, hp:hp + 1]),
                          reads=[("obtok", 0), ("obtok", 1)], writes=["junkB", ("ssqb", hp)])
                for i8 in range(2):
                    bk = 6 + i8
                    pbf = bank(bk).bitcast(BF16)
                    for j in range(8):
                        i = i8 * 8 + j
                        P.pe(lambda e, i=i, j=j, pbf=pbf: e.transpose(out=pbf[:, j * 128:(j + 1) * 128], in_=obtok[:, i, :], identity=identb[:]),
                             reads=[("obtok", 0), ("obtok", 1), "identb"], writes=[("ps", bk)])
                    P.act(lambda e, i8=i8, pbf=pbf: e.activation(out=ost[i8], in_=pbf, func=AF.Copy),
                          reads=[("ps", bk)], writes=[("ost", i8)])
                    P.dma("sp", "ob", lambda e, i8=i8: e.dma_start(out=obT_d[:, hp, i8 * 1024:(i8 + 1) * 1024], in_=ost[i8]),
                          reads=[("ost", i8)], writes=["obT_d"])

            sidx = 0
            for hp_ in range(4):
                B_proj(hp_)
                if stop == ("Bp", l):
                    P.barrier(); dump_and_finish(); return True
                for hl_ in range(2):
                    sidx += B_head(hp_, hl_, sidx)
                    if stop in (("Bs", l), ("Bh", l)):
                        P.barrier(); dump_and_finish(); return True
                B_pair_end(hp_)
                if stop == ("Be", l):
                    P.barrier(); dump_and_finish(); return True
            P.dve(lambda e: e.tensor_reduce(out=rsb, in_=ssqb, axis=AX.X, op=ALU.add), reads=[("ssqb", j) for j in range(4)], writes=["rsb0"])
            P.dve(lambda e: e.tensor_scalar(out=rsb, in0=rsb, scalar1=1.0 / 512, scalar2=EPS, op0=ALU.mult, op1=ALU.add),
                  reads=["rsb0"], writes=["rsb1"])
            P.pool(lambda e: e.tensor_tensor(out=rsb, in0=rsb, in1=nhalf[:, 0:1].to_broadcast([128, 16]), op=ALU.pow),
                   reads=["rsb1", "nhalf"], writes=["rsb"])
            if stop == ("B1", l):
                P.barrier()
                P.dma("sp", "dbg", lambda e: e.dma_start(out=dbg_obT, in_=obT_d))
                P.barrier(); dump_and_finish(); return True
            P.barrier()
            ar.reset()
            obT = ar.bf16([128, 4, 2048])
            P.dma("sp", "a0", lambda e: e.dma_start(out=obT, in_=obT_d), writes=["obT"])
            w_ob = ar.bf16([128, 4, 1024])
            junkb = ar.bf16([128, 1024])
            g1b2 = ar.f32([128, 1024])
            gabf2 = ar.f32([128, 8])
            P.dma("pool", "wA", lambda e: e.dma_start(out=w_ob, in_=wout_d[l, 512:1024, :].rearrange("(k p) n -> p k n", p=128)),
                  writes=["w_ob"])
            P.dma("sp", "a0", lambda e: e.dma_start(out=g1b2, in_=modscr[l, 2048:3072].partition_broadcast(128)), writes=["g1b2"])
            P.dma("sp", "a0", lambda e: e.dma_start(out=gabf2, in_=gab_d[l]), writes=["gabf2"])
            for c in range(4):
                P.dve(lambda e, c=c: e.scalar_tensor_tensor(out=w_ob[:, c, :], in0=w_ob[:, c, :], scalar=gabf2[:, 4 + c:5 + c], in1=g1b2,
                                                             op0=ALU.mult, op1=ALU.mult),
                      reads=["w_ob", "gabf2", "g1b2"], writes=["w_ob"])
            for i in range(16):
                for hf in range(2):
                    bk = 2 * (i % 2) + hf
                    for c in range(4):
                        P.pe(lambda e, c=c, hf=hf, bk=bk, i=i: e.matmul(bank(bk), lhsT=obT[:, c, i * 128:(i + 1) * 128],
                                                                        rhs=w_ob[:, c, hf * 512:(hf + 1) * 512], start=(c == 0), stop=(c == 3)),
                             reads=["w_ob", "obT"], writes=[("ps", bk)])
                    P.dve(lambda e, hf=hf, bk=bk, i=i: e.scalar_tensor_tensor(out=x[:, i, hf * 512:(hf + 1) * 512], in0=bank(bk),
                                                                              scalar=rsb[:, i:i + 1], in1=x[:, i, hf * 512:(hf + 1) * 512],
                                                                              op0=ALU.mult, op1=ALU.add),
                          reads=[("ps", bk), ("x", i)], writes=[("x", i)])
                P.act(lambda e, i=i: e.activation(out=junkb, in_=x[:, i, :], func=AF.Square, accum_out=ssqN[:, i:i + 1]),
                      reads=[("x", i)], writes=["junkb", ("ssq", i)])
            if stop == ("B", l):
                P.barrier(); dump_and_finish(); return True

            ar2.reset()
            sg = [ar2.bf16([128, 512]) for _ in range(2)]
            actT = [ar2.bf16([128, 4, 512]) for _ in range(2)]
            junkM = ar2.bf16([128, 1024])
            if l == 0:
                stageM = [ar2.bf16([128, 8, 512]) for _ in range(2)]
                modbrM = [ar2.f32([1, 512]) for _ in range(2)]
                mrowM = [ar2.f32([1, 512]) for _ in range(2)]
            assert ar2.off <= 11 * 1024, ar2.off
            ar2.off = 11 * 1024
            wgb = [ar2.bf16([128, 8, 512]) for _ in range(2)]
            wub = [ar2.bf16([128, 8, 512]) for _ in range(2)]
            wdb = [ar2.bf16([128, 4, 1024]) for _ in range(2)]
            g2b = ar2.f32([128, 1024])

            def load_expert(ex):
                pb = ex % 2
                for kk in range(2):
                    P.dma("pool", "wg%d" % pb, lambda e, kk=kk: e.dma_start(
                        out=wgb[pb][:, kk * 4:(kk + 1) * 4, :],
                        in_=wg_d[l, ex, kk * 512:(kk + 1) * 512, :].rearrange("(k p) n -> p k n", p=128)), writes=[("wg", pb)])
                    P.dma("pool", "wu%d" % pb, lambda e, kk=kk: e.dma_start(
                        out=wub[pb][:, kk * 4:(kk + 1) * 4, :],
                        in_=wu_d[l, ex, kk * 512:(kk + 1) * 512, :].rearrange("(k p) n -> p k n", p=128)), writes=[("wu", pb)])
                    P.dma("pool", "wd%d" % pb, lambda e, kk=kk: e.dma_start(
                        out=wdb[pb][:, kk * 2:(kk + 1) * 2, :],
                        in_=wd_d[l, ex, kk * 256:(kk + 1) * 256, :].rearrange("(k p) n -> p k n", p=128)), writes=[("wd", pb)])
                for c in range(4):
                    P.pool(lambda e, c=c: e.tensor_tensor(out=wdb[pb][:, c, :], in0=wdb[pb][:, c, :], in1=g2b, op=ALU.mult),
                           reads=[("wd", pb), "g2b"], writes=[("wd", pb)])

            def M_pre():
                P.dma("sp", "m0", lambda e: e.dma_start(out=g2b, in_=modscr[l, 5120:6144].partition_broadcast(128)), writes=["g2b"])
                load_expert(0)
                load_expert(1)

            norm_phase(l, 2, pre=M_pre, have_ssq=True)
            if stop == ("N2a", l):
                P.barrier(); dump_and_finish(); return True
            P.barrier()

            def GU(ex, tb, n):
                pb = ex % 2
                ab = n % 2
                for fc in range(4):
                    q = fc % 2
                    for k in range(8):
                        P.pe(lambda e, k=k, fc=fc, q=q: e.matmul(bank(q), lhsT=wgb[pb][:, k, fc * 128:(fc + 1) * 128],
                                                                 rhs=hT[:, k, tb * 512:(tb + 1) * 512], start=(k == 0), stop=(k == 7)),
                             reads=[("wg", pb)], writes=[("ps", q)])
                    for k in range(8):
                        P.pe(lambda e, k=k, fc=fc, q=q: e.matmul(bank(2 + q), lhsT=wub[pb][:, k, fc * 128:(fc + 1) * 128],
                                                                 rhs=hT[:, k, tb * 512:(tb + 1) * 512], start=(k == 0), stop=(k == 7)),
                             reads=[("wu", pb)], writes=[("ps", 2 + q)])
                    P.act(lambda e, q=q: e.activation(out=sg[q], in_=bank(q), func=AF.Silu), reads=[("ps", q)], writes=[("sg", q)])
                    P.dve(lambda e, q=q, fc=fc: e.tensor_tensor(out=actT[ab][:, fc, :], in0=bank(2 + q), in1=sg[q], op=ALU.mult),
                          reads=[("ps", 2 + q), ("sg", q)], writes=[("actT", ab)])

            def DN(ex, tb, n):
                pb = ex % 2
                ab = n % 2
                for tt in range(4):
                    i = tb * 4 + tt
                    for hf in range(2):
                        bk = 4 + ((tt * 2 + hf) % 4)
                        for fc in range(4):
                            P.pe(lambda e, fc=fc, hf=hf, tt=tt, bk=bk: e.matmul(bank(bk), lhsT=actT[ab][:, fc, tt * 128:(tt + 1) * 128],
                                                                                rhs=wdb[pb][:, fc, hf * 512:(hf + 1) * 512],
                                                                                start=(fc == 0), stop=(fc == 3)),
                                 reads=[("actT", ab), ("wd", pb)], writes=[("ps", bk)])
                        P.dve(lambda e, hf=hf, i=i, bk=bk: e.scalar_tensor_tensor(out=x[:, i, hf * 512:(hf + 1) * 512], in0=bank(bk),
                                                                                  scalar=gates[:, i, ex:ex + 1], in1=x[:, i, hf * 512:(hf + 1) * 512],
                                                                                  op0=ALU.mult, op1=ALU.add),
                              reads=[("ps", bk), "gates", ("x", i)], writes=[("x", i)])
                    if ex == 15:
                        P.act(lambda e, i=i: e.activation(out=junkM, in_=x[:, i, :], func=AF.Square, accum_out=ssqN[:, i:i + 1]),
                              reads=[("x", i)], writes=["junkM", ("ssq", i)])

            seqs = [(ex, tb) for ex in range(16) for tb in range(4)]
            for n in range(len(seqs) + 1):
                if n < len(seqs):
                    GU(seqs[n][0], seqs[n][1], n)
                if n == 0:
                    router_phase()
                    assert ar.off <= 11 * 1024, ar.off
                    if stop == ("N2", l):
                        P.barrier(); dump_and_finish(); return True
                if n >= 1:
                    ex, tb = seqs[n - 1]
                    DN(ex, tb, n - 1)
                    if tb == 3 and ex + 2 < 16:
                        load_expert(ex + 2)
                    if tb == 3 and l == 0:
                        if 1 <= ex <= 12:
                            j = ex - 1
                            mod_chunk(1, j, j % 2, stageM[j % 2], modbrM[j % 2], mrowM[j % 2], 7, part=2)
                        if ex < 12:
                            mod_chunk(1, ex, ex % 2, stageM[ex % 2], modbrM[ex % 2], mrowM[ex % 2], 7, part=1,
                                      extra_w=["rt"] if ex < 2 else ())
            if stop == ("M", l):
                P.barrier(); dump_and_finish(); return True

            return False

        for l_ in range(n_layers):
            if layer(l_):
                return nc

        P.barrier()
        ar.reset()
        fgb = ar.f32([128, 1024])
        ob = [ar.f32([128, 1024]) for _ in range(3)]
        junk = ar.bf16([128, 1024])
        ssq = ssqN; rstd = ar.f32([128, 16])
        P.dma("sp", "n0", lambda e: e.dma_start(out=fgb, in_=fg_d[0, :].partition_broadcast(128)), writes=["fgb"])
        P.act(lambda e: e.activation(out=rstd, in_=ssq, func=AF.Sqrt, bias=eps_t[:, 0:1], scale=1.0 / 1024),
              reads=[("ssq", i) for i in range(16)] + ["eps"], writes=["rstd0"])
        P.dve(lambda e: e.reciprocal(out=rstd, in_=rstd), reads=["rstd0"], writes=["rstd"])
        for i in range(16):
            o = ob[i % 3]
            P.dve(lambda e, i=i, o=o: e.scalar_tensor_tensor(out=o, in0=x[:, i, :], scalar=rstd[:, i:i + 1], in1=fgb,
                                                             op0=ALU.mult, op1=ALU.mult),
                  reads=[("x", i), "rstd", "fgb"], writes=[("ob", i % 3)])
            P.dma("sp", "out", lambda e, i=i, o=o: e.dma_start(out=out_d[i * 128:(i + 1) * 128, :], in_=o),
                  reads=[("ob", i % 3)], writes=[("out", i)])
        if dbg:
            P.barrier(); dump_and_finish(); return nc
        P.emit(final_wait_streams=["out"])
    return nc


def t5_bucket_np(dist):
    dist = np.maximum(dist, 0)
    ratio = np.log(np.maximum(dist, 1) / 16) / np.log(2048 / 16)
    large = 16 + np.floor(ratio * 16).astype(np.int64)
    large = np.minimum(large, 31)
    return np.where(dist < 16, dist, large).astype(np.int32)


def host_consts():
    btab = np.zeros((3, 33, 384), np.float32)
    for di, (win, d) in enumerate(CONFIGS):
        for j in range(384):
            rel = j - 127
            if 0 <= rel <= 128:
                btab[di, int(t5_bucket_np(np.array(rel * d))), j] = 1.0
            else:
                btab[di, 32, j] = NEG
    ind = np.zeros((8, 512), np.float32)
    for g in range(8):
        ind[g, g * 64:(g + 1) * 64] = 1.0
    trilm = np.triu(np.ones((128, 128), np.float32))
    return dict(identf=np.eye(128, dtype=np.float32), btab=btab, ind=ind, trilm=trilm,
                jmat=np.ascontiguousarray(np.eye(128, dtype=np.float32)[::-1]))


def make_in_maps(inputs, cores):
    f = lambda a: np.ascontiguousarray(np.asarray(a, dtype=np.float32))
    shared = dict(
        rel_bias=f(inputs["rel_bias"]), router_w=f(inputs["router_w"]), router_b=f(inputs["router_b"]).reshape(1, 16),
        mod_w=f(inputs["mod_w"]), mod_b=f(inputs["mod_b"]), norm1_g=f(inputs["norm1_g"]), w_in=f(inputs["w_in"]),
        gmlp_ln_g=f(inputs["gmlp_ln_g"]), gmlp_ln_b=f(inputs["gmlp_ln_b"]), gmlp_ws=f(inputs["gmlp_ws"]),
        gmlp_bs=f(inputs["gmlp_bs"]), w_out=f(inputs["w_out"]), norm2_g=f(inputs["norm2_g"]),
        moe_w_gate=f(inputs["moe_w_gate"]), moe_w_up=f(inputs["moe_w_up"]), moe_w_down=f(inputs["moe_w_down"]),
        final_g=f(inputs["final_g"]).reshape(1, 1024),
    )
    gab = np.concatenate([f(inputs["out_norm_a_g"]), f(inputs["out_norm_b_g"])], axis=1)
    shared["gab"] = np.ascontiguousarray(gab.reshape(2, 8, 128).transpose(0, 2, 1))
    shared.update(host_consts())
    x = f(inputs["x"]); c = f(inputs["c"])
    maps = []
    for b in cores:
        m = dict(shared)
        m["x"] = np.ascontiguousarray(x[b])
        m["cT"] = np.ascontiguousarray(c[b].reshape(8, 128).T)
        maps.append(m)
    return maps


def kernel(**inputs):
    nc = build()
    maps = make_in_maps(inputs, list(range(8)))
    res = run_bass_kernel_spmd(nc, maps, core_ids=list(range(8)))
    return np.stack([np.asarray(r["out"], dtype=np.float32) for r in res.results], axis=0)
```

```python
import contextlib
import os
import numpy as np
import concourse.bass as bass
import concourse.mybir as mybir
from concourse.bass_utils import run_bass_kernel_spmd

F32 = mybir.dt.float32
BF16 = mybir.dt.bfloat16
ALU = mybir.AluOpType
AF = mybir.ActivationFunctionType
AX = mybir.AxisListType
ENGS = ("pe", "act", "dve", "pool", "sp")
EPS = 1e-6
NEG = -30000.0
CONFIGS = ((128, 1), (512, 4), (2048, 16))


class Prog:
    def __init__(self, nc):
        self.nc = nc
        self.ins = []
        self.last_w = {}
        self.readers = {}
        self.stream_cnt = {}
        self.stream_last = {}

    def add(self, eng, fn, reads=(), writes=(), dma=None):
        i = len(self.ins)
        deps = {}

        def dep(j):
            if j is None:
                return
            pj = self.ins[j]
            deps[j] = self.stream_cnt[pj["dma"]] if pj["dma"] is not None else None

        for r in reads:
            dep(self.last_w.get(r))
        for w in writes:
            dep(self.last_w.get(w))
            for j in self.readers.get(w, ()):
                dep(j)
        pruned = {}
        for j, c in deps.items():
            pj = self.ins[j]
            if pj["dma"] is None and pj["eng"] == eng:
                if eng == "pe":
                    continue
                if not any(self.last_w.get(r) == j for r in reads):
                    continue
            pruned[j] = c
            if pj["dma"] is None:
                pj["needs_inc"] = True
        rec = dict(eng=eng, fn=fn, deps=pruned, dma=dma, needs_inc=False, val=None)
        if dma is not None:
            self.stream_cnt[dma] = self.stream_cnt.get(dma, 0) + 1
            self.stream_last[dma] = i
        self.ins.append(rec)
        for r in reads:
            self.readers.setdefault(r, []).append(i)
        for w in writes:
            self.last_w[w] = i
            self.readers[w] = []
        return i

    def pe(self, fn, reads=(), writes=()):
        return self.add("pe", fn, reads, writes)

    def act(self, fn, reads=(), writes=()):
        return self.add("act", fn, reads, writes)

    def dve(self, fn, reads=(), writes=()):
        return self.add("dve", fn, reads, writes)

    def pool(self, fn, reads=(), writes=()):
        return self.add("pool", fn, reads, writes)

    def dma(self, q, stream, fn, reads=(), writes=()):
        return self.add(q, fn, reads, writes, dma=stream)

    def barrier(self):
        last = {}
        for idx, rec in enumerate(self.ins):
            if rec["dma"] is None and not rec.get("bar"):
                last[rec["eng"]] = idx
        for e in ENGS:
            deps = {}
            for e2, j in last.items():
                if e2 == e and e == "pe":
                    continue
                deps[j] = None
                self.ins[j]["needs_inc"] = True
            for s, c in self.stream_cnt.items():
                deps[self.stream_last[s]] = c
            self.ins.append(dict(eng=e, fn=None, deps=deps, dma=None, needs_inc=False, val=None, bar=True))
        self.last_w = {}
        self.readers = {}

    def emit(self, final_wait_streams=()):
        nc = self.nc
        streams = sorted(self.stream_cnt.keys())
        with contextlib.ExitStack() as es:
            esem = {e: es.enter_context(nc.semaphore("s_" + e)) for e in ENGS}
            ssem = {s: es.enter_context(nc.semaphore("d_" + str(s))) for s in streams}
            cnt = {e: 0 for e in ENGS}
            for rec in self.ins:
                if rec["dma"] is None and rec["needs_inc"]:
                    cnt[rec["eng"]] += 1
                    rec["val"] = cnt[rec["eng"]]
            per_eng = {e: [] for e in ENGS}
            for rec in self.ins:
                per_eng[rec["eng"]].append(rec)
            ins = self.ins
            block = es.enter_context(nc.Block())

            def run(ename, eng):
                waited = {}
                for rec in per_eng[ename]:
                    for j, c in rec["deps"].items():
                        pj = ins[j]
                        if pj["dma"] is not None:
                            sem, v, key = ssem[pj["dma"]], c * 16, ("d", pj["dma"])
                        else:
                            sem, v, key = esem[pj["eng"]], pj["val"], ("e", pj["eng"])
                        if waited.get(key, 0) >= v:
                            continue
                        waited[key] = v
                        eng.wait_ge(sem, v)
                    if rec["fn"] is None:
                        continue
                    bi = rec["fn"](eng)
                    if rec["dma"] is not None:
                        bi.then_inc(ssem[rec["dma"]], 16)
                    elif rec["needs_inc"]:
                        bi.then_inc(esem[ename], 1)
                if ename == "sp":
                    for s in final_wait_streams:
                        eng.wait_ge(ssem[s], self.stream_cnt[s] * 16)

            block.tensor(lambda e: run("pe", e))
            block.scalar(lambda e: run("act", e))
            block.vector(lambda e: run("dve", e))
            block.gpsimd(lambda e: run("pool", e))
            block.sync(lambda e: run("sp", e))


class Arena:
    def __init__(self, t, words):
        self.t = t
        self.words = words
        self.off = 0

    def reset(self):
        self.off = 0

    def _take(self, nwords):
        nwords = (nwords + 7) // 8 * 8
        a = self.off
        self.off += nwords
        assert self.off <= self.words, ("arena overflow", self.off, self.words)
        return a

    def f32(self, shape):
        n = int(np.prod(shape[1:]))
        a = self._take(n)
        ap = self.t[0:shape[0], a:a + n]
        return self._shape(ap, shape)

    def bf16(self, shape):
        n = int(np.prod(shape[1:]))
        a = self._take((n + 1) // 2)
        ap = self.t[0:shape[0], a:a + (n + 1) // 2].bitcast(BF16)[:, 0:n]
        return self._shape(ap, shape)

    @staticmethod
    def _shape(ap, shape):
        if len(shape) == 2:
            return ap
        if len(shape) == 3:
            return ap.rearrange("p (a b) -> p a b", a=shape[1])
        if len(shape) == 4:
            return ap.rearrange("p (a b c) -> p a b c", a=shape[1], b=shape[2])
        raise ValueError(shape)


NSB = 4
ARENA_WORDS = 25 * 1024


def build(stop=None, n_layers=2):
    nc = bass.Bass("TRN2", target_bir_lowering=False)
    din = lambda name, shape: nc.dram_tensor(name, shape, F32, kind="ExternalInput").ap()
    x_d = din("x", [2048, 1024])
    cT_d = din("cT", [128, 8])
    relb_d = din("rel_bias", [32, 8])
    rw_d = din("router_w", [1024, 16])
    rb_d = din("router_b", [1, 16])
    modw_d = din("mod_w", [2, 1024, 6144])
    modb_d = din("mod_b", [2, 6144])
    n1g_d = din("norm1_g", [2, 1024])
    win_d = din("w_in", [2, 1024, 2560])
    lng_d = din("gmlp_ln_g", [2, 512])
    lnb_d = din("gmlp_ln_b", [2, 512])
    ws_d = din("gmlp_ws", [2, 8, 128, 128])
    bs_d = din("gmlp_bs", [2, 8, 128])
    gab_d = din("gab", [2, 128, 8])
    wout_d = din("w_out", [2, 1024, 1024])
    n2g_d = din("norm2_g", [2, 1024])
    wg_d = din("moe_w_gate", [2, 16, 1024, 512])
    wu_d = din("moe_w_up", [2, 16, 1024, 512])
    wd_d = din("moe_w_down", [2, 16, 512, 1024])
    fg_d = din("final_g", [1, 1024])
    identf_d = din("identf", [128, 128])
    btab_d = din("btab", [3, 33, 384])
    ind_d = din("ind", [8, 512])
    tril_d = din("trilm", [128, 128])
    jmat_d = din("jmat", [128, 128])
    out_d = nc.dram_tensor("out", [2048, 1024], F32, kind="ExternalOutput").ap()
    modscr = nc.dram_tensor("modscr", [2, 6144], F32, kind="Internal").ap()
    gscr_h = nc.dram_tensor("gscr", [3, 8, 384], F32, kind="Internal")
    gscr = gscr_h.ap()
    texp = nc.dram_tensor("texp", [3, 8, 128, 256], F32, kind="Internal").ap()
    obT_d = nc.dram_tensor("obT_d", [128, 4, 2048], BF16, kind="Internal").ap()
    dbg = stop is not None
    if dbg:
        dbg_x = nc.dram_tensor("dbg_x", [2048, 1024], F32, kind="ExternalOutput").ap()
        dbg_hT = nc.dram_tensor("dbg_hT", [128, 8, 2048], BF16, kind="ExternalOutput").ap()
        dbg_g = nc.dram_tensor("dbg_g", [128, 256], F32, kind="ExternalOutput").ap()
        dbg_obT = nc.dram_tensor("dbg_obT", [128, 4, 2048], BF16, kind="ExternalOutput").ap()

    P = Prog(nc)
    es = contextlib.ExitStack()
    with es:
        sb = lambda name, shape, dt: es.enter_context(nc.sbuf_tensor(name, shape, dt))
        x = sb("x_sb", [128, 16, 1024], F32)
        hT = sb("hT", [128, 8, 2048], BF16)
        identf = sb("identf_sb", [128, 128], F32)
        identb = sb("identb_sb", [128, 128], BF16)
        trilm = sb("trilm_sb", [128, 128], F32)
        ind = sb("ind_sb", [8, 512], F32)
        ones_bf = sb("ones_bf", [1, 128], BF16)
        eps_t = sb("eps_t", [128, 1], F32)
        nhalf = sb("nhalf_t", [128, 1], F32)
        gates = sb("gates_sb", [128, 16, 16], F32)
        rbb = sb("rbb_sb", [128, 16, 16], F32)
        rw = sb("rw_sb", [128, 8, 16], F32)
        statA = sb("statA", [128, 64], F32)
        cact = sb("cact_sb", [128, 8], BF16)
        arena_t = sb("arena", [128, ARENA_WORDS], F32)
        PS = es.enter_context(nc.psum_tensor("ps", [128, 4096], F32))
        ar = Arena(arena_t, ARENA_WORDS)
        ar2 = Arena(arena_t, ARENA_WORDS)

        def bank(b, n=512, parts=128):
            return PS[0:parts, b * 512:b * 512 + n]

        P.dma("sp", "c0", lambda e: e.dma_start(out=identf[:], in_=identf_d), writes=["identf"])
        P.dma("sp", "c0", lambda e: e.dma_start(out=trilm[:], in_=tril_d), writes=["trilm"])
        P.dma("sp", "c0", lambda e: e.dma_start(out=ind[:], in_=ind_d), writes=["ind"])
        P.dma("sp", "c0", lambda e: e.dma_start(out=rw[:], in_=rw_d.rearrange("(k p) e -> p k e", p=128)), writes=["rw"])
        P.dma("sp", "c0", lambda e: e.dma_start(
            out=rbb[:], in_=bass.AP(rb_d.tensor, 0, [[0, 128], [0, 16], [1, 16]])), writes=["rbb"])
        P.dve(lambda e: e.tensor_copy(out=identb[:], in_=identf[:]), reads=["identf"], writes=["identb"])
        P.dve(lambda e: e.memset(ones_bf[:], 1.0), writes=["ones_bf"])
        P.dve(lambda e: e.memset(eps_t[:], EPS), writes=["eps"])
        P.dve(lambda e: e.memset(nhalf[:], -0.5), writes=["nhalf"])

        ar.reset()
        cT = ar.f32([128, 8])
        stageR = [ar.bf16([128, 3072]) for _ in range(3)]
        modbrR = ar.f32([1, 3072])
        mrowR = ar.f32([1, 3072])
        rb33 = ar.f32([33, 8])
        btab = ar.f32([33, 3, 384])
        grow = ar.f32([8, 3, 384])
        P.dma("sp", "c0", lambda e: e.dma_start(out=cT, in_=cT_d), writes=["cT"])
        P.act(lambda e: e.activation(out=cact[:], in_=cT, func=AF.Silu), reads=["cT"], writes=["cact"])
        def mod_chunk(l, j, pb, stage_, modbr_, mrow_, bk, part=3, extra_w=()):
            if part & 1:
                P.dma("pool", "mw%d" % pb, lambda e: e.dma_start(
                    out=stage_, in_=modw_d[l, :, j * 512:(j + 1) * 512].rearrange("(k p) n -> p k n", p=128)),
                    writes=[("stage", pb)] + list(extra_w))
                P.dma("sp", "mb%d" % pb, lambda e: e.dma_start(out=modbr_, in_=modb_d[l:l + 1, j * 512:(j + 1) * 512]),
                      writes=[("modbr", pb)] + list(extra_w))
            if not (part & 2):
                return
            for k in range(8):
                P.pe(lambda e, k=k: e.matmul(bank(bk, 512, 1), lhsT=cact[:, k:k + 1], rhs=stage_[:, k, :],
                                             start=(k == 0), stop=(k == 7)),
                     reads=["cact", ("stage", pb)], writes=[("ps", bk)])
            P.dve(lambda e: e.tensor_tensor(out=mrow_, in0=bank(bk, 512, 1), in1=modbr_, op=ALU.add),
                  reads=[("ps", bk), ("modbr", pb)], writes=[("mrow", pb)])
            P.dma("sp", "ms%d" % pb, lambda e: e.dma_start(out=modscr[l:l + 1, j * 512:(j + 1) * 512], in_=mrow_),
                  reads=[("mrow", pb)], writes=[("modscr", l)])

        for i in range(16):
            P.dma("act", "x", lambda e, i=i: e.dma_start(out=x[:, i, :], in_=x_d[i * 128:(i + 1) * 128, :]),
                  writes=[("x", i)])
        P.dve(lambda e: e.memset(rb33, 1.0), writes=["rb33"])
        P.dma("sp", "c1", lambda e: e.dma_start(out=rb33[0:32, :], in_=relb_d), writes=["rb33"])
        P.dma("sp", "c1", lambda e: e.dma_start(out=btab, in_=btab_d.rearrange("d r j -> r d j")), writes=["btab"])
        jmat = ar.f32([128, 128])
        thk = [ar.f32([128, 8, 256]) for _ in range(3)]
        tfx = [ar.f32([128, 8, 256]) for _ in range(2)]
        P.dma("sp", "c1", lambda e: e.dma_start(out=jmat, in_=jmat_d), writes=["jmat"])
        for d in range(3):
            P.pe(lambda e, d=d: e.matmul(bank(6 + d % 2, 384, 8), lhsT=rb33[:, :], rhs=btab[:, d, :], start=True, stop=True),
                 reads=["rb33", "btab"], writes=[("ps", 6 + d % 2)])
            P.dve(lambda e, d=d: e.tensor_copy(out=grow[:, d, :], in_=bank(6 + d % 2, 384, 8)),
                  reads=[("ps", 6 + d % 2)], writes=["grow"])
        P.dma("act", "c2", lambda e: e.dma_start(out=gscr.rearrange("d h j -> h d j"), in_=grow),
              reads=["grow"], writes=["gscr"])
        for d in range(3):
            P.dma("act", "tk%d" % d, lambda e, d=d: e.dma_start(
                out=thk[d], in_=bass.AP(gscr_h, d * 8 * 384, [[1, 128], [384, 8], [1, 256]])),
                reads=["gscr"], writes=[("thk", d)])

        def texp_flip():
            nj = 0
            for d in range(3):
                pb = d % 2
                for j in range(4):
                    bk = 6 + nj % 2
                    nj += 1
                    P.pe(lambda e, d=d, j=j, bk=bk: e.matmul(bank(bk), lhsT=jmat, rhs=thk[d][:, 2 * j:2 * j + 2, :].rearrange("p a b -> p (a b)"),
                                                             start=True, stop=True),
                         reads=["jmat", ("thk", d)], writes=[("ps", bk)])
                    P.dve(lambda e, pb=pb, j=j, bk=bk: e.tensor_copy(out=tfx[pb][:, 2 * j:2 * j + 2, :].rearrange("p a b -> p (a b)"), in_=bank(bk)),
                          reads=[("ps", bk)], writes=[("tfx", pb)])
                P.dma("act", "tx%d" % pb, lambda e, d=d, pb=pb: e.dma_start(out=texp[d].rearrange("h p q -> p h q"), in_=tfx[pb]),
                      reads=[("tfx", pb)], writes=["texp"])

        nblk = 0
        for hh in range(2):
            P.dma("sp", "mb0", lambda e, hh=hh: e.dma_start(out=modbrR, in_=modb_d[0:1, hh * 3072:(hh + 1) * 3072]),
                  writes=["modbrR"])
            for k in range(8):
                sb_ = nblk % 3
                nblk += 1
                P.dma("pool", "mr%d" % sb_, lambda e, hh=hh, k=k, sb_=sb_: e.dma_start(
                    out=stageR[sb_], in_=modw_d[0, k * 128:(k + 1) * 128, hh * 3072:(hh + 1) * 3072]),
                    writes=[("stageR", sb_)])
                for j in range(6):
                    P.pe(lambda e, k=k, j=j, sb_=sb_: e.matmul(bank(j, 512, 1), lhsT=cact[:, k:k + 1], rhs=stageR[sb_][:, j * 512:(j + 1) * 512],
                                                               start=(k == 0), stop=(k == 7)),
                         reads=["cact", ("stageR", sb_)], writes=[("ps", j)])
            for j in range(6):
                P.dve(lambda e, j=j: e.tensor_tensor(out=mrowR[:, j * 512:(j + 1) * 512], in0=bank(j, 512, 1),
                                                     in1=modbrR[:, j * 512:(j + 1) * 512], op=ALU.add),
                      reads=[("ps", j), "modbrR"], writes=["mrowR"])
            P.dma("sp", "ms0", lambda e, hh=hh: e.dma_start(out=modscr[0:1, hh * 3072:(hh + 1) * 3072], in_=mrowR),
                  reads=["mrowR"], writes=[("modscr", 0)])
            if hh == 0:
                texp_flip()

        ssqN = statA[:, 16:32]

        def norm_phase(l, which, pre=None, post=None, have_ssq=False):
            P.barrier()
            ar.reset()
            if pre is not None:
                pre()
            gmod = ar.f32([128, 1024])
            ngb = ar.f32([128, 1024])
            shb = ar.f32([128, 1024])
            h32 = [ar.f32([128, 1024]) for _ in range(2)]
            junk = ar.bf16([128, 1024])
            ssq = ssqN
            rstd = ar.f32([128, 16])
            xh32 = [ar.f32([128, 8, 128]) for _ in range(2)] if which == 2 else None
            off_sh, off_sc = (0, 1024) if which == 1 else (3072, 4096)
            ng_d = n1g_d if which == 1 else n2g_d
            P.dma("sp", "n0", lambda e: e.dma_start(out=gmod, in_=modscr[l, off_sc:off_sc + 1024].partition_broadcast(128)),
                  writes=["gmod"])
            P.dma("sp", "n0", lambda e: e.dma_start(out=ngb, in_=ng_d[l, :].partition_broadcast(128)), writes=["ngb"])
            P.dma("sp", "n0", lambda e: e.dma_start(out=shb, in_=modscr[l, off_sh:off_sh + 1024].partition_broadcast(128)),
                  writes=["shb"])
            P.dve(lambda e: e.scalar_tensor_tensor(out=gmod, in0=gmod, scalar=1.0, in1=ngb, op0=ALU.add, op1=ALU.mult),
                  reads=["gmod", "ngb"], writes=["gmod"])
            if not have_ssq:
                for i in range(16):
                    P.act(lambda e, i=i: e.activation(out=junk, in_=x[:, i, :], func=AF.Square, accum_out=ssq[:, i:i + 1]),
                          reads=[("x", i)], writes=["junk", ("ssq", i)])
            P.act(lambda e: e.activation(out=rstd, in_=ssq, func=AF.Sqrt, bias=eps_t[:, 0:1], scale=1.0 / 1024),
                  reads=[("ssq", i) for i in range(16)] + ["eps"], writes=["rstd0"])
            P.dve(lambda e: e.reciprocal(out=rstd, in_=rstd), reads=["rstd0"], writes=["rstd"])
            for i in range(16):
                hb = h32[i % 2]
                pp = i % 2
                P.dve(lambda e, i=i, hb=hb: e.scalar_tensor_tensor(out=hb, in0=x[:, i, :], scalar=rstd[:, i:i + 1], in1=gmod,
                                                                    op0=ALU.mult, op1=ALU.mult),
                      reads=[("x", i), "rstd", "gmod"], writes=[("h32", pp)])
                P.dve(lambda e, hb=hb: e.tensor_tensor(out=hb, in0=hb, in1=shb, op=ALU.add),
                      reads=[("h32", pp), "shb"], writes=[("h32", pp)])
                for c in range(8):
                    P.pe(lambda e, c=c, hb=hb, pp=pp: e.transpose(out=PS[:, pp * 1024 + c * 128:pp * 1024 + (c + 1) * 128],
                                                                  in_=hb[:, c * 128:(c + 1) * 128], identity=identf[:]),
                         reads=[("h32", pp), "identf"], writes=[("ps", 2 * pp + c // 4)])
                if which == 1:
                    P.act(lambda e, i=i, pp=pp: e.activation(out=hT[:, :, i * 128:(i + 1) * 128],
                                                             in_=PS[:, pp * 1024:(pp + 1) * 1024].rearrange("p (c t) -> p c t", c=8),
                                                             func=AF.Copy),
                          reads=[("ps", 2 * pp), ("ps", 2 * pp + 1)], writes=[("hT", i)])
                if which == 2:
                    xb = xh32[pp]
                    P.dve(lambda e, pp=pp, xb=xb: e.tensor_copy(out=xb, in_=PS[:, pp * 1024:(pp + 1) * 1024].rearrange("p (c t) -> p c t", c=8)),
                          reads=[("ps", 2 * pp), ("ps", 2 * pp + 1)], writes=[("xh32", pp)])
                    P.act(lambda e, i=i, xb=xb: e.activation(out=hT[:, :, i * 128:(i + 1) * 128], in_=xb, func=AF.Copy),
                          reads=[("xh32", pp)], writes=[("hT", i)])
                    for c in range(8):
                        P.pe(lambda e, i=i, c=c, xb=xb: e.matmul(PS[:, 4 * 512 + i * 16:4 * 512 + (i + 1) * 16], lhsT=xb[:, c, :],
                                                                 rhs=rw[:, c, :], start=(c == 0), stop=(c == 7)),
                             reads=[("xh32", pp), "rw"], writes=[("ps", 4)])

        def norm_phase_post(post):
            if post is not None:
                post()

        def router_phase():
            T = lambda: ar.f32([128, 16, 16])
            L, E_, pr, sel, t1, t2, t3, selm = T(), T(), T(), T(), T(), T(), T(), T()
            mx = ar.f32([128, 16]); sm = ar.f32([128, 16])
            g4 = ar.f32([128, 16, 4]); p6 = [ar.f32([128, 16, 4]) for _ in range(6)]
            gmx = ar.f32([128, 16]); gm = ar.f32([128, 16, 4])
            m1 = ar.f32([128, 16]); m2 = ar.f32([128, 16])
            bc = lambda a, n: a.unsqueeze(2).to_broadcast([128, 16, n])
            v = lambda e: e
            P.dve(lambda e: e.tensor_copy(out=L, in_=PS[:, 2048:2048 + 256].rearrange("p (a b) -> p a b", a=16)),
                  reads=[("ps", 4)], writes=["rt"])
            seq = []
            seq.append(lambda e: e.tensor_reduce(out=mx, in_=L, axis=AX.X, op=ALU.max))
            seq.append(lambda e: e.tensor_tensor(out=t1, in0=L, in1=bc(mx, 16), op=ALU.subtract))
            for f in seq:
                P.dve(f, reads=["rt"], writes=["rt"])
            P.act(lambda e: e.activation(out=E_, in_=t1, func=AF.Exp), reads=["rt"], writes=["rt"])
            seq = []
            seq.append(lambda e: e.tensor_reduce(out=sm, in_=E_, axis=AX.X, op=ALU.add))
            seq.append(lambda e: e.reciprocal(out=sm, in_=sm))
            seq.append(lambda e: e.tensor_tensor(out=pr, in0=E_, in1=bc(sm, 16), op=ALU.mult))
            seq.append(lambda e: e.tensor_tensor(out=sel, in0=pr, in1=rbb[:], op=ALU.add))
            s4 = sel.rearrange("p a (g k) -> p a g k", k=4)
            pairs = [(0, 1), (0, 2), (0, 3), (1, 2), (1, 3), (2, 3)]
            for q, (a, b) in enumerate(pairs):
                seq.append(lambda e, q=q, a=a, b=b: e.tensor_tensor(out=p6[q], in0=s4[:, :, :, a], in1=s4[:, :, :, b], op=ALU.add))
            seq.append(lambda e: e.tensor_tensor(out=g4, in0=p6[0], in1=p6[1], op=ALU.max))
            for q in range(2, 6):
                seq.append(lambda e, q=q: e.tensor_tensor(out=g4, in0=g4, in1=p6[q], op=ALU.max))
            seq.append(lambda e: e.tensor_reduce(out=gmx, in_=g4, axis=AX.X, op=ALU.max))
            seq.append(lambda e: e.tensor_tensor(out=gm, in0=g4, in1=bc(gmx, 4), op=ALU.is_ge))
            gm16 = gm.unsqueeze(3).to_broadcast([128, 16, 4, 4])
            sm4 = selm.rearrange("p a (g k) -> p a g k", k=4)
            seq.append(lambda e: e.scalar_tensor_tensor(out=sm4, in0=s4, scalar=100.0, in1=gm16, op0=ALU.add, op1=ALU.mult))
            seq.append(lambda e: e.tensor_scalar(out=selm, in0=selm, scalar1=-100.0, scalar2=None, op0=ALU.add))
            seq.append(lambda e: e.tensor_reduce(out=m1, in_=selm, axis=AX.X, op=ALU.max))
            seq.append(lambda e: e.tensor_tensor(out=t2, in0=selm, in1=bc(m1, 16), op=ALU.is_ge))
            seq.append(lambda e: e.scalar_tensor_tensor(out=t3, in0=t2, scalar=-1000.0, in1=selm, op0=ALU.mult, op1=ALU.add))
            seq.append(lambda e: e.tensor_reduce(out=m2, in_=t3, axis=AX.X, op=ALU.max))
            seq.append(lambda e: e.tensor_tensor(out=t2, in0=selm, in1=bc(m2, 16), op=ALU.is_ge))
            seq.append(lambda e: e.tensor_tensor(out=t3, in0=t2, in1=pr, op=ALU.mult))
            seq.append(lambda e: e.tensor_reduce(out=sm, in_=t3, axis=AX.X, op=ALU.add))
            seq.append(lambda e: e.reciprocal(out=sm, in_=sm))
            seq.append(lambda e: e.tensor_tensor(out=gates[:], in0=t3, in1=bc(sm, 16), op=ALU.mult))
            for f in seq:
                P.dve(f, reads=["rt", "rbb"], writes=["rt", "gates"])

        def dump_and_finish():
            for i in range(16):
                P.dma("sp", "dbg", lambda e, i=i: e.dma_start(out=dbg_x[i * 128:(i + 1) * 128, :], in_=x[:, i, :]),
                      reads=[("x", i)])
            P.dma("sp", "dbg", lambda e: e.dma_start(out=dbg_hT, in_=hT[:]), reads=[("hT", i) for i in range(16)])
            P.dma("sp", "dbg", lambda e: e.dma_start(out=dbg_g, in_=gates[:].rearrange("p a b -> p (a b)")), reads=["gates"])
            P.emit(final_wait_streams=["dbg"])

        def layer(l):
            ar2.reset()
            vball = ar2.bf16([128, 16, 512])
            gub = ar2.bf16([128, 16, 512])
            w_uva = ar2.bf16([128, 8, 1024])
            w_oa = ar2.bf16([128, 4, 1024])
            WT = ar2.bf16([128, 8, 128])
            oaT = [ar2.bf16([128, 4, 128]) for _ in range(2)]
            g1b = ar2.f32([128, 1024])
            Wld = ar2.f32([128, 8, 128])
            lngb = ar2.f32([128, 512]); lnbb = ar2.f32([128, 512])
            gv = [ar2.f32([128, 512]) for _ in range(2)]
            vn = [ar2.f32([128, 512]) for _ in range(2)]
            oa = [ar2.f32([128, 512]) for _ in range(2)]
            junkA = ar2.bf16([128, 512])
            bsT = ar2.f32([8, 128])
            gabf = ar2.f32([128, 8])
            st6 = ar2.f32([128, 2, 6]); mv = ar2.f32([128, 2, 2]); vpe = ar2.f32([128, 2]); rsv = ar2.f32([128, 2])
            ssqa = ar2.f32([128, 16]); rsa = ar2.f32([128, 16])
            assert vball is not None

            def A_pre():
                for h2 in range(2):
                    P.dma("pool", "wA", lambda e, h2=h2: e.dma_start(
                        out=w_uva[:, :, h2 * 512:(h2 + 1) * 512],
                        in_=win_d[l, :, h2 * 512:(h2 + 1) * 512].rearrange("(k p) n -> p k n", p=128)), writes=[("w_uva", h2)])
                P.dma("pool", "wA", lambda e: e.dma_start(out=w_oa, in_=wout_d[l, 0:512, :].rearrange("(k p) n -> p k n", p=128)),
                      writes=["w_oa"])
                P.dma("sp", "a0", lambda e: e.dma_start(out=g1b, in_=modscr[l, 2048:3072].partition_broadcast(128)), writes=["g1b"])
                P.dma("sp", "a0", lambda e: e.dma_start(out=gabf, in_=gab_d[l]), writes=["gabf"])
                P.dma("sp", "a0", lambda e: e.dma_start(out=Wld, in_=ws_d[l].rearrange("g t s -> t g s")), writes=["Wld"])
                P.dma("sp", "a0", lambda e: e.dma_start(out=bsT, in_=bs_d[l]), writes=["bsT"])
                P.dma("sp", "a0", lambda e: e.dma_start(out=lngb, in_=lng_d[l, :].partition_broadcast(128)), writes=["lngb"])
                P.dma("sp", "a0", lambda e: e.dma_start(out=lnbb, in_=lnb_d[l, :].partition_broadcast(128)), writes=["lnbb"])

            def A_post():
                for c in range(4):
                    P.dve(lambda e, c=c: e.scalar_tensor_tensor(out=w_oa[:, c, :], in0=w_oa[:, c, :], scalar=gabf[:, c:c + 1], in1=g1b,
                                                                 op0=ALU.mult, op1=ALU.mult),
                          reads=["w_oa", "gabf", "g1b"], writes=["w_oa"])
                for g in range(8):
                    P.pe(lambda e, g=g: e.transpose(out=PS[:, 3072 + g * 128:3072 + (g + 1) * 128], in_=Wld[:, g, :], identity=identf[:]),
                         reads=["Wld", "identf"], writes=[("ps", 6 + g // 4)])
                for g in range(8):
                    P.dve(lambda e, g=g: e.tensor_tensor(out=WT[:, g, :], in0=PS[:, 3072 + g * 128:3072 + (g + 1) * 128], in1=trilm[:], op=ALU.mult),
                          reads=[("ps", 6 + g // 4), "trilm"], writes=["WT"])


            norm_phase(l, 1, pre=A_pre, have_ssq=(l > 0))
            assert ar.off <= 8 * 1024, ar.off
            A_post()
            if stop == ("N1", l):
                P.barrier(); dump_and_finish(); return True

            P.barrier()
            def A1(i):
                pp = i % 2
                for k in range(8):
                    P.pe(lambda e, k=k: e.matmul(bank(pp), lhsT=hT[:, k, i * 128:(i + 1) * 128], rhs=w_uva[:, k, 0:512],
                                                 start=(k == 0), stop=(k == 7)),
                         reads=[("hT", i), ("w_uva", 0)], writes=[("ps", pp)])
                for k in range(8):
                    P.pe(lambda e, k=k: e.matmul(bank(2 + pp), lhsT=hT[:, k, i * 128:(i + 1) * 128], rhs=w_uva[:, k, 512:1024],
                                                 start=(k == 0), stop=(k == 7)),
                         reads=[("hT", i), ("w_uva", 1)], writes=[("ps", 2 + pp)])
                P.act(lambda e: e.activation(out=gub[:, i, :], in_=bank(pp), func=AF.Gelu), reads=[("ps", pp)], writes=[("gub", i)])
                P.act(lambda e: e.activation(out=gv[pp], in_=bank(2 + pp), func=AF.Gelu), reads=[("ps", 2 + pp)], writes=[("gv", pp)])
                P.dve(lambda e: e.bn_stats(out=st6[:, pp, :], in_=gv[pp]), reads=[("gv", pp)], writes=[("st6", pp)])
                P.dve(lambda e: e.bn_aggr(out=mv[:, pp, :], in_=st6[:, pp, :]), reads=[("st6", pp)], writes=[("mv", pp)])
                P.dve(lambda e: e.tensor_scalar(out=vpe[:, pp:pp + 1], in0=mv[:, pp, 1:2], scalar1=EPS, scalar2=None, op0=ALU.add),
                      reads=[("mv", pp)], writes=[("vpe", pp)])
                P.pool(lambda e: e.tensor_tensor(out=rsv[:, pp:pp + 1], in0=vpe[:, pp:pp + 1], in1=nhalf[:, 0:1], op=ALU.pow),
                       reads=[("vpe", pp), "nhalf"], writes=[("rsv", pp)])
                P.dve(lambda e: e.tensor_scalar(out=vn[pp], in0=gv[pp], scalar1=mv[:, pp, 0:1], scalar2=rsv[:, pp:pp + 1],
                                                op0=ALU.subtract, op1=ALU.mult),
                      reads=[("gv", pp), ("mv", pp), ("rsv", pp)], writes=[("vn", pp)])
                P.dve(lambda e: e.tensor_tensor(out=vn[pp], in0=vn[pp], in1=lngb, op=ALU.mult),
                      reads=[("vn", pp), "lngb"], writes=[("vn", pp)])
                P.dve(lambda e: e.tensor_tensor(out=vball[:, i, :], in0=vn[pp], in1=lnbb, op=ALU.add),
                      reads=[("vn", pp), "lnbb"], writes=[("vball", i)])

            def A2(i):
                pp = i % 2
                P.pe(lambda e: e.matmul(bank(pp), lhsT=bsT[:, :], rhs=ind[:, :], start=True, stop=False),
                     reads=["bsT", "ind"], writes=[("ps", pp)])
                for g in range(8):
                    P.pe(lambda e, g=g: e.matmul(PS[:, pp * 512 + g * 64:pp * 512 + (g + 1) * 64], lhsT=WT[:, g, :],
                                                 rhs=vball[:, i, g * 64:(g + 1) * 64], start=False, stop=(g == 7)),
                         reads=["WT", ("vball", i)], writes=[("ps", pp)])
                P.dve(lambda e: e.tensor_tensor(out=oa[pp], in0=bank(pp), in1=gub[:, i, :], op=ALU.mult),
                      reads=[("ps", pp), ("gub", i)], writes=[("oa", pp)])
                P.act(lambda e: e.activation(out=junkA, in_=oa[pp], func=AF.Square, accum_out=ssqa[:, i:i + 1]),
                      reads=[("oa", pp)], writes=["junkA", ("ssqa", i)])
                P.dve(lambda e: e.tensor_scalar(out=rsa[:, i:i + 1], in0=ssqa[:, i:i + 1], scalar1=1.0 / 512, scalar2=EPS,
                                                op0=ALU.mult, op1=ALU.add),
                      reads=[("ssqa", i)], writes=[("rsa0", i)])
                P.pool(lambda e: e.tensor_tensor(out=rsa[:, i:i + 1], in0=rsa[:, i:i + 1], in1=nhalf[:, 0:1], op=ALU.pow),
                       reads=[("rsa0", i), "nhalf"], writes=[("rsa", i)])

            def A2b(i):
                pp = i % 2
                for c in range(4):
                    P.pe(lambda e, c=c: e.transpose(out=PS[:, (2 + pp) * 512 + c * 128:(2 + pp) * 512 + (c + 1) * 128],
                                                    in_=oa[pp][:, c * 128:(c + 1) * 128], identity=identf[:]),
                         reads=[("oa", pp), "identf"], writes=[("ps", 2 + pp)])
                P.act(lambda e: e.activation(out=oaT[pp], in_=bank(2 + pp).rearrange("p (c t) -> p c t", c=4), func=AF.Copy),
                      reads=[("ps", 2 + pp)], writes=[("oaT", pp)])

            def A3(i):
                pp = i % 2
                for hf in range(2):
                    bk = 4 + 2 * pp + hf
                    for c in range(4):
                        P.pe(lambda e, c=c, hf=hf, bk=bk: e.matmul(bank(bk), lhsT=oaT[pp][:, c, :], rhs=w_oa[:, c, hf * 512:(hf + 1) * 512],
                                                                   start=(c == 0), stop=(c == 3)),
                             reads=[("oaT", pp), "w_oa"], writes=[("ps", bk)])
                    P.dve(lambda e, hf=hf, bk=bk: e.scalar_tensor_tensor(out=x[:, i, hf * 512:(hf + 1) * 512], in0=bank(bk),
                                                                         scalar=rsa[:, i:i + 1], in1=x[:, i, hf * 512:(hf + 1) * 512],
                                                                         op0=ALU.mult, op1=ALU.add),
                          reads=[("ps", bk), ("rsa", i), ("x", i)], writes=[("x", i)])

            for i in range(16):
                A1(i)
            for s in range(18):
                if s < 16:
                    A2(s)
                if 0 <= s - 1 < 16:
                    A2b(s - 1)
                if 0 <= s - 2 < 16:
                    A3(s - 2)
            if stop == ("A", l):
                P.barrier(); dump_and_finish(); return True

            P.barrier()
            ar.reset()
            ost = [ar.bf16([128, 1024]) for _ in range(2)]
            wq = ar.bf16([128, 8, 128]); wk = ar.bf16([128, 8, 128]); wv = ar.bf16([128, 8, 128])
            QTd = [[ar.bf16([128, 2048]) for _ in range(3)] for _ in range(2)]
            KTd = [ar.bf16([128, 2048]) for _ in range(3)]
            VT = ar.bf16([128, 2048])
            Vb = [ar.bf16([128, 16, 2, 65]) for _ in range(3)]
            Pt = [ar.bf16([128, 512]) for _ in range(NSB)]
            obtok = ar.bf16([128, 16, 128])
            Tt = [[ar.f32([128, 512]) for _ in range(3)] for _ in range(2)]
            E32 = [ar.f32([128, 512]) for _ in range(NSB)]
            accS = [ar.f32([65, 512]) for _ in range(2)]
            rden = ar.f32([128, 4])
            ssqb = ar.f32([128, 16, 4]); rsb = statA[:, 0:16]
            junkB = ar.bf16([128, 128])
            b_end = ar.off
            for d in range(3):
                P.dve(lambda e, d=d: e.memset(Vb[d][:, :, :, 64:65], 1.0), writes=[("Vb", d)])
            for di in range(3):
                P.dve(lambda e, di=di: e.memset(QTd[0][di][64:128, :], 0.0), writes=["qz0"])
                P.dve(lambda e, di=di: e.memset(QTd[1][di][0:64, :], 0.0), writes=["qz1"])
            def B_proj(hp):
                for nm, wt, col in (("wq", wq, 1024), ("wk", wk, 1536), ("wv", wv, 2048)):
                    P.dma("pool", "wB", lambda e, wt=wt, col=col: e.dma_start(
                        out=wt, in_=win_d[l, :, col + hp * 128:col + (hp + 1) * 128].rearrange("(k p) n -> p k n", p=128)),
                        writes=[nm])
                nbk = 0
                for tb in range(4):
                    for nm, wt in (("wq", wq), ("wk", wk), ("wv", wv)):
                        bk = 6 + (nbk % 2)
                        nbk += 1
                        for k in range(8):
                            P.pe(lambda e, k=k, wt=wt, bk=bk, tb=tb: e.matmul(bank(bk), lhsT=wt[:, k, :], rhs=hT[:, k, tb * 512:(tb + 1) * 512],
                                                                              start=(k == 0), stop=(k == 7)),
                                 reads=[nm], writes=[("ps", bk)])
                        if nm == "wk":
                            P.act(lambda e, bk=bk, tb=tb: e.activation(out=KTd[0][:, tb * 512:(tb + 1) * 512], in_=bank(bk), func=AF.Copy),
                                  reads=[("ps", bk)], writes=[("wkT", tb)])
                        elif nm == "wv":
                            P.dve(lambda e, bk=bk, tb=tb: e.tensor_copy(out=VT[:, tb * 512:(tb + 1) * 512], in_=bank(bk)),
                                  reads=[("ps", bk)], writes=[("VT", tb)])
                        else:
                            for hh in range(2):
                                P.act(lambda e, bk=bk, tb=tb, hh=hh: e.activation(
                                    out=QTd[hh][0][64 * hh:64 * hh + 64, tb * 512:(tb + 1) * 512],
                                    in_=PS[64 * hh:64 * hh + 64, bk * 512:(bk + 1) * 512], func=AF.Copy, scale=0.125),
                                    reads=[("ps", bk)], writes=[("wqT", tb)])
                def deint(t, d):
                    return t.rearrange("p (r i) -> p r i", r=d), None
                for di in (1, 2):
                    d = CONFIGS[di][1]
                    P.pool(lambda e, di=di, d=d: e.tensor_copy(out=KTd[di].rearrange("p (r i) -> p r i", r=d),
                                                               in_=KTd[0].rearrange("p (i r) -> p r i", r=d)),
                           reads=[("wkT", j) for j in range(4)], writes=[("wkTd", di)])
                    P.pool(lambda e, di=di, d=d: e.tensor_copy(out=QTd[0][di][0:64, :].rearrange("p (r i) -> p r i", r=d),
                                                               in_=QTd[0][0][0:64, :].rearrange("p (i r) -> p r i", r=d)),
                           reads=[("wqT", j) for j in range(4)], writes=[("wqTd", 0, di)])
                for di in (1, 2):
                    d = CONFIGS[di][1]
                    P.pool(lambda e, di=di, d=d: e.tensor_copy(out=QTd[1][di][64:128, :].rearrange("p (r i) -> p r i", r=d),
                                                               in_=QTd[1][0][64:128, :].rearrange("p (i r) -> p r i", r=d)),
                           reads=[("wqT", j) for j in range(4)], writes=[("wqTd", 1, di)])
                for di, (win, d) in enumerate(CONFIGS):
                    nb = 16 // d
                    for q8 in range(2):
                        bk = 6 + (nbk % 2)
                        nbk += 1
                        pbf = bank(bk).bitcast(BF16)
                        for t8 in range(8):
                            tile = q8 * 8 + t8
                            r, m = tile // nb, tile % nb
                            t0 = r + d * 128 * m
                            P.pe(lambda e, t0=t0, d=d, pbf=pbf, t8=t8: e.transpose(
                                out=pbf[:, t8 * 128:(t8 + 1) * 128], in_=VT[:, t0:t0 + d * 127 + 1:d], identity=identb[:]),
                                reads=[("VT", j) for j in range(4)] + ["identb"], writes=[("ps", bk)])
                        P.act(lambda e, di=di, q8=q8, pbf=pbf: e.activation(
                            out=Vb[di][:, q8 * 8:(q8 + 1) * 8, :, 0:64],
                            in_=pbf.rearrange("p (a h e) -> p a h e", a=8, h=2), func=AF.Copy),
                            reads=[("ps", bk)], writes=[("Vb", di)])

            def B_head(hp, hl, base):
                h = 2 * hp + hl
                hpar = h % 2
                p0 = 64 * hl
                for di in range(3):
                    P.dma("sp", "tt%d" % hpar, lambda e, di=di: e.dma_start(
                        out=Tt[hpar][di].rearrange("p (a b) -> p a b", a=2),
                        in_=bass.AP(texp.tensor, (di * 8 + h) * 128 * 256, [[256, 128], [0, 2], [1, 256]])),
                        writes=[("Tt", hpar, di)])
                started = [False] * 4
                steps = []
                for di, (win, d) in enumerate(CONFIGS):
                    nb = 16 // d
                    if nb >= 2:
                        for r in range(d):
                            for m in range(0, nb, 2):
                                steps.append((di, d, nb, [(r, m, 256, 0), (r, m + 1, 256 if m + 2 < nb else 128, 256)]))
                    else:
                        for r in range(0, d, 2):
                            steps.append((di, d, nb, [(r, 0, 128, 0), (r + 1, 0, 128, 256)]))

                def region(ap512, subs):
                    if subs[0][2] == 256:
                        return ap512[:, 0:256 + subs[1][2]]
                    return ap512.rearrange("p (a b) -> p a b", a=2)[:, :, 0:128]

                def S_step(st, sidx):
                    di, d, nb, subs = st
                    sb_ = sidx % NSB
                    bk = 4 + sb_
                    for (r, m, width, coff) in subs:
                        bs0 = r * (2048 // d) + 128 * m
                        P.pe(lambda e, bs0=bs0, width=width, coff=coff: e.matmul(
                            PS[:, bk * 512 + coff:bk * 512 + coff + width], lhsT=KTd[di][:, bs0:bs0 + 128],
                            rhs=QTd[hl][di][:, bs0:bs0 + width], start=True, stop=True),
                            reads=[("wkT", j) for j in range(4)] + [("wqT", j) for j in range(4)] +
                            ([("wkTd", di), ("wqTd", hl, di)] if di > 0 else []), writes=[("ps", bk)])
                    P.dve(lambda e: e.tensor_tensor(out=region(E32[sb_], subs), in0=region(bank(bk), subs),
                                                    in1=region(Tt[hpar][di], subs), op=ALU.add),
                          reads=[("ps", bk), ("Tt", hpar, di)], writes=[("E32", sb_)])
                    P.act(lambda e: e.activation(out=region(Pt[sb_], subs), in_=region(E32[sb_], subs), func=AF.Exp),
                          reads=[("E32", sb_)], writes=[("Pt", sb_)])

                def PV_step(st, sidx):
                    di, d, nb, subs = st
                    sb_ = sidx % NSB
                    for (r, m, width, coff) in subs:
                        tile = r * nb + m
                        lhs = Vb[di][:, tile, hl, :]
                        for qb in ((m, m + 1) if m + 1 < nb else (m,)):
                            c0 = coff + (qb - m) * 128
                            if d < 16:
                                if d == 1:
                                    bk, cs, step = qb // 4, (qb % 4) * 128, 1
                                else:
                                    bk, cs, step = qb, r, 4
                                pieces = [(bk, cs, step, 128, c0)]
                            else:
                                pieces = [(b4, r, 16, 32, c0 + 32 * b4) for b4 in range(4)]
                            for (bk, cs, step, cnt, pc) in pieces:
                                st_flag = not started[bk]
                                started[bk] = True
                                P.pe(lambda e, bk=bk, cs=cs, step=step, cnt=cnt, pc=pc, st_flag=st_flag, lhs=lhs: e.matmul(
                                    PS[0:65, bk * 512 + cs:bk * 512 + cs + step * (cnt - 1) + 1:step], lhsT=lhs,
                                    rhs=Pt[sb_][:, pc:pc + cnt], start=st_flag, stop=False, skip_group_check=True),
                                    reads=[("Vb", di), ("Pt", sb_)], writes=[("ps", bk)])

                SK = NSB - 1
                for j in range(len(steps) + SK):
                    if j < len(steps):
                        S_step(steps[j], base + j)
                    if j - SK >= 0:
                        PV_step(steps[j - SK], base + j - SK)
                if stop == ("Bs", l):
                    return len(steps)
                for b4 in range(4):
                    ab = accS[b4 % 2]
                    P.act(lambda e, b4=b4, ab=ab: e.activation(out=ab, in_=bank(b4, 512, 65), func=AF.Copy),
                          reads=[("ps", b4)], writes=[("accS", b4 % 2)])
                    bk = 6 + (b4 % 2)
                    for j in range(4):
                        P.pe(lambda e, j=j, ab=ab, bk=bk: e.transpose(out=PS[:, bk * 512 + j * 65:bk * 512 + (j + 1) * 65],
                                                                      in_=ab[:, j * 128:(j + 1) * 128], identity=identf[0:65, 0:65]),
                             reads=[("accS", b4 % 2), "identf"], writes=[("ps", bk)])
                    pv = bank(bk, 260).rearrange("p (j e) -> p j e", j=4)
                    P.dve(lambda e, pv=pv: e.reciprocal(out=rden.unsqueeze(2), in_=pv[:, :, 64:65]),
                          reads=[("ps", bk)], writes=["rden"])
                    P.dve(lambda e, pv=pv, b4=b4: e.tensor_tensor(out=obtok[:, b4 * 4:(b4 + 1) * 4, p0:p0 + 64], in0=pv[:, :, 0:64],
                                                                  in1=rden.unsqueeze(2).to_broadcast([128, 4, 64]), op=ALU.mult),
                          reads=[("ps", bk), "rden"], writes=[("obtok", hl)])
                return len(steps)

            def B_pair_end(hp):
                for i in range(16):
                    P.act(lambda e, i=i: e.activation(out=junkB, in_=obtok[:, i, :], func=AF.Square, accum_out=ssqb[:, i, hp:hp + 1]),
                          reads=[("obtok", 0), ("obtok", 1)], writes=["junkB", ("ssqb", hp)])
                for i8 in range(2):
                    bk = 6 + i8
                    pbf = bank(bk).bitcast(BF16)
                    for j in range(8):
                        i = i8 * 8 + j
                        P.pe(lambda e, i=i, j=j, pbf=pbf: e.transpose(out=pbf[:, j * 128:(j + 1) * 128], in_=obtok[:, i, :], identity=identb[:]),
                             reads=[("obtok", 0), ("obtok", 1), "identb"], writes=[("ps", bk)])
                    P.act(lambda e, i8=i8, pbf=pbf: e.activation(out=ost[i8], in_=pbf, func=AF.Copy),
                          reads=[("ps", bk)], writes=[("ost", i8)])
                    P.dma("sp", "ob", lambda e, i8=i8: e.dma_start(out=obT_d[:, hp, i8 * 1024:(i8 + 1) * 1024], in_=ost[i8]),
                          reads=[("ost", i8)], writes=["obT_d"])

            sidx = 0
            for hp_ in range(4):
                B_proj(hp_)
                if stop == ("Bp", l):
                    P.barrier(); dump_and_finish(); return True
                for hl_ in range(2):
                    sidx += B_head(hp_, hl_, sidx)
                    if stop in (("Bs", l), ("Bh", l)):
                        P.barrier(); dump_and_finish(); return True
                B_pair_end(hp_)
                if stop == ("Be", l):
                    P.barrier(); dump_and_finish(); return True
            P.dve(lambda e: e.tensor_reduce(out=rsb, in_=ssqb, axis=AX.X, op=ALU.add), reads=[("ssqb", j) for j in range(4)], writes=["rsb0"])
            P.dve(lambda e: e.tensor_scalar(out=rsb, in0=rsb, scalar1=1.0 / 512, scalar2=EPS, op0=ALU.mult, op1=ALU.add),
                  reads=["rsb0"], writes=["rsb1"])
            P.pool(lambda e: e.tensor_tensor(out=rsb, in0=rsb, in1=nhalf[:, 0:1].to_broadcast([128, 16]), op=ALU.pow),
                   reads=["rsb1", "nhalf"], writes=["rsb"])
            if stop == ("B1", l):
                P.barrier()
                P.dma("sp", "dbg", lambda e: e.dma_start(out=dbg_obT, in_=obT_d))
                P.barrier(); dump_and_finish(); return True
            P.barrier()
            ar.reset()
            obT = ar.bf16([128, 4, 2048])
            P.dma("sp", "a0", lambda e: e.dma_start(out=obT, in_=obT_d), writes=["obT"])
            w_ob = ar.bf16([128, 4, 1024])
            junkb = ar.bf16([128, 1024])
            g1b2 = ar.f32([128, 1024])
            gabf2 = ar.f32([128, 8])
            P.dma("pool", "wA", lambda e: e.dma_start(out=w_ob, in_=wout_d[l, 512:1024, :].rearrange("(k p) n -> p k n", p=128)),
                  writes=["w_ob"])
            P.dma("sp", "a0", lambda e: e.dma_start(out=g1b2, in_=modscr[l, 2048:3072].partition_broadcast(128)), writes=["g1b2"])
            P.dma("sp", "a0", lambda e: e.dma_start(out=gabf2, in_=gab_d[l]), writes=["gabf2"])
            for c in range(4):
                P.dve(lambda e, c=c: e.scalar_tensor_tensor(out=w_ob[:, c, :], in0=w_ob[:, c, :], scalar=gabf2[:, 4 + c:5 + c], in1=g1b2,
                                                             op0=ALU.mult, op1=ALU.mult),
                      reads=["w_ob", "gabf2", "g1b2"], writes=["w_ob"])
            for i in range(16):
                for hf in range(2):
                    bk = 2 * (i % 2) + hf
                    for c in range(4):
                        P.pe(lambda e, c=c, hf=hf, bk=bk, i=i: e.matmul(bank(bk), lhsT=obT[:, c, i * 128:(i + 1) * 128],
                                                                        rhs=w_ob[:, c, hf * 512:(hf + 1) * 512], start=(c == 0), stop=(c == 3)),
                             reads=["w_ob", "obT"], writes=[("ps", bk)])
                    P.dve(lambda e, hf=hf, bk=bk, i=i: e.scalar_tensor_tensor(out=x[:, i, hf * 512:(hf + 1) * 512], in0=bank(bk),
                                                                              scalar=rsb[:, i:i + 1], in1=x[:, i, hf * 512:(hf + 1) * 512],
                                                                              op0=ALU.mult, op1=ALU.add),
                          reads=[("ps", bk), ("x", i)], writes=[("x", i)])
                P.act(lambda e, i=i: e.activation(out=junkb, in_=x[:, i, :], func=AF.Square, accum_out=ssqN[:, i:i + 1]),
                      reads=[("x", i)], writes=["junkb", ("ssq", i)])
            if stop == ("B", l):
                P.barrier(); dump_and_finish(); return True

            ar2.reset()
            sg = [ar2.bf16([128, 512]) for _ in range(2)]
            actT = [ar2.bf16([128, 4, 512]) for _ in range(2)]
            junkM = ar2.bf16([128, 1024])
            if l == 0:
                stageM = [ar2.bf16([128, 8, 512]) for _ in range(2)]
                modbrM = [ar2.f32([1, 512]) for _ in range(2)]
                mrowM = [ar2.f32([1, 512]) for _ in range(2)]
            assert ar2.off <= 11 * 1024, ar2.off
            ar2.off = 11 * 1024
            wgb = [ar2.bf16([128, 8, 512]) for _ in range(2)]
            wub = [ar2.bf16([128, 8, 512]) for _ in range(2)]
            wdb = [ar2.bf16([128, 4, 1024]) for _ in range(2)]
            g2b = ar2.f32([128, 1024])

            def load_expert(ex):
                pb = ex % 2
                for kk in range(2):
                    P.dma("pool", "wg%d" % pb, lambda e, kk=kk: e.dma_start(
                        out=wgb[pb][:, kk * 4:(kk + 1) * 4, :],
                        in_=wg_d[l, ex, kk * 512:(kk + 1) * 512, :].rearrange("(k p) n -> p k n", p=128)), writes=[("wg", pb)])
                    P.dma("pool", "wu%d" % pb, lambda e, kk=kk: e.dma_start(
                        out=wub[pb][:, kk * 4:(kk + 1) * 4, :],
                        in_=wu_d[l, ex, kk * 512:(kk + 1) * 512, :].rearrange("(k p) n -> p k n", p=128)), writes=[("wu", pb)])
                    P.dma("pool", "wd%d" % pb, lambda e, kk=kk: e.dma_start(
                        out=wdb[pb][:, kk * 2:(kk + 1) * 2, :],
                        in_=wd_d[l, ex, kk * 256:(kk + 1) * 256, :].rearrange("(k p) n -> p k n", p=128)), writes=[("wd", pb)])
                for c in range(4):
                    P.pool(lambda e, c=c: e.tensor_tensor(out=wdb[pb][:, c, :], in0=wdb[pb][:, c, :], in1=g2b, op=ALU.mult),
                           reads=[("wd", pb), "g2b"], writes=[("wd", pb)])

            def M_pre():
                P.dma("sp", "m0", lambda e: e.dma_start(out=g2b, in_=modscr[l, 5120:6144].partition_broadcast(128)), writes=["g2b"])
                load_expert(0)
                load_expert(1)

            norm_phase(l, 2, pre=M_pre, have_ssq=True)
            if stop == ("N2a", l):
                P.barrier(); dump_and_finish(); return True
            P.barrier()

            def GU(ex, tb, n):
                pb = ex % 2
                ab = n % 2
                for fc in range(4):
                    q = fc % 2
                    for k in range(8):
                        P.pe(lambda e, k=k, fc=fc, q=q: e.matmul(bank(q), lhsT=wgb[pb][:, k, fc * 128:(fc + 1) * 128],
                                                                 rhs=hT[:, k, tb * 512:(tb + 1) * 512], start=(k == 0), stop=(k == 7)),
                             reads=[("wg", pb)], writes=[("ps", q)])
                    for k in range(8):
                        P.pe(lambda e, k=k, fc=fc, q=q: e.matmul(bank(2 + q), lhsT=wub[pb][:, k, fc * 128:(fc + 1) * 128],
                                                                 rhs=hT[:, k, tb * 512:(tb + 1) * 512], start=(k == 0), stop=(k == 7)),
                             reads=[("wu", pb)], writes=[("ps", 2 + q)])
                    P.act(lambda e, q=q: e.activation(out=sg[q], in_=bank(q), func=AF.Silu), reads=[("ps", q)], writes=[("sg", q)])
                    P.dve(lambda e, q=q, fc=fc: e.tensor_tensor(out=actT[ab][:, fc, :], in0=bank(2 + q), in1=sg[q], op=ALU.mult),
                          reads=[("ps", 2 + q), ("sg", q)], writes=[("actT", ab)])

            def DN(ex, tb, n):
                pb = ex % 2
                ab = n % 2
                for tt in range(4):
                    i = tb * 4 + tt
                    for hf in range(2):
                        bk = 4 + ((tt * 2 + hf) % 4)
                        for fc in range(4):
                            P.pe(lambda e, fc=fc, hf=hf, tt=tt, bk=bk: e.matmul(bank(bk), lhsT=actT[ab][:, fc, tt * 128:(tt + 1) * 128],
                                                                                rhs=wdb[pb][:, fc, hf * 512:(hf + 1) * 512],
                                                                                start=(fc == 0), stop=(fc == 3)),
                                 reads=[("actT", ab), ("wd", pb)], writes=[("ps", bk)])
                        P.dve(lambda e, hf=hf, i=i, bk=bk: e.scalar_tensor_tensor(out=x[:, i, hf * 512:(hf + 1) * 512], in0=bank(bk),
                                                                                  scalar=gates[:, i, ex:ex + 1], in1=x[:, i, hf * 512:(hf + 1) * 512],
                                                                                  op0=ALU.mult, op1=ALU.add),
                              reads=[("ps", bk), "gates", ("x", i)], writes=[("x", i)])
                    if ex == 15:
                        P.act(lambda e, i=i: e.activation(out=junkM, in_=x[:, i, :], func=AF.Square, accum_out=ssqN[:, i:i + 1]),
                              reads=[("x", i)], writes=["junkM", ("ssq", i)])

            seqs = [(ex, tb) for ex in range(16) for tb in range(4)]
            for n in range(len(seqs) + 1):
                if n < len(seqs):
                    GU(seqs[n][0], seqs[n][1], n)
                if n == 0:
                    router_phase()
                    assert ar.off <= 11 * 1024, ar.off
                    if stop == ("N2", l):
                        P.barrier(); dump_and_finish(); return True
                if n >= 1:
                    ex, tb = seqs[n - 1]
                    DN(ex, tb, n - 1)
                    if tb == 3 and ex + 2 < 16:
                        load_expert(ex + 2)
                    if tb == 3 and l == 0:
                        if 1 <= ex <= 12:
                            j = ex - 1
                            mod_chunk(1, j, j % 2, stageM[j % 2], modbrM[j % 2], mrowM[j % 2], 7, part=2)
                        if ex < 12:
                            mod_chunk(1, ex, ex % 2, stageM[ex % 2], modbrM[ex % 2], mrowM[ex % 2], 7, part=1,
                                      extra_w=["rt"] if ex < 2 else ())
            if stop == ("M", l):
                P.barrier(); dump_and_finish(); return True

            return False

        for l_ in range(n_layers):
            if layer(l_):
                return nc

        P.barrier()
        ar.reset()
        fgb = ar.f32([128, 1024])
        ob = [ar.f32([128, 1024]) for _ in range(3)]
        junk = ar.bf16([128, 1024])
        ssq = ssqN; rstd = ar.f32([128, 16])
        P.dma("sp", "n0", lambda e: e.dma_start(out=fgb, in_=fg_d[0, :].partition_broadcast(128)), writes=["fgb"])
        P.act(lambda e: e.activation(out=rstd, in_=ssq, func=AF.Sqrt, bias=eps_t[:, 0:1], scale=1.0 / 1024),
              reads=[("ssq", i) for i in range(16)] + ["eps"], writes=["rstd0"])
        P.dve(lambda e: e.reciprocal(out=rstd, in_=rstd), reads=["rstd0"], writes=["rstd"])
        for i in range(16):
            o = ob[i % 3]
            P.dve(lambda e, i=i, o=o: e.scalar_tensor_tensor(out=o, in0=x[:, i, :], scalar=rstd[:, i:i + 1], in1=fgb,
                                                             op0=ALU.mult, op1=ALU.mult),
                  reads=[("x", i), "rstd", "fgb"], writes=[("ob", i % 3)])
            P.dma("sp", "out", lambda e, i=i, o=o: e.dma_start(out=out_d[i * 128:(i + 1) * 128, :], in_=o),
                  reads=[("ob", i % 3)], writes=[("out", i)])
        if dbg:
            P.barrier(); dump_and_finish(); return nc
        P.emit(final_wait_streams=["out"])
    return nc


def t5_bucket_np(dist):
    dist = np.maximum(dist, 0)
    ratio = np.log(np.maximum(dist, 1) / 16) / np.log(2048 / 16)
    large = 16 + np.floor(ratio * 16).astype(np.int64)
    large = np.minimum(large, 31)
    return np.where(dist < 16, dist, large).astype(np.int32)


def host_consts():
    btab = np.zeros((3, 33, 384), np.float32)
    for di, (win, d) in enumerate(CONFIGS):
        for j in range(384):
            rel = j - 127
            if 0 <= rel <= 128:
                btab[di, int(t5_bucket_np(np.array(rel * d))), j] = 1.0
            else:
                btab[di, 32, j] = NEG
    ind = np.zeros((8, 512), np.float32)
    for g in range(8):
        ind[g, g * 64:(g + 1) * 64] = 1.0
    trilm = np.triu(np.ones((128, 128), np.float32))
    return dict(identf=np.eye(128, dtype=np.float32), btab=btab, ind=ind, trilm=trilm,
                jmat=np.ascontiguousarray(np.eye(128, dtype=np.float32)[::-1]))


def make_in_maps(inputs, cores):
    f = lambda a: np.ascontiguousarray(np.asarray(a, dtype=np.float32))
    shared = dict(
        rel_bias=f(inputs["rel_bias"]), router_w=f(inputs["router_w"]), router_b=f(inputs["router_b"]).reshape(1, 16),
        mod_w=f(inputs["mod_w"]), mod_b=f(inputs["mod_b"]), norm1_g=f(inputs["norm1_g"]), w_in=f(inputs["w_in"]),
        gmlp_ln_g=f(inputs["gmlp_ln_g"]), gmlp_ln_b=f(inputs["gmlp_ln_b"]), gmlp_ws=f(inputs["gmlp_ws"]),
        gmlp_bs=f(inputs["gmlp_bs"]), w_out=f(inputs["w_out"]), norm2_g=f(inputs["norm2_g"]),
        moe_w_gate=f(inputs["moe_w_gate"]), moe_w_up=f(inputs["moe_w_up"]), moe_w_down=f(inputs["moe_w_down"]),
        final_g=f(inputs["final_g"]).reshape(1, 1024),
    )
    gab = np.concatenate([f(inputs["out_norm_a_g"]), f(inputs["out_norm_b_g"])], axis=1)
    shared["gab"] = np.ascontiguousarray(gab.reshape(2, 8, 128).transpose(0, 2, 1))
    shared.update(host_consts())
    x = f(inputs["x"]); c = f(inputs["c"])
    maps = []
    for b in cores:
        m = dict(shared)
        m["x"] = np.ascontiguousarray(x[b])
        m["cT"] = np.ascontiguousarray(c[b].reshape(8, 128).T)
        maps.append(m)
    return maps


def kernel(**inputs):
    nc = build()
    maps = make_in_maps(inputs, list(range(8)))
    res = run_bass_kernel_spmd(nc, maps, core_ids=list(range(8)))
    return np.stack([np.asarray(r["out"], dtype=np.float32) for r in res.results], axis=0)
```

```python
import contextlib
import os
import numpy as np
import concourse.bass as bass
import concourse.mybir as mybir
from concourse.bass_utils import run_bass_kernel_spmd

F32 = mybir.dt.float32
BF16 = mybir.dt.bfloat16
ALU = mybir.AluOpType
AF = mybir.ActivationFunctionType
AX = mybir.AxisListType
ENGS = ("pe", "act", "dve", "pool", "sp")
EPS = 1e-6
NEG = -30000.0
CONFIGS = ((128, 1), (512, 4), (2048, 16))


class Prog:
    def __init__(self, nc):
        self.nc = nc
        self.ins = []
        self.last_w = {}
        self.readers = {}
        self.stream_cnt = {}
        self.stream_last = {}

    def add(self, eng, fn, reads=(), writes=(), dma=None):
        i = len(self.ins)
        deps = {}

        def dep(j):
            if j is None:
                return
            pj = self.ins[j]
            deps[j] = self.stream_cnt[pj["dma"]] if pj["dma"] is not None else None

        for r in reads:
            dep(self.last_w.get(r))
        for w in writes:
            dep(self.last_w.get(w))
            for j in self.readers.get(w, ()):
                dep(j)
        pruned = {}
        for j, c in deps.items():
            pj = self.ins[j]
            if pj["dma"] is None and pj["eng"] == eng:
                if eng == "pe":
                    continue
                if not any(self.last_w.get(r) == j for r in reads):
                    continue
            pruned[j] = c
            if pj["dma"] is None:
                pj["needs_inc"] = True
        rec = dict(eng=eng, fn=fn, deps=pruned, dma=dma, needs_inc=False, val=None)
        if dma is not None:
            self.stream_cnt[dma] = self.stream_cnt.get(dma, 0) + 1
            self.stream_last[dma] = i
        self.ins.append(rec)
        for r in reads:
            self.readers.setdefault(r, []).append(i)
        for w in writes:
            self.last_w[w] = i
            self.readers[w] = []
        return i

    def pe(self, fn, reads=(), writes=()):
        return self.add("pe", fn, reads, writes)

    def act(self, fn, reads=(), writes=()):
        return self.add("act", fn, reads, writes)

    def dve(self, fn, reads=(), writes=()):
        return self.add("dve", fn, reads, writes)

    def pool(self, fn, reads=(), writes=()):
        return self.add("pool", fn, reads, writes)

    def dma(self, q, stream, fn, reads=(), writes=()):
        return self.add(q, fn, reads, writes, dma=stream)

    def barrier(self):
        last = {}
        for idx, rec in enumerate(self.ins):
            if rec["dma"] is None and not rec.get("bar"):
                last[rec["eng"]] = idx
        for e in ENGS:
            deps = {}
            for e2, j in last.items():
                if e2 == e and e == "pe":
                    continue
                deps[j] = None
                self.ins[j]["needs_inc"] = True
            for s, c in self.stream_cnt.items():
                deps[self.stream_last[s]] = c
            self.ins.append(dict(eng=e, fn=None, deps=deps, dma=None, needs_inc=False, val=None, bar=True))
        self.last_w = {}
        self.readers = {}

    def emit(self, final_wait_streams=()):
        nc = self.nc
        streams = sorted(self.stream_cnt.keys())
        with contextlib.ExitStack() as es:
            esem = {e: es.enter_context(nc.semaphore("s_" + e)) for e in ENGS}
            ssem = {s: es.enter_context(nc.semaphore("d_" + str(s))) for s in streams}
            cnt = {e: 0 for e in ENGS}
            for rec in self.ins:
                if rec["dma"] is None and rec["needs_inc"]:
                    cnt[rec["eng"]] += 1
                    rec["val"] = cnt[rec["eng"]]
            per_eng = {e: [] for e in ENGS}
            for rec in self.ins:
                per_eng[rec["eng"]].append(rec)
            ins = self.ins
            block = es.enter_context(nc.Block())

            def run(ename, eng):
                waited = {}
                for rec in per_eng[ename]:
                    for j, c in rec["deps"].items():
                        pj = ins[j]
                        if pj["dma"] is not None:
                            sem, v, key = ssem[pj["dma"]], c * 16, ("d", pj["dma"])
                        else:
                            sem, v, key = esem[pj["eng"]], pj["val"], ("e", pj["eng"])
                        if waited.get(key, 0) >= v:
                            continue
                        waited[key] = v
                        eng.wait_ge(sem, v)
                    if rec["fn"] is None:
                        continue
                    bi = rec["fn"](eng)
                    if rec["dma"] is not None:
                        bi.then_inc(ssem[rec["dma"]], 16)
                    elif rec["needs_inc"]:
                        bi.then_inc(esem[ename], 1)
                if ename == "sp":
                    for s in final_wait_streams:
                        eng.wait_ge(ssem[s], self.stream_cnt[s] * 16)

            block.tensor(lambda e: run("pe", e))
            block.scalar(lambda e: run("act", e))
            block.vector(lambda e: run("dve", e))
            block.gpsimd(lambda e: run("pool", e))
            block.sync(lambda e: run("sp", e))


class Arena:
    def __init__(self, t, words):
        self.t = t
        self.words = words
        self.off = 0

    def reset(self):
        self.off = 0

    def _take(self, nwords):
        nwords = (nwords + 7) // 8 * 8
        a = self.off
        self.off += nwords
        assert self.off <= self.words, ("arena overflow", self.off, self.words)
        return a

    def f32(self, shape):
        n = int(np.prod(shape[1:]))
        a = self._take(n)
        ap = self.t[0:shape[0], a:a + n]
        return self._shape(ap, shape)

    def bf16(self, shape):
        n = int(np.prod(shape[1:]))
        a = self._take((n + 1) // 2)
        ap = self.t[0:shape[0], a:a + (n + 1) // 2].bitcast(BF16)[:, 0:n]
        return self._shape(ap, shape)

    @staticmethod
    def _shape(ap, shape):
        if len(shape) == 2:
            return ap
        if len(shape) == 3:
            return ap.rearrange("p (a b) -> p a b", a=shape[1])
        if len(shape) == 4:
            return ap.rearrange("p (a b c) -> p a b c", a=shape[1], b=shape[2])
        raise ValueError(shape)


NSB = 4
ARENA_WORDS = 25 * 1024


def build(stop=None, n_layers=2):
    nc = bass.Bass("TRN2", target_bir_lowering=False)
    din = lambda name, shape: nc.dram_tensor(name, shape, F32, kind="ExternalInput").ap()
    x_d = din("x", [2048, 1024])
    cT_d = din("cT", [128, 8])
    relb_d = din("rel_bias", [32, 8])
    rw_d = din("router_w", [1024, 16])
    rb_d = din("router_b", [1, 16])
    modw_d = din("mod_w", [2, 1024, 6144])
    modb_d = din("mod_b", [2, 6144])
    n1g_d = din("norm1_g", [2, 1024])
    win_d = din("w_in", [2, 1024, 2560])
    lng_d = din("gmlp_ln_g", [2, 512])
    lnb_d = din("gmlp_ln_b", [2, 512])
    ws_d = din("gmlp_ws", [2, 8, 128, 128])
    bs_d = din("gmlp_bs", [2, 8, 128])
    gab_d = din("gab", [2, 128, 8])
    wout_d = din("w_out", [2, 1024, 1024])
    n2g_d = din("norm2_g", [2, 1024])
    wg_d = din("moe_w_gate", [2, 16, 1024, 512])
    wu_d = din("moe_w_up", [2, 16, 1024, 512])
    wd_d = din("moe_w_down", [2, 16, 512, 1024])
    fg_d = din("final_g", [1, 1024])
    identf_d = din("identf", [128, 128])
    btab_d = din("btab", [3, 33, 384])
    ind_d = din("ind", [8, 512])
    tril_d = din("trilm", [128, 128])
    jmat_d = din("jmat", [128, 128])
    out_d = nc.dram_tensor("out", [2048, 1024], F32, kind="ExternalOutput").ap()
    modscr = nc.dram_tensor("modscr", [2, 6144], F32, kind="Internal").ap()
    gscr_h = nc.dram_tensor("gscr", [3, 8, 384], F32, kind="Internal")
    gscr = gscr_h.ap()
    texp = nc.dram_tensor("texp", [3, 8, 128, 256], F32, kind="Internal").ap()
    obT_d = nc.dram_tensor("obT_d", [128, 4, 2048], BF16, kind="Internal").ap()
    dbg = stop is not None
    if dbg:
        dbg_x = nc.dram_tensor("dbg_x", [2048, 1024], F32, kind="ExternalOutput").ap()
        dbg_hT = nc.dram_tensor("dbg_hT", [128, 8, 2048], BF16, kind="ExternalOutput").ap()
        dbg_g = nc.dram_tensor("dbg_g", [128, 256], F32, kind="ExternalOutput").ap()
        dbg_obT = nc.dram_tensor("dbg_obT", [128, 4, 2048], BF16, kind="ExternalOutput").ap()

    P = Prog(nc)
    es = contextlib.ExitStack()
    with es:
        sb = lambda name, shape, dt: es.enter_context(nc.sbuf_tensor(name, shape, dt))
        x = sb("x_sb", [128, 16, 1024], F32)
        hT = sb("hT", [128, 8, 2048], BF16)
        identf = sb("identf_sb", [128, 128], F32)
        identb = sb("identb_sb", [128, 128], BF16)
        trilm = sb("trilm_sb", [128, 128], F32)
        ind = sb("ind_sb", [8, 512], F32)
        ones_bf = sb("ones_bf", [1, 128], BF16)
        eps_t = sb("eps_t", [128, 1], F32)
        nhalf = sb("nhalf_t", [128, 1], F32)
        gates = sb("gates_sb", [128, 16, 16], F32)
        rbb = sb("rbb_sb", [128, 16, 16], F32)
        rw = sb("rw_sb", [128, 8, 16], F32)
        statA = sb("statA", [128, 64], F32)
        cact = sb("cact_sb", [128, 8], BF16)
        arena_t = sb("arena", [128, ARENA_WORDS], F32)
        PS = es.enter_context(nc.psum_tensor("ps", [128, 4096], F32))
        ar = Arena(arena_t, ARENA_WORDS)
        ar2 = Arena(arena_t, ARENA_WORDS)

        def bank(b, n=512, parts=128):
            return PS[0:parts, b * 512:b * 512 + n]

        P.dma("sp", "c0", lambda e: e.dma_start(out=identf[:], in_=identf_d), writes=["identf"])
        P.dma("sp", "c0", lambda e: e.dma_start(out=trilm[:], in_=tril_d), writes=["trilm"])
        P.dma("sp", "c0", lambda e: e.dma_start(out=ind[:], in_=ind_d), writes=["ind"])
        P.dma("sp", "c0", lambda e: e.dma_start(out=rw[:], in_=rw_d.rearrange("(k p) e -> p k e", p=128)), writes=["rw"])
        P.dma("sp", "c0", lambda e: e.dma_start(
            out=rbb[:], in_=bass.AP(rb_d.tensor, 0, [[0, 128], [0, 16], [1, 16]])), writes=["rbb"])
        P.dve(lambda e: e.tensor_copy(out=identb[:], in_=identf[:]), reads=["identf"], writes=["identb"])
        P.dve(lambda e: e.memset(ones_bf[:], 1.0), writes=["ones_bf"])
        P.dve(lambda e: e.memset(eps_t[:], EPS), writes=["eps"])
        P.dve(lambda e: e.memset(nhalf[:], -0.5), writes=["nhalf"])

        ar.reset()
        cT = ar.f32([128, 8])
        stageR = [ar.bf16([128, 3072]) for _ in range(3)]
        modbrR = ar.f32([1, 3072])
        mrowR = ar.f32([1, 3072])
        rb33 = ar.f32([33, 8])
        btab = ar.f32([33, 3, 384])
        grow = ar.f32([8, 3, 384])
        P.dma("sp", "c0", lambda e: e.dma_start(out=cT, in_=cT_d), writes=["cT"])
        P.act(lambda e: e.activation(out=cact[:], in_=cT, func=AF.Silu), reads=["cT"], writes=["cact"])
        def mod_chunk(l, j, pb, stage_, modbr_, mrow_, bk, part=3, extra_w=()):
            if part & 1:
                P.dma("pool", "mw%d" % pb, lambda e: e.dma_start(
                    out=stage_, in_=modw_d[l, :, j * 512:(j + 1) * 512].rearrange("(k p) n -> p k n", p=128)),
                    writes=[("stage", pb)] + list(extra_w))
                P.dma("sp", "mb%d" % pb, lambda e: e.dma_start(out=modbr_, in_=modb_d[l:l + 1, j * 512:(j + 1) * 512]),
                      writes=[("modbr", pb)] + list(extra_w))
            if not (part & 2):
                return
            for k in range(8):
                P.pe(lambda e, k=k: e.matmul(bank(bk, 512, 1), lhsT=cact[:, k:k + 1], rhs=stage_[:, k, :],
                                             start=(k == 0), stop=(k == 7)),
                     reads=["cact", ("stage", pb)], writes=[("ps", bk)])
            P.dve(lambda e: e.tensor_tensor(out=mrow_, in0=bank(bk, 512, 1), in1=modbr_, op=ALU.add),
                  reads=[("ps", bk), ("modbr", pb)], writes=[("mrow", pb)])
            P.dma("sp", "ms%d" % pb, lambda e: e.dma_start(out=modscr[l:l + 1, j * 512:(j + 1) * 512], in_=mrow_),
                  reads=[("mrow", pb)], writes=[("modscr", l)])

        for i in range(16):
            P.dma("act", "x", lambda e, i=i: e.dma_start(out=x[:, i, :], in_=x_d[i * 128:(i + 1) * 128, :]),
                  writes=[("x", i)])
        P.dve(lambda e: e.memset(rb33, 1.0), writes=["rb33"])
        P.dma("sp", "c1", lambda e: e.dma_start(out=rb33[0:32, :], in_=relb_d), writes=["rb33"])
        P.dma("sp", "c1", lambda e: e.dma_start(out=btab, in_=btab_d.rearrange("d r j -> r d j")), writes=["btab"])
        jmat = ar.f32([128, 128])
        thk = [ar.f32([128, 8, 256]) for _ in range(3)]
        tfx = [ar.f32([128, 8, 256]) for _ in range(2)]
        P.dma("sp", "c1", lambda e: e.dma_start(out=jmat, in_=jmat_d), writes=["jmat"])
        for d in range(3):
            P.pe(lambda e, d=d: e.matmul(bank(6 + d % 2, 384, 8), lhsT=rb33[:, :], rhs=btab[:, d, :], start=True, stop=True),
                 reads=["rb33", "btab"], writes=[("ps", 6 + d % 2)])
            P.dve(lambda e, d=d: e.tensor_copy(out=grow[:, d, :], in_=bank(6 + d % 2, 384, 8)),
                  reads=[("ps", 6 + d % 2)], writes=["grow"])
        P.dma("act", "c2", lambda e: e.dma_start(out=gscr.rearrange("d h j -> h d j"), in_=grow),
              reads=["grow"], writes=["gscr"])
        for d in range(3):
            P.dma("act", "tk%d" % d, lambda e, d=d: e.dma_start(
                out=thk[d], in_=bass.AP(gscr_h, d * 8 * 384, [[1, 128], [384, 8], [1, 256]])),
                reads=["gscr"], writes=[("thk", d)])

        def texp_flip():
            nj = 0
            for d in range(3):
                pb = d % 2
                for j in range(4):
                    bk = 6 + nj % 2
                    nj += 1
                    P.pe(lambda e, d=d, j=j, bk=bk: e.matmul(bank(bk), lhsT=jmat, rhs=thk[d][:, 2 * j:2 * j + 2, :].rearrange("p a b -> p (a b)"),
                                                             start=True, stop=True),
                         reads=["jmat", ("thk", d)], writes=[("ps", bk)])
                    P.dve(lambda e, pb=pb, j=j, bk=bk: e.tensor_copy(out=tfx[pb][:, 2 * j:2 * j + 2, :].rearrange("p a b -> p (a b)"), in_=bank(bk)),
                          reads=[("ps", bk)], writes=[("tfx", pb)])
                P.dma("act", "tx%d" % pb, lambda e, d=d, pb=pb: e.dma_start(out=texp[d].rearrange("h p q -> p h q"), in_=tfx[pb]),
                      reads=[("tfx", pb)], writes=["texp"])

        nblk = 0
        for hh in range(2):
            P.dma("sp", "mb0", lambda e, hh=hh: e.dma_start(out=modbrR, in_=modb_d[0:1, hh * 3072:(hh + 1) * 3072]),
                  writes=["modbrR"])
            for k in range(8):
                sb_ = nblk % 3
                nblk += 1
                P.dma("pool", "mr%d" % sb_, lambda e, hh=hh, k=k, sb_=sb_: e.dma_start(
                    out=stageR[sb_], in_=modw_d[0, k * 128:(k + 1) * 128, hh * 3072:(hh + 1) * 3072]),
                    writes=[("stageR", sb_)])
                for j in range(6):
                    P.pe(lambda e, k=k, j=j, sb_=sb_: e.matmul(bank(j, 512, 1), lhsT=cact[:, k:k + 1], rhs=stageR[sb_][:, j * 512:(j + 1) * 512],
                                                               start=(k == 0), stop=(k == 7)),
                         reads=["cact", ("stageR", sb_)], writes=[("ps", j)])
            for j in range(6):
                P.dve(lambda e, j=j: e.tensor_tensor(out=mrowR[:, j * 512:(j + 1) * 512], in0=bank(j, 512, 1),
                                                     in1=modbrR[:, j * 512:(j + 1) * 512], op=ALU.add),
                      reads=[("ps", j), "modbrR"], writes=["mrowR"])
            P.dma("sp", "ms0", lambda e, hh=hh: e.dma_start(out=modscr[0:1, hh * 3072:(hh + 1) * 3072], in_=mrowR),
                  reads=["mrowR"], writes=[("modscr", 0)])
            if hh == 0:
                texp_flip()

        ssqN = statA[:, 16:32]

        def norm_phase(l, which, pre=None, post=None, have_ssq=False):
            P.barrier()
            ar.reset()
            if pre is not None:
                pre()
            gmod = ar.f32([128, 1024])
            ngb = ar.f32([128, 1024])
            shb = ar.f32([128, 1024])
            h32 = [ar.f32([128, 1024]) for _ in range(2)]
            junk = ar.bf16([128, 1024])
            ssq = ssqN
            rstd = ar.f32([128, 16])
            xh32 = [ar.f32([128, 8, 128]) for _ in range(2)] if which == 2 else None
            off_sh, off_sc = (0, 1024) if which == 1 else (3072, 4096)
            ng_d = n1g_d if which == 1 else n2g_d
            P.dma("sp", "n0", lambda e: e.dma_start(out=gmod, in_=modscr[l, off_sc:off_sc + 1024].partition_broadcast(128)),
                  writes=["gmod"])
            P.dma("sp", "n0", lambda e: e.dma_start(out=ngb, in_=ng_d[l, :].partition_broadcast(128)), writes=["ngb"])
            P.dma("sp", "n0", lambda e: e.dma_start(out=shb, in_=modscr[l, off_sh:off_sh + 1024].partition_broadcast(128)),
                  writes=["shb"])
            P.dve(lambda e: e.scalar_tensor_tensor(out=gmod, in0=gmod, scalar=1.0, in1=ngb, op0=ALU.add, op1=ALU.mult),
                  reads=["gmod", "ngb"], writes=["gmod"])
            if not have_ssq:
                for i in range(16):
                    P.act(lambda e, i=i: e.activation(out=junk, in_=x[:, i, :], func=AF.Square, accum_out=ssq[:, i:i + 1]),
                          reads=[("x", i)], writes=["junk", ("ssq", i)])
            P.act(lambda e: e.activation(out=rstd, in_=ssq, func=AF.Sqrt, bias=eps_t[:, 0:1], scale=1.0 / 1024),
                  reads=[("ssq", i) for i in range(16)] + ["eps"], writes=["rstd0"])
            P.dve(lambda e: e.reciprocal(out=rstd, in_=rstd), reads=["rstd0"], writes=["rstd"])
            def stage1(i):
                hb = h32[i % 2]
                pp = i % 2
                P.dve(lambda e: e.scalar_tensor_tensor(out=hb, in0=x[:, i, :], scalar=rstd[:, i:i + 1], in1=gmod,
                                                       op0=ALU.mult, op1=ALU.mult),
                      reads=[("x", i), "rstd", "gmod"], writes=[("h32", pp)])
                P.dve(lambda e: e.tensor_tensor(out=hb, in0=hb, in1=shb, op=ALU.add),
                      reads=[("h32", pp), "shb"], writes=[("h32", pp)])
                for c in range(8):
                    P.pe(lambda e, c=c: e.transpose(out=PS[:, pp * 1024 + c * 128:pp * 1024 + (c + 1) * 128],
                                                    in_=hb[:, c * 128:(c + 1) * 128], identity=identf[:]),
                         reads=[("h32", pp), "identf"], writes=[("ps", 2 * pp + c // 4)])

            def stage2(i):
                pp = i % 2
                if which == 1:
                    P.act(lambda e: e.activation(out=hT[:, :, i * 128:(i + 1) * 128],
                                                 in_=PS[:, pp * 1024:(pp + 1) * 1024].rearrange("p (c t) -> p c t", c=8),
                                                 func=AF.Copy),
                          reads=[("ps", 2 * pp), ("ps", 2 * pp + 1)], writes=[("hT", i)])
                if which == 2:
                    xb = xh32[pp]
                    P.dve(lambda e: e.tensor_copy(out=xb, in_=PS[:, pp * 1024:(pp + 1) * 1024].rearrange("p (c t) -> p c t", c=8)),
                          reads=[("ps", 2 * pp), ("ps", 2 * pp + 1)], writes=[("xh32", pp)])
                    P.act(lambda e: e.activation(out=hT[:, :, i * 128:(i + 1) * 128], in_=xb, func=AF.Copy),
                          reads=[("xh32", pp)], writes=[("hT", i)])
                    for c in range(8):
                        P.pe(lambda e, c=c: e.matmul(PS[:, 4 * 512 + i * 16:4 * 512 + (i + 1) * 16], lhsT=xb[:, c, :],
                                                     rhs=rw[:, c, :], start=(c == 0), stop=(c == 7)),
                             reads=[("xh32", pp), "rw"], writes=[("ps", 4)])

            for s in range(17):
                if s < 16:
                    stage1(s)
                if s >= 1:
                    stage2(s - 1)

        def norm_phase_post(post):
            if post is not None:
                post()

        def router_phase():
            T = lambda: ar.f32([128, 16, 16])
            L, E_, pr, sel, t1, t2, t3, selm = T(), T(), T(), T(), T(), T(), T(), T()
            mx = ar.f32([128, 16]); sm = ar.f32([128, 16])
            g4 = ar.f32([128, 16, 4]); p6 = [ar.f32([128, 16, 4]) for _ in range(6)]
            gmx = ar.f32([128, 16]); gm = ar.f32([128, 16, 4])
            m1 = ar.f32([128, 16]); m2 = ar.f32([128, 16])
            bc = lambda a, n: a.unsqueeze(2).to_broadcast([128, 16, n])
            v = lambda e: e
            P.dve(lambda e: e.tensor_copy(out=L, in_=PS[:, 2048:2048 + 256].rearrange("p (a b) -> p a b", a=16)),
                  reads=[("ps", 4)], writes=["rt"])
            seq = []
            seq.append(lambda e: e.tensor_reduce(out=mx, in_=L, axis=AX.X, op=ALU.max))
            seq.append(lambda e: e.tensor_tensor(out=t1, in0=L, in1=bc(mx, 16), op=ALU.subtract))
            for f in seq:
                P.dve(f, reads=["rt"], writes=["rt"])
            P.act(lambda e: e.activation(out=E_, in_=t1, func=AF.Exp), reads=["rt"], writes=["rt"])
            seq = []
            seq.append(lambda e: e.tensor_reduce(out=sm, in_=E_, axis=AX.X, op=ALU.add))
            seq.append(lambda e: e.reciprocal(out=sm, in_=sm))
            seq.append(lambda e: e.tensor_tensor(out=pr, in0=E_, in1=bc(sm, 16), op=ALU.mult))
            seq.append(lambda e: e.tensor_tensor(out=sel, in0=pr, in1=rbb[:], op=ALU.add))
            s4 = sel.rearrange("p a (g k) -> p a g k", k=4)
            pairs = [(0, 1), (0, 2), (0, 3), (1, 2), (1, 3), (2, 3)]
            for q, (a, b) in enumerate(pairs):
                seq.append(lambda e, q=q, a=a, b=b: e.tensor_tensor(out=p6[q], in0=s4[:, :, :, a], in1=s4[:, :, :, b], op=ALU.add))
            seq.append(lambda e: e.tensor_tensor(out=g4, in0=p6[0], in1=p6[1], op=ALU.max))
            for q in range(2, 6):
                seq.append(lambda e, q=q: e.tensor_tensor(out=g4, in0=g4, in1=p6[q], op=ALU.max))
            seq.append(lambda e: e.tensor_reduce(out=gmx, in_=g4, axis=AX.X, op=ALU.max))
            seq.append(lambda e: e.tensor_tensor(out=gm, in0=g4, in1=bc(gmx, 4), op=ALU.is_ge))
            gm16 = gm.unsqueeze(3).to_broadcast([128, 16, 4, 4])
            sm4 = selm.rearrange("p a (g k) -> p a g k", k=4)
            seq.append(lambda e: e.scalar_tensor_tensor(out=sm4, in0=s4, scalar=100.0, in1=gm16, op0=ALU.add, op1=ALU.mult))
            seq.append(lambda e: e.tensor_scalar(out=selm, in0=selm, scalar1=-100.0, scalar2=None, op0=ALU.add))
            seq.append(lambda e: e.tensor_reduce(out=m1, in_=selm, axis=AX.X, op=ALU.max))
            seq.append(lambda e: e.tensor_tensor(out=t2, in0=selm, in1=bc(m1, 16), op=ALU.is_ge))
            seq.append(lambda e: e.scalar_tensor_tensor(out=t3, in0=t2, scalar=-1000.0, in1=selm, op0=ALU.mult, op1=ALU.add))
            seq.append(lambda e: e.tensor_reduce(out=m2, in_=t3, axis=AX.X, op=ALU.max))
            seq.append(lambda e: e.tensor_tensor(out=t2, in0=selm, in1=bc(m2, 16), op=ALU.is_ge))
            seq.append(lambda e: e.tensor_tensor(out=t3, in0=t2, in1=pr, op=ALU.mult))
            seq.append(lambda e: e.tensor_reduce(out=sm, in_=t3, axis=AX.X, op=ALU.add))
            seq.append(lambda e: e.reciprocal(out=sm, in_=sm))
            seq.append(lambda e: e.tensor_tensor(out=gates[:], in0=t3, in1=bc(sm, 16), op=ALU.mult))
            for f in seq:
                P.dve(f, reads=["rt", "rbb"], writes=["rt", "gates"])

        def dump_and_finish():
            for i in range(16):
                P.dma("sp", "dbg", lambda e, i=i: e.dma_start(out=dbg_x[i * 128:(i + 1) * 128, :], in_=x[:, i, :]),
                      reads=[("x", i)])
            P.dma("sp", "dbg", lambda e: e.dma_start(out=dbg_hT, in_=hT[:]), reads=[("hT", i) for i in range(16)])
            P.dma("sp", "dbg", lambda e: e.dma_start(out=dbg_g, in_=gates[:].rearrange("p a b -> p (a b)")), reads=["gates"])
            P.emit(final_wait_streams=["dbg"])

        def layer(l):
            ar2.reset()
            vball = ar2.bf16([128, 16, 512])
            gub = ar2.bf16([128, 16, 512])
            w_uva = ar2.bf16([128, 8, 1024])
            w_oa = ar2.bf16([128, 4, 1024])
            WT = ar2.bf16([128, 8, 128])
            oaT = [ar2.bf16([128, 4, 128]) for _ in range(2)]
            g1b = ar2.f32([128, 1024])
            Wld = ar2.f32([128, 8, 128])
            lngb = ar2.f32([128, 512]); lnbb = ar2.f32([128, 512])
            gv = [ar2.f32([128, 512]) for _ in range(2)]
            vn = [ar2.f32([128, 512]) for _ in range(2)]
            oa = [ar2.f32([128, 512]) for _ in range(2)]
            junkA = ar2.bf16([128, 512])
            bsT = ar2.f32([8, 128])
            gabf = ar2.f32([128, 8])
            st6 = ar2.f32([128, 2, 6]); mv = ar2.f32([128, 2, 2]); vpe = ar2.f32([128, 2]); rsv = ar2.f32([128, 2])
            ssqa = ar2.f32([128, 16]); rsa = ar2.f32([128, 16])
            assert vball is not None

            def A_pre():
                for h2 in range(2):
                    P.dma("pool", "wA", lambda e, h2=h2: e.dma_start(
                        out=w_uva[:, :, h2 * 512:(h2 + 1) * 512],
                        in_=win_d[l, :, h2 * 512:(h2 + 1) * 512].rearrange("(k p) n -> p k n", p=128)), writes=[("w_uva", h2)])
                P.dma("pool", "wA", lambda e: e.dma_start(out=w_oa, in_=wout_d[l, 0:512, :].rearrange("(k p) n -> p k n", p=128)),
                      writes=["w_oa"])
                P.dma("sp", "a0", lambda e: e.dma_start(out=g1b, in_=modscr[l, 2048:3072].partition_broadcast(128)), writes=["g1b"])
                P.dma("sp", "a0", lambda e: e.dma_start(out=gabf, in_=gab_d[l]), writes=["gabf"])
                P.dma("sp", "a0", lambda e: e.dma_start(out=Wld, in_=ws_d[l].rearrange("g t s -> t g s")), writes=["Wld"])
                P.dma("sp", "a0", lambda e: e.dma_start(out=bsT, in_=bs_d[l]), writes=["bsT"])
                P.dma("sp", "a0", lambda e: e.dma_start(out=lngb, in_=lng_d[l, :].partition_broadcast(128)), writes=["lngb"])
                P.dma("sp", "a0", lambda e: e.dma_start(out=lnbb, in_=lnb_d[l, :].partition_broadcast(128)), writes=["lnbb"])

            def A_post():
                for c in range(4):
                    P.dve(lambda e, c=c: e.scalar_tensor_tensor(out=w_oa[:, c, :], in0=w_oa[:, c, :], scalar=gabf[:, c:c + 1], in1=g1b,
                                                                 op0=ALU.mult, op1=ALU.mult),
                          reads=["w_oa", "gabf", "g1b"], writes=["w_oa"])
                for g in range(8):
                    P.pe(lambda e, g=g: e.transpose(out=PS[:, 3072 + g * 128:3072 + (g + 1) * 128], in_=Wld[:, g, :], identity=identf[:]),
                         reads=["Wld", "identf"], writes=[("ps", 6 + g // 4)])
                for g in range(8):
                    P.dve(lambda e, g=g: e.tensor_tensor(out=WT[:, g, :], in0=PS[:, 3072 + g * 128:3072 + (g + 1) * 128], in1=trilm[:], op=ALU.mult),
                          reads=[("ps", 6 + g // 4), "trilm"], writes=["WT"])


            norm_phase(l, 1, pre=A_pre, have_ssq=(l > 0))
            assert ar.off <= 8 * 1024, ar.off
            A_post()
            if stop == ("N1", l):
                P.barrier(); dump_and_finish(); return True

            P.barrier()
            def A1(i):
                pp = i % 2
                for k in range(8):
                    P.pe(lambda e, k=k: e.matmul(bank(pp), lhsT=hT[:, k, i * 128:(i + 1) * 128], rhs=w_uva[:, k, 0:512],
                                                 start=(k == 0), stop=(k == 7)),
                         reads=[("hT", i), ("w_uva", 0)], writes=[("ps", pp)])
                for k in range(8):
                    P.pe(lambda e, k=k: e.matmul(bank(2 + pp), lhsT=hT[:, k, i * 128:(i + 1) * 128], rhs=w_uva[:, k, 512:1024],
                                                 start=(k == 0), stop=(k == 7)),
                         reads=[("hT", i), ("w_uva", 1)], writes=[("ps", 2 + pp)])
                P.act(lambda e: e.activation(out=gub[:, i, :], in_=bank(pp), func=AF.Gelu), reads=[("ps", pp)], writes=[("gub", i)])
                P.act(lambda e: e.activation(out=gv[pp], in_=bank(2 + pp), func=AF.Gelu), reads=[("ps", 2 + pp)], writes=[("gv", pp)])
                P.dve(lambda e: e.bn_stats(out=st6[:, pp, :], in_=gv[pp]), reads=[("gv", pp)], writes=[("st6", pp)])
                P.dve(lambda e: e.bn_aggr(out=mv[:, pp, :], in_=st6[:, pp, :]), reads=[("st6", pp)], writes=[("mv", pp)])
                P.dve(lambda e: e.tensor_scalar(out=vpe[:, pp:pp + 1], in0=mv[:, pp, 1:2], scalar1=EPS, scalar2=None, op0=ALU.add),
                      reads=[("mv", pp)], writes=[("vpe", pp)])
                P.pool(lambda e: e.tensor_tensor(out=rsv[:, pp:pp + 1], in0=vpe[:, pp:pp + 1], in1=nhalf[:, 0:1], op=ALU.pow),
                       reads=[("vpe", pp), "nhalf"], writes=[("rsv", pp)])
                P.dve(lambda e: e.tensor_scalar(out=vn[pp], in0=gv[pp], scalar1=mv[:, pp, 0:1], scalar2=rsv[:, pp:pp + 1],
                                                op0=ALU.subtract, op1=ALU.mult),
                      reads=[("gv", pp), ("mv", pp), ("rsv", pp)], writes=[("vn", pp)])
                P.dve(lambda e: e.tensor_tensor(out=vn[pp], in0=vn[pp], in1=lngb, op=ALU.mult),
                      reads=[("vn", pp), "lngb"], writes=[("vn", pp)])
                P.dve(lambda e: e.tensor_tensor(out=vball[:, i, :], in0=vn[pp], in1=lnbb, op=ALU.add),
                      reads=[("vn", pp), "lnbb"], writes=[("vball", i)])

            def A2(i):
                pp = i % 2
                P.pe(lambda e: e.matmul(bank(pp), lhsT=bsT[:, :], rhs=ind[:, :], start=True, stop=False),
                     reads=["bsT", "ind"], writes=[("ps", pp)])
                for g in range(8):
                    P.pe(lambda e, g=g: e.matmul(PS[:, pp * 512 + g * 64:pp * 512 + (g + 1) * 64], lhsT=WT[:, g, :],
                                                 rhs=vball[:, i, g * 64:(g + 1) * 64], start=False, stop=(g == 7)),
                         reads=["WT", ("vball", i)], writes=[("ps", pp)])
                P.dve(lambda e: e.tensor_tensor(out=oa[pp], in0=bank(pp), in1=gub[:, i, :], op=ALU.mult),
                      reads=[("ps", pp), ("gub", i)], writes=[("oa", pp)])
                P.act(lambda e: e.activation(out=junkA, in_=oa[pp], func=AF.Square, accum_out=ssqa[:, i:i + 1]),
                      reads=[("oa", pp)], writes=["junkA", ("ssqa", i)])
                P.dve(lambda e: e.tensor_scalar(out=rsa[:, i:i + 1], in0=ssqa[:, i:i + 1], scalar1=1.0 / 512, scalar2=EPS,
                                                op0=ALU.mult, op1=ALU.add),
                      reads=[("ssqa", i)], writes=[("rsa0", i)])
                P.pool(lambda e: e.tensor_tensor(out=rsa[:, i:i + 1], in0=rsa[:, i:i + 1], in1=nhalf[:, 0:1], op=ALU.pow),
                       reads=[("rsa0", i), "nhalf"], writes=[("rsa", i)])

            def A2b(i):
                pp = i % 2
                for c in range(4):
                    P.pe(lambda e, c=c: e.transpose(out=PS[:, (2 + pp) * 512 + c * 128:(2 + pp) * 512 + (c + 1) * 128],
                                                    in_=oa[pp][:, c * 128:(c + 1) * 128], identity=identf[:]),
                         reads=[("oa", pp), "identf"], writes=[("ps", 2 + pp)])
                P.act(lambda e: e.activation(out=oaT[pp], in_=bank(2 + pp).rearrange("p (c t) -> p c t", c=4), func=AF.Copy),
                      reads=[("ps", 2 + pp)], writes=[("oaT", pp)])

            def A3(i):
                pp = i % 2
                for hf in range(2):
                    bk = 4 + 2 * pp + hf
                    for c in range(4):
                        P.pe(lambda e, c=c, hf=hf, bk=bk: e.matmul(bank(bk), lhsT=oaT[pp][:, c, :], rhs=w_oa[:, c, hf * 512:(hf + 1) * 512],
                                                                   start=(c == 0), stop=(c == 3)),
                             reads=[("oaT", pp), "w_oa"], writes=[("ps", bk)])
                    P.dve(lambda e, hf=hf, bk=bk: e.scalar_tensor_tensor(out=x[:, i, hf * 512:(hf + 1) * 512], in0=bank(bk),
                                                                         scalar=rsa[:, i:i + 1], in1=x[:, i, hf * 512:(hf + 1) * 512],
                                                                         op0=ALU.mult, op1=ALU.add),
                          reads=[("ps", bk), ("rsa", i), ("x", i)], writes=[("x", i)])

            for i in range(16):
                A1(i)
            for s in range(18):
                if s < 16:
                    A2(s)
                if 0 <= s - 1 < 16:
                    A2b(s - 1)
                if 0 <= s - 2 < 16:
                    A3(s - 2)
            if stop == ("A", l):
                P.barrier(); dump_and_finish(); return True

            P.barrier()
            ar.reset()
            ost = [ar.bf16([128, 1024]) for _ in range(2)]
            wq = ar.bf16([128, 8, 128]); wk = ar.bf16([128, 8, 128]); wv = ar.bf16([128, 8, 128])
            QTd = [[ar.bf16([128, 2048]) for _ in range(3)] for _ in range(2)]
            KTd = [ar.bf16([128, 2048]) for _ in range(3)]
            VT = ar.bf16([128, 2048])
            Vb = [ar.bf16([128, 16, 2, 65]) for _ in range(3)]
            Pt = [ar.bf16([128, 512]) for _ in range(NSB)]
            obtok = ar.bf16([128, 16, 128])
            Tt = [[ar.f32([128, 512]) for _ in range(3)] for _ in range(2)]
            E32 = [ar.f32([128, 512]) for _ in range(NSB)]
            accS = [ar.f32([65, 512]) for _ in range(2)]
            rden = ar.f32([128, 4])
            ssqb = ar.f32([128, 16, 4]); rsb = statA[:, 0:16]
            junkB = ar.bf16([128, 128])
            b_end = ar.off
            for d in range(3):
                P.dve(lambda e, d=d: e.memset(Vb[d][:, :, :, 64:65], 1.0), writes=[("Vb", d)])
            for di in range(3):
                P.dve(lambda e, di=di: e.memset(QTd[0][di][64:128, :], 0.0), writes=["qz0"])
                P.dve(lambda e, di=di: e.memset(QTd[1][di][0:64, :], 0.0), writes=["qz1"])
            def B_proj(hp):
                for nm, wt, col in (("wq", wq, 1024), ("wk", wk, 1536), ("wv", wv, 2048)):
                    P.dma("pool", "wB", lambda e, wt=wt, col=col: e.dma_start(
                        out=wt, in_=win_d[l, :, col + hp * 128:col + (hp + 1) * 128].rearrange("(k p) n -> p k n", p=128)),
                        writes=[nm])
                nbk = 0
                for tb in range(4):
                    for nm, wt in (("wq", wq), ("wk", wk), ("wv", wv)):
                        bk = 6 + (nbk % 2)
                        nbk += 1
                        for k in range(8):
                            P.pe(lambda e, k=k, wt=wt, bk=bk, tb=tb: e.matmul(bank(bk), lhsT=wt[:, k, :], rhs=hT[:, k, tb * 512:(tb + 1) * 512],
                                                                              start=(k == 0), stop=(k == 7)),
                                 reads=[nm], writes=[("ps", bk)])
                        if nm == "wk":
                            P.act(lambda e, bk=bk, tb=tb: e.activation(out=KTd[0][:, tb * 512:(tb + 1) * 512], in_=bank(bk), func=AF.Copy),
                                  reads=[("ps", bk)], writes=[("wkT", tb)])
                        elif nm == "wv":
                            P.dve(lambda e, bk=bk, tb=tb: e.tensor_copy(out=VT[:, tb * 512:(tb + 1) * 512], in_=bank(bk)),
                                  reads=[("ps", bk)], writes=[("VT", tb)])
                        else:
                            for hh in range(2):
                                P.act(lambda e, bk=bk, tb=tb, hh=hh: e.activation(
                                    out=QTd[hh][0][64 * hh:64 * hh + 64, tb * 512:(tb + 1) * 512],
                                    in_=PS[64 * hh:64 * hh + 64, bk * 512:(bk + 1) * 512], func=AF.Copy, scale=0.125),
                                    reads=[("ps", bk)], writes=[("wqT", tb)])
                def deint(t, d):
                    return t.rearrange("p (r i) -> p r i", r=d), None
                for di in (1, 2):
                    d = CONFIGS[di][1]
                    P.pool(lambda e, di=di, d=d: e.tensor_copy(out=KTd[di].rearrange("p (r i) -> p r i", r=d),
                                                               in_=KTd[0].rearrange("p (i r) -> p r i", r=d)),
                           reads=[("wkT", j) for j in range(4)], writes=[("wkTd", di)])
                    P.pool(lambda e, di=di, d=d: e.tensor_copy(out=QTd[0][di][0:64, :].rearrange("p (r i) -> p r i", r=d),
                                                               in_=QTd[0][0][0:64, :].rearrange("p (i r) -> p r i", r=d)),
                           reads=[("wqT", j) for j in range(4)], writes=[("wqTd", 0, di)])
                for di in (1, 2):
                    d = CONFIGS[di][1]
                    P.pool(lambda e, di=di, d=d: e.tensor_copy(out=QTd[1][di][64:128, :].rearrange("p (r i) -> p r i", r=d),
                                                               in_=QTd[1][0][64:128, :].rearrange("p (i r) -> p r i", r=d)),
                           reads=[("wqT", j) for j in range(4)], writes=[("wqTd", 1, di)])
                for di, (win, d) in enumerate(CONFIGS):
                    nb = 16 // d
                    for q8 in range(2):
                        bk = 6 + (nbk % 2)
                        nbk += 1
                        pbf = bank(bk).bitcast(BF16)
                        for t8 in range(8):
                            tile = q8 * 8 + t8
                            r, m = tile // nb, tile % nb
                            t0 = r + d * 128 * m
                            P.pe(lambda e, t0=t0, d=d, pbf=pbf, t8=t8: e.transpose(
                                out=pbf[:, t8 * 128:(t8 + 1) * 128], in_=VT[:, t0:t0 + d * 127 + 1:d], identity=identb[:]),
                                reads=[("VT", j) for j in range(4)] + ["identb"], writes=[("ps", bk)])
                        P.act(lambda e, di=di, q8=q8, pbf=pbf: e.activation(
                            out=Vb[di][:, q8 * 8:(q8 + 1) * 8, :, 0:64],
                            in_=pbf.rearrange("p (a h e) -> p a h e", a=8, h=2), func=AF.Copy),
                            reads=[("ps", bk)], writes=[("Vb", di)])

            def B_head(hp, hl, base):
                h = 2 * hp + hl
                hpar = h % 2
                p0 = 64 * hl
                for di in range(3):
                    P.dma("sp", "tt%d" % hpar, lambda e, di=di: e.dma_start(
                        out=Tt[hpar][di].rearrange("p (a b) -> p a b", a=2),
                        in_=bass.AP(texp.tensor, (di * 8 + h) * 128 * 256, [[256, 128], [0, 2], [1, 256]])),
                        writes=[("Tt", hpar, di)])
                started = [False] * 4
                steps = []
                for di, (win, d) in enumerate(CONFIGS):
                    nb = 16 // d
                    if nb >= 2:
                        for r in range(d):
                            for m in range(0, nb, 2):
                                steps.append((di, d, nb, [(r, m, 256, 0), (r, m + 1, 256 if m + 2 < nb else 128, 256)]))
                    else:
                        for r in range(0, d, 2):
                            steps.append((di, d, nb, [(r, 0, 128, 0), (r + 1, 0, 128, 256)]))

                def region(ap512, subs):
                    if subs[0][2] == 256:
                        return ap512[:, 0:256 + subs[1][2]]
                    return ap512.rearrange("p (a b) -> p a b", a=2)[:, :, 0:128]

                def S_step(st, sidx):
                    di, d, nb, subs = st
                    sb_ = sidx % NSB
                    bk = 4 + sb_
                    for (r, m, width, coff) in subs:
                        bs0 = r * (2048 // d) + 128 * m
                        P.pe(lambda e, bs0=bs0, width=width, coff=coff: e.matmul(
                            PS[:, bk * 512 + coff:bk * 512 + coff + width], lhsT=KTd[di][:, bs0:bs0 + 128],
                            rhs=QTd[hl][di][:, bs0:bs0 + width], start=True, stop=True),
                            reads=[("wkT", j) for j in range(4)] + [("wqT", j) for j in range(4)] +
                            ([("wkTd", di), ("wqTd", hl, di)] if di > 0 else []), writes=[("ps", bk)])
                    P.dve(lambda e: e.tensor_tensor(out=region(E32[sb_], subs), in0=region(bank(bk), subs),
                                                    in1=region(Tt[hpar][di], subs), op=ALU.add),
                          reads=[("ps", bk), ("Tt", hpar, di)], writes=[("E32", sb_)])
                    P.act(lambda e: e.activation(out=region(Pt[sb_], subs), in_=region(E32[sb_], subs), func=AF.Exp),
                          reads=[("E32", sb_)], writes=[("Pt", sb_)])

                def PV_step(st, sidx):
                    di, d, nb, subs = st
                    sb_ = sidx % NSB
                    for (r, m, width, coff) in subs:
                        tile = r * nb + m
                        lhs = Vb[di][:, tile, hl, :]
                        for qb in ((m, m + 1) if m + 1 < nb else (m,)):
                            c0 = coff + (qb - m) * 128
                            if d < 16:
                                if d == 1:
                                    bk, cs, step = qb // 4, (qb % 4) * 128, 1
                                else:
                                    bk, cs, step = qb, r, 4
                                pieces = [(bk, cs, step, 128, c0)]
                            else:
                                pieces = [(b4, r, 16, 32, c0 + 32 * b4) for b4 in range(4)]
                            for (bk, cs, step, cnt, pc) in pieces:
                                st_flag = not started[bk]
                                started[bk] = True
                                P.pe(lambda e, bk=bk, cs=cs, step=step, cnt=cnt, pc=pc, st_flag=st_flag, lhs=lhs: e.matmul(
                                    PS[0:65, bk * 512 + cs:bk * 512 + cs + step * (cnt - 1) + 1:step], lhsT=lhs,
                                    rhs=Pt[sb_][:, pc:pc + cnt], start=st_flag, stop=False, skip_group_check=True),
                                    reads=[("Vb", di), ("Pt", sb_)], writes=[("ps", bk)])

                SK = NSB - 1
                for j in range(len(steps) + SK):
                    if j < len(steps):
                        S_step(steps[j], base + j)
                    if j - SK >= 0:
                        PV_step(steps[j - SK], base + j - SK)
                if stop == ("Bs", l):
                    return len(steps)
                for b4 in range(4):
                    ab = accS[b4 % 2]
                    P.act(lambda e, b4=b4, ab=ab: e.activation(out=ab, in_=bank(b4, 512, 65), func=AF.Copy),
                          reads=[("ps", b4)], writes=[("accS", b4 % 2)])
                    bk = 6 + (b4 % 2)
                    for j in range(4):
                        P.pe(lambda e, j=j, ab=ab, bk=bk: e.transpose(out=PS[:, bk * 512 + j * 65:bk * 512 + (j + 1) * 65],
                                                                      in_=ab[:, j * 128:(j + 1) * 128], identity=identf[0:65, 0:65]),
                             reads=[("accS", b4 % 2), "identf"], writes=[("ps", bk)])
                    pv = bank(bk, 260).rearrange("p (j e) -> p j e", j=4)
                    P.dve(lambda e, pv=pv: e.reciprocal(out=rden.unsqueeze(2), in_=pv[:, :, 64:65]),
                          reads=[("ps", bk)], writes=["rden"])
                    P.dve(lambda e, pv=pv, b4=b4: e.tensor_tensor(out=obtok[:, b4 * 4:(b4 + 1) * 4, p0:p0 + 64], in0=pv[:, :, 0:64],
                                                                  in1=rden.unsqueeze(2).to_broadcast([128, 4, 64]), op=ALU.mult),
                          reads=[("ps", bk), "rden"], writes=[("obtok", hl)])
                return len(steps)

            def B_pair_end(hp):
                for i in range(16):
                    P.act(lambda e, i=i: e.activation(out=junkB, in_=obtok[:, i, :], func=AF.Square, accum_out=ssqb[:, i, hp:hp + 1]),
                          reads=[("obtok", 0), ("obtok", 1)], writes=["junkB", ("ssqb", hp)])
                for i8 in range(2):
                    bk = 6 + i8
                    pbf = bank(bk).bitcast(BF16)
                    for j in range(8):
                        i = i8 * 8 + j
                        P.pe(lambda e, i=i, j=j, pbf=pbf: e.transpose(out=pbf[:, j * 128:(j + 1) * 128], in_=obtok[:, i, :], identity=identb[:]),
                             reads=[("obtok", 0), ("obtok", 1), "identb"], writes=[("ps", bk)])
                    P.act(lambda e, i8=i8, pbf=pbf: e.activation(out=ost[i8], in_=pbf, func=AF.Copy),
                          reads=[("ps", bk)], writes=[("ost", i8)])
                    P.dma("sp", "ob", lambda e, i8=i8: e.dma_start(out=obT_d[:, hp, i8 * 1024:(i8 + 1) * 1024], in_=ost[i8]),
                          reads=[("ost", i8)], writes=["obT_d"])

            sidx = 0
            for hp_ in range(4):
                B_proj(hp_)
                if stop == ("Bp", l):
                    P.barrier(); dump_and_finish(); return True
                for hl_ in range(2):
                    sidx += B_head(hp_, hl_, sidx)
                    if stop in (("Bs", l), ("Bh", l)):
                        P.barrier(); dump_and_finish(); return True
                B_pair_end(hp_)
                if stop == ("Be", l):
                    P.barrier(); dump_and_finish(); return True
            P.dve(lambda e: e.tensor_reduce(out=rsb, in_=ssqb, axis=AX.X, op=ALU.add), reads=[("ssqb", j) for j in range(4)], writes=["rsb0"])
            P.dve(lambda e: e.tensor_scalar(out=rsb, in0=rsb, scalar1=1.0 / 512, scalar2=EPS, op0=ALU.mult, op1=ALU.add),
                  reads=["rsb0"], writes=["rsb1"])
            P.pool(lambda e: e.tensor_tensor(out=rsb, in0=rsb, in1=nhalf[:, 0:1].to_broadcast([128, 16]), op=ALU.pow),
                   reads=["rsb1", "nhalf"], writes=["rsb"])
            if stop == ("B1", l):
                P.barrier()
                P.dma("sp", "dbg", lambda e: e.dma_start(out=dbg_obT, in_=obT_d))
                P.barrier(); dump_and_finish(); return True
            P.barrier()
            ar.reset()
            obT = ar.bf16([128, 4, 2048])
            P.dma("sp", "a0", lambda e: e.dma_start(out=obT, in_=obT_d), writes=["obT"])
            w_ob = ar.bf16([128, 4, 1024])
            junkb = ar.bf16([128, 1024])
            g1b2 = ar.f32([128, 1024])
            gabf2 = ar.f32([128, 8])
            P.dma("pool", "wA", lambda e: e.dma_start(out=w_ob, in_=wout_d[l, 512:1024, :].rearrange("(k p) n -> p k n", p=128)),
                  writes=["w_ob"])
            P.dma("sp", "a0", lambda e: e.dma_start(out=g1b2, in_=modscr[l, 2048:3072].partition_broadcast(128)), writes=["g1b2"])
            P.dma("sp", "a0", lambda e: e.dma_start(out=gabf2, in_=gab_d[l]), writes=["gabf2"])
            for c in range(4):
                P.dve(lambda e, c=c: e.scalar_tensor_tensor(out=w_ob[:, c, :], in0=w_ob[:, c, :], scalar=gabf2[:, 4 + c:5 + c], in1=g1b2,
                                                             op0=ALU.mult, op1=ALU.mult),
                      reads=["w_ob", "gabf2", "g1b2"], writes=["w_ob"])
            for i in range(16):
                for hf in range(2):
                    bk = 2 * (i % 2) + hf
                    for c in range(4):
                        P.pe(lambda e, c=c, hf=hf, bk=bk, i=i: e.matmul(bank(bk), lhsT=obT[:, c, i * 128:(i + 1) * 128],
                                                                        rhs=w_ob[:, c, hf * 512:(hf + 1) * 512], start=(c == 0), stop=(c == 3)),
                             reads=["w_ob", "obT"], writes=[("ps", bk)])
                    P.dve(lambda e, hf=hf, bk=bk, i=i: e.scalar_tensor_tensor(out=x[:, i, hf * 512:(hf + 1) * 512], in0=bank(bk),
                                                                              scalar=rsb[:, i:i + 1], in1=x[:, i, hf * 512:(hf + 1) * 512],
                                                                              op0=ALU.mult, op1=ALU.add),
                          reads=[("ps", bk), ("x", i)], writes=[("x", i)])
                P.act(lambda e, i=i: e.activation(out=junkb, in_=x[:, i, :], func=AF.Square, accum_out=ssqN[:, i:i + 1]),
                      reads=[("x", i)], writes=["junkb", ("ssq", i)])
            if stop == ("B", l):
                P.barrier(); dump_and_finish(); return True

            ar2.reset()
            sg = [ar2.bf16([128, 512]) for _ in range(2)]
            actT = [ar2.bf16([128, 4, 512]) for _ in range(2)]
            junkM = ar2.bf16([128, 1024])
            if l == 0:
                stageM = [ar2.bf16([128, 8, 512]) for _ in range(2)]
                modbrM = [ar2.f32([1, 512]) for _ in range(2)]
                mrowM = [ar2.f32([1, 512]) for _ in range(2)]
            assert ar2.off <= 11 * 1024, ar2.off
            ar2.off = 11 * 1024
            wgb = [ar2.bf16([128, 8, 512]) for _ in range(2)]
            wub = [ar2.bf16([128, 8, 512]) for _ in range(2)]
            wdb = [ar2.bf16([128, 4, 1024]) for _ in range(2)]
            g2b = ar2.f32([128, 1024])

            def load_expert(ex):
                pb = ex % 2
                for kk in range(2):
                    P.dma("pool", "wg%d" % pb, lambda e, kk=kk: e.dma_start(
                        out=wgb[pb][:, kk * 4:(kk + 1) * 4, :],
                        in_=wg_d[l, ex, kk * 512:(kk + 1) * 512, :].rearrange("(k p) n -> p k n", p=128)), writes=[("wg", pb)])
                    P.dma("pool", "wu%d" % pb, lambda e, kk=kk: e.dma_start(
                        out=wub[pb][:, kk * 4:(kk + 1) * 4, :],
                        in_=wu_d[l, ex, kk * 512:(kk + 1) * 512, :].rearrange("(k p) n -> p k n", p=128)), writes=[("wu", pb)])
                    P.dma("pool", "wd%d" % pb, lambda e, kk=kk: e.dma_start(
                        out=wdb[pb][:, kk * 2:(kk + 1) * 2, :],
                        in_=wd_d[l, ex, kk * 256:(kk + 1) * 256, :].rearrange("(k p) n -> p k n", p=128)), writes=[("wd", pb)])
                for c in range(4):
                    P.pool(lambda e, c=c: e.tensor_tensor(out=wdb[pb][:, c, :], in0=wdb[pb][:, c, :], in1=g2b, op=ALU.mult),
                           reads=[("wd", pb), "g2b"], writes=[("wd", pb)])

            def M_pre():
                P.dma("sp", "m0", lambda e: e.dma_start(out=g2b, in_=modscr[l, 5120:6144].partition_broadcast(128)), writes=["g2b"])
                load_expert(0)
                load_expert(1)

            norm_phase(l, 2, pre=M_pre, have_ssq=True)
            if stop == ("N2a", l):
                P.barrier(); dump_and_finish(); return True
            P.barrier()

            def GU(ex, tb, n):
                pb = ex % 2
                ab = n % 2
                for fc in range(4):
                    q = fc % 2
                    for k in range(8):
                        P.pe(lambda e, k=k, fc=fc, q=q: e.matmul(bank(q), lhsT=wgb[pb][:, k, fc * 128:(fc + 1) * 128],
                                                                 rhs=hT[:, k, tb * 512:(tb + 1) * 512], start=(k == 0), stop=(k == 7)),
                             reads=[("wg", pb)], writes=[("ps", q)])
                    for k in range(8):
                        P.pe(lambda e, k=k, fc=fc, q=q: e.matmul(bank(2 + q), lhsT=wub[pb][:, k, fc * 128:(fc + 1) * 128],
                                                                 rhs=hT[:, k, tb * 512:(tb + 1) * 512], start=(k == 0), stop=(k == 7)),
                             reads=[("wu", pb)], writes=[("ps", 2 + q)])
                    P.act(lambda e, q=q: e.activation(out=sg[q], in_=bank(q), func=AF.Silu), reads=[("ps", q)], writes=[("sg", q)])
                    P.dve(lambda e, q=q, fc=fc: e.tensor_tensor(out=actT[ab][:, fc, :], in0=bank(2 + q), in1=sg[q], op=ALU.mult),
                          reads=[("ps", 2 + q), ("sg", q)], writes=[("actT", ab)])

            def DN(ex, tb, n):
                pb = ex % 2
                ab = n % 2
                for tt in range(4):
                    i = tb * 4 + tt
                    for hf in range(2):
                        bk = 4 + ((tt * 2 + hf) % 4)
                        for fc in range(4):
                            P.pe(lambda e, fc=fc, hf=hf, tt=tt, bk=bk: e.matmul(bank(bk), lhsT=actT[ab][:, fc, tt * 128:(tt + 1) * 128],
                                                                                rhs=wdb[pb][:, fc, hf * 512:(hf + 1) * 512],
                                                                                start=(fc == 0), stop=(fc == 3)),
                                 reads=[("actT", ab), ("wd", pb)], writes=[("ps", bk)])
                        P.dve(lambda e, hf=hf, i=i, bk=bk: e.scalar_tensor_tensor(out=x[:, i, hf * 512:(hf + 1) * 512], in0=bank(bk),
                                                                                  scalar=gates[:, i, ex:ex + 1], in1=x[:, i, hf * 512:(hf + 1) * 512],
                                                                                  op0=ALU.mult, op1=ALU.add),
                              reads=[("ps", bk), "gates", ("x", i)], writes=[("x", i)])
                    if ex == 15:
                        P.act(lambda e, i=i: e.activation(out=junkM, in_=x[:, i, :], func=AF.Square, accum_out=ssqN[:, i:i + 1]),
                              reads=[("x", i)], writes=["junkM", ("ssq", i)])

            seqs = [(ex, tb) for ex in range(16) for tb in range(4)]
            for n in range(len(seqs) + 1):
                if n < len(seqs):
                    GU(seqs[n][0], seqs[n][1], n)
                if n == 0:
                    router_phase()
                    assert ar.off <= 11 * 1024, ar.off
                    if stop == ("N2", l):
                        P.barrier(); dump_and_finish(); return True
                if n >= 1:
                    ex, tb = seqs[n - 1]
                    DN(ex, tb, n - 1)
                    if tb == 3 and ex + 2 < 16:
                        load_expert(ex + 2)
                    if tb == 3 and l == 0:
                        if 1 <= ex <= 12:
                            j = ex - 1
                            mod_chunk(1, j, j % 2, stageM[j % 2], modbrM[j % 2], mrowM[j % 2], 7, part=2)
                        if ex < 12:
                            mod_chunk(1, ex, ex % 2, stageM[ex % 2], modbrM[ex % 2], mrowM[ex % 2], 7, part=1,
                                      extra_w=["rt"] if ex < 2 else ())
            if stop == ("M", l):
                P.barrier(); dump_and_finish(); return True

            return False

        for l_ in range(n_layers):
            if layer(l_):
                return nc

        P.barrier()
        ar.reset()
        fgb = ar.f32([128, 1024])
        ob = [ar.f32([128, 1024]) for _ in range(3)]
        junk = ar.bf16([128, 1024])
        ssq = ssqN; rstd = ar.f32([128, 16])
        P.dma("sp", "n0", lambda e: e.dma_start(out=fgb, in_=fg_d[0, :].partition_broadcast(128)), writes=["fgb"])
        P.act(lambda e: e.activation(out=rstd, in_=ssq, func=AF.Sqrt, bias=eps_t[:, 0:1], scale=1.0 / 1024),
              reads=[("ssq", i) for i in range(16)] + ["eps"], writes=["rstd0"])
        P.dve(lambda e: e.reciprocal(out=rstd, in_=rstd), reads=["rstd0"], writes=["rstd"])
        for i in range(16):
            o = ob[i % 3]
            P.dve(lambda e, i=i, o=o: e.scalar_tensor_tensor(out=o, in0=x[:, i, :], scalar=rstd[:, i:i + 1], in1=fgb,
                                                             op0=ALU.mult, op1=ALU.mult),
                  reads=[("x", i), "rstd", "fgb"], writes=[("ob", i % 3)])
            P.dma("sp", "out", lambda e, i=i, o=o: e.dma_start(out=out_d[i * 128:(i + 1) * 128, :], in_=o),
                  reads=[("ob", i % 3)], writes=[("out", i)])
        if dbg:
            P.barrier(); dump_and_finish(); return nc
        P.emit(final_wait_streams=["out"])
    return nc


def t5_bucket_np(dist):
    dist = np.maximum(dist, 0)
    ratio = np.log(np.maximum(dist, 1) / 16) / np.log(2048 / 16)
    large = 16 + np.floor(ratio * 16).astype(np.int64)
    large = np.minimum(large, 31)
    return np.where(dist < 16, dist, large).astype(np.int32)


def host_consts():
    btab = np.zeros((3, 33, 384), np.float32)
    for di, (win, d) in enumerate(CONFIGS):
        for j in range(384):
            rel = j - 127
            if 0 <= rel <= 128:
                btab[di, int(t5_bucket_np(np.array(rel * d))), j] = 1.0
            else:
                btab[di, 32, j] = NEG
    ind = np.zeros((8, 512), np.float32)
    for g in range(8):
        ind[g, g * 64:(g + 1) * 64] = 1.0
    trilm = np.triu(np.ones((128, 128), np.float32))
    return dict(identf=np.eye(128, dtype=np.float32), btab=btab, ind=ind, trilm=trilm,
                jmat=np.ascontiguousarray(np.eye(128, dtype=np.float32)[::-1]))


def make_in_maps(inputs, cores):
    f = lambda a: np.ascontiguousarray(np.asarray(a, dtype=np.float32))
    shared = dict(
        rel_bias=f(inputs["rel_bias"]), router_w=f(inputs["router_w"]), router_b=f(inputs["router_b"]).reshape(1, 16),
        mod_w=f(inputs["mod_w"]), mod_b=f(inputs["mod_b"]), norm1_g=f(inputs["norm1_g"]), w_in=f(inputs["w_in"]),
        gmlp_ln_g=f(inputs["gmlp_ln_g"]), gmlp_ln_b=f(inputs["gmlp_ln_b"]), gmlp_ws=f(inputs["gmlp_ws"]),
        gmlp_bs=f(inputs["gmlp_bs"]), w_out=f(inputs["w_out"]), norm2_g=f(inputs["norm2_g"]),
        moe_w_gate=f(inputs["moe_w_gate"]), moe_w_up=f(inputs["moe_w_up"]), moe_w_down=f(inputs["moe_w_down"]),
        final_g=f(inputs["final_g"]).reshape(1, 1024),
    )
    gab = np.concatenate([f(inputs["out_norm_a_g"]), f(inputs["out_norm_b_g"])], axis=1)
    shared["gab"] = np.ascontiguousarray(gab.reshape(2, 8, 128).transpose(0, 2, 1))
    shared.update(host_consts())
    x = f(inputs["x"]); c = f(inputs["c"])
    maps = []
    for b in cores:
        m = dict(shared)
        m["x"] = np.ascontiguousarray(x[b])
        m["cT"] = np.ascontiguousarray(c[b].reshape(8, 128).T)
        maps.append(m)
    return maps


def kernel(**inputs):
    nc = build()
    maps = make_in_maps(inputs, list(range(8)))
    res = run_bass_kernel_spmd(nc, maps, core_ids=list(range(8)))
    return np.stack([np.asarray(r["out"], dtype=np.float32) for r in res.results], axis=0)
```

```python
import contextlib
import os
import numpy as np
import concourse.bass as bass
import concourse.mybir as mybir
from concourse.bass_utils import run_bass_kernel_spmd

F32 = mybir.dt.float32
BF16 = mybir.dt.bfloat16
ALU = mybir.AluOpType
AF = mybir.ActivationFunctionType
AX = mybir.AxisListType
ENGS = ("pe", "act", "dve", "pool", "sp")
EPS = 1e-6
NEG = -30000.0
CONFIGS = ((128, 1), (512, 4), (2048, 16))


class Prog:
    def __init__(self, nc):
        self.nc = nc
        self.ins = []
        self.last_w = {}
        self.readers = {}
        self.stream_cnt = {}
        self.stream_last = {}

    def add(self, eng, fn, reads=(), writes=(), dma=None):
        i = len(self.ins)
        deps = {}

        def dep(j):
            if j is None:
                return
            pj = self.ins[j]
            deps[j] = self.stream_cnt[pj["dma"]] if pj["dma"] is not None else None

        for r in reads:
            dep(self.last_w.get(r))
        for w in writes:
            dep(self.last_w.get(w))
            for j in self.readers.get(w, ()):
                dep(j)
        pruned = {}
        for j, c in deps.items():
            pj = self.ins[j]
            if pj["dma"] is None and pj["eng"] == eng:
                if eng == "pe":
                    continue
                if not any(self.last_w.get(r) == j for r in reads):
                    continue
            pruned[j] = c
            if pj["dma"] is None:
                pj["needs_inc"] = True
        rec = dict(eng=eng, fn=fn, deps=pruned, dma=dma, needs_inc=False, val=None)
        if dma is not None:
            self.stream_cnt[dma] = self.stream_cnt.get(dma, 0) + 1
            self.stream_last[dma] = i
        self.ins.append(rec)
        for r in reads:
            self.readers.setdefault(r, []).append(i)
        for w in writes:
            self.last_w[w] = i
            self.readers[w] = []
        return i

    def pe(self, fn, reads=(), writes=()):
        return self.add("pe", fn, reads, writes)

    def act(self, fn, reads=(), writes=()):
        return self.add("act", fn, reads, writes)

    def dve(self, fn, reads=(), writes=()):
        return self.add("dve", fn, reads, writes)

    def pool(self, fn, reads=(), writes=()):
        return self.add("pool", fn, reads, writes)

    def dma(self, q, stream, fn, reads=(), writes=()):
        return self.add(q, fn, reads, writes, dma=stream)

    def barrier(self):
        last = {}
        for idx, rec in enumerate(self.ins):
            if rec["dma"] is None and not rec.get("bar"):
                last[rec["eng"]] = idx
        for e in ENGS:
            deps = {}
            for e2, j in last.items():
                if e2 == e and e == "pe":
                    continue
                deps[j] = None
                self.ins[j]["needs_inc"] = True
            for s, c in self.stream_cnt.items():
                deps[self.stream_last[s]] = c
            self.ins.append(dict(eng=e, fn=None, deps=deps, dma=None, needs_inc=False, val=None, bar=True))
        self.last_w = {}
        self.readers = {}

    def emit(self, final_wait_streams=()):
        nc = self.nc
        streams = sorted(self.stream_cnt.keys())
        with contextlib.ExitStack() as es:
            esem = {e: es.enter_context(nc.semaphore("s_" + e)) for e in ENGS}
            ssem = {s: es.enter_context(nc.semaphore("d_" + str(s))) for s in streams}
            cnt = {e: 0 for e in ENGS}
            for rec in self.ins:
                if rec["dma"] is None and rec["needs_inc"]:
                    cnt[rec["eng"]] += 1
                    rec["val"] = cnt[rec["eng"]]
            per_eng = {e: [] for e in ENGS}
            for rec in self.ins:
                per_eng[rec["eng"]].append(rec)
            ins = self.ins
            block = es.enter_context(nc.Block())

            def run(ename, eng):
                waited = {}
                for rec in per_eng[ename]:
                    for j, c in rec["deps"].items():
                        pj = ins[j]
                        if pj["dma"] is not None:
                            sem, v, key = ssem[pj["dma"]], c * 16, ("d", pj["dma"])
                        else:
                            sem, v, key = esem[pj["eng"]], pj["val"], ("e", pj["eng"])
                        if waited.get(key, 0) >= v:
                            continue
                        waited[key] = v
                        eng.wait_ge(sem, v)
                    if rec["fn"] is None:
                        continue
                    bi = rec["fn"](eng)
                    if rec["dma"] is not None:
                        bi.then_inc(ssem[rec["dma"]], 16)
                    elif rec["needs_inc"]:
                        bi.then_inc(esem[ename], 1)
                if ename == "sp":
                    for s in final_wait_streams:
                        eng.wait_ge(ssem[s], self.stream_cnt[s] * 16)

            block.tensor(lambda e: run("pe", e))
            block.scalar(lambda e: run("act", e))
            block.vector(lambda e: run("dve", e))
            block.gpsimd(lambda e: run("pool", e))
            block.sync(lambda e: run("sp", e))


class Arena:
    def __init__(self, t, words):
        self.t = t
        self.words = words
        self.off = 0

    def reset(self):
        self.off = 0

    def _take(self, nwords):
        nwords = (nwords + 7) // 8 * 8
        a = self.off
        self.off += nwords
        assert self.off <= self.words, ("arena overflow", self.off, self.words)
        return a

    def f32(self, shape):
        n = int(np.prod(shape[1:]))
        a = self._take(n)
        ap = self.t[0:shape[0], a:a + n]
        return self._shape(ap, shape)

    def bf16(self, shape):
        n = int(np.prod(shape[1:]))
        a = self._take((n + 1) // 2)
        ap = self.t[0:shape[0], a:a + (n + 1) // 2].bitcast(BF16)[:, 0:n]
        return self._shape(ap, shape)

    @staticmethod
    def _shape(ap, shape):
        if len(shape) == 2:
            return ap
        if len(shape) == 3:
            return ap.rearrange("p (a b) -> p a b", a=shape[1])
        if len(shape) == 4:
            return ap.rearrange("p (a b c) -> p a b c", a=shape[1], b=shape[2])
        raise ValueError(shape)


NSB = 4
ARENA_WORDS = 25 * 1024


def build(stop=None, n_layers=2):
    nc = bass.Bass("TRN2", target_bir_lowering=False)
    din = lambda name, shape: nc.dram_tensor(name, shape, F32, kind="ExternalInput").ap()
    x_d = din("x", [2048, 1024])
    cT_d = din("cT", [128, 8])
    relb_d = din("rel_bias", [32, 8])
    rw_d = din("router_w", [1024, 16])
    rb_d = din("router_b", [1, 16])
    modw_d = din("mod_w", [2, 1024, 6144])
    modb_d = din("mod_b", [2, 6144])
    n1g_d = din("norm1_g", [2, 1024])
    win_d = din("w_in", [2, 1024, 2560])
    lng_d = din("gmlp_ln_g", [2, 512])
    lnb_d = din("gmlp_ln_b", [2, 512])
    ws_d = din("gmlp_ws", [2, 8, 128, 128])
    bs_d = din("gmlp_bs", [2, 8, 128])
    gab_d = din("gab", [2, 128, 8])
    wout_d = din("w_out", [2, 1024, 1024])
    n2g_d = din("norm2_g", [2, 1024])
    wg_d = din("moe_w_gate", [2, 16, 1024, 512])
    wu_d = din("moe_w_up", [2, 16, 1024, 512])
    wd_d = din("moe_w_down", [2, 16, 512, 1024])
    fg_d = din("final_g", [1, 1024])
    identf_d = din("identf", [128, 128])
    btab_d = din("btab", [3, 33, 384])
    ind_d = din("ind", [8, 512])
    tril_d = din("trilm", [128, 128])
    jmat_d = din("jmat", [128, 128])
    out_d = nc.dram_tensor("out", [2048, 1024], F32, kind="ExternalOutput").ap()
    modscr = nc.dram_tensor("modscr", [2, 6144], F32, kind="Internal").ap()
    gscr_h = nc.dram_tensor("gscr", [3, 8, 384], F32, kind="Internal")
    gscr = gscr_h.ap()
    texp = nc.dram_tensor("texp", [3, 8, 128, 256], F32, kind="Internal").ap()
    obT_d = nc.dram_tensor("obT_d", [128, 4, 2048], BF16, kind="Internal").ap()
    dbg = stop is not None
    if dbg:
        dbg_x = nc.dram_tensor("dbg_x", [2048, 1024], F32, kind="ExternalOutput").ap()
        dbg_hT = nc.dram_tensor("dbg_hT", [128, 8, 2048], BF16, kind="ExternalOutput").ap()
        dbg_g = nc.dram_tensor("dbg_g", [128, 256], F32, kind="ExternalOutput").ap()
        dbg_obT = nc.dram_tensor("dbg_obT", [128, 4, 2048], BF16, kind="ExternalOutput").ap()

    P = Prog(nc)
    es = contextlib.ExitStack()
    with es:
        sb = lambda name, shape, dt: es.enter_context(nc.sbuf_tensor(name, shape, dt))
        x = sb("x_sb", [128, 16, 1024], F32)
        hT = sb("hT", [128, 8, 2048], BF16)
        identf = sb("identf_sb", [128, 128], F32)
        identb = sb("identb_sb", [128, 128], BF16)
        trilm = sb("trilm_sb", [128, 128], F32)
        ind = sb("ind_sb", [8, 512], F32)
        ones_bf = sb("ones_bf", [1, 128], BF16)
        eps_t = sb("eps_t", [128, 1], F32)
        nhalf = sb("nhalf_t", [128, 1], F32)
        gates = sb("gates_sb", [128, 16, 16], F32)
        rbb = sb("rbb_sb", [128, 16, 16], F32)
        rw = sb("rw_sb", [128, 8, 16], F32)
        statA = sb("statA", [128, 64], F32)
        cact = sb("cact_sb", [128, 8], BF16)
        arena_t = sb("arena", [128, ARENA_WORDS], F32)
        PS = es.enter_context(nc.psum_tensor("ps", [128, 4096], F32))
        ar = Arena(arena_t, ARENA_WORDS)
        ar2 = Arena(arena_t, ARENA_WORDS)

        def bank(b, n=512, parts=128):
            return PS[0:parts, b * 512:b * 512 + n]

        P.dma("sp", "c0", lambda e: e.dma_start(out=identf[:], in_=identf_d), writes=["identf"])
        P.dma("sp", "c0", lambda e: e.dma_start(out=trilm[:], in_=tril_d), writes=["trilm"])
        P.dma("sp", "c0", lambda e: e.dma_start(out=ind[:], in_=ind_d), writes=["ind"])
        P.dma("sp", "c0", lambda e: e.dma_start(out=rw[:], in_=rw_d.rearrange("(k p) e -> p k e", p=128)), writes=["rw"])
        P.dma("sp", "c0", lambda e: e.dma_start(
            out=rbb[:], in_=bass.AP(rb_d.tensor, 0, [[0, 128], [0, 16], [1, 16]])), writes=["rbb"])
        P.dve(lambda e: e.tensor_copy(out=identb[:], in_=identf[:]), reads=["identf"], writes=["identb"])
        P.dve(lambda e: e.memset(ones_bf[:], 1.0), writes=["ones_bf"])
        P.dve(lambda e: e.memset(eps_t[:], EPS), writes=["eps"])
        P.dve(lambda e: e.memset(nhalf[:], -0.5), writes=["nhalf"])

        ar.reset()
        cT = ar.f32([128, 8])
        stageR = [ar.bf16([128, 3072]) for _ in range(3)]
        modbrR = ar.f32([1, 3072])
        mrowR = ar.f32([1, 3072])
        rb33 = ar.f32([33, 8])
        btab = ar.f32([33, 3, 384])
        grow = ar.f32([8, 3, 384])
        P.dma("sp", "c0", lambda e: e.dma_start(out=cT, in_=cT_d), writes=["cT"])
        P.act(lambda e: e.activation(out=cact[:], in_=cT, func=AF.Silu), reads=["cT"], writes=["cact"])
        def mod_chunk(l, j, pb, stage_, modbr_, mrow_, bk, part=3, extra_w=()):
            if part & 1:
                P.dma("pool", "mw%d" % pb, lambda e: e.dma_start(
                    out=stage_, in_=modw_d[l, :, j * 512:(j + 1) * 512].rearrange("(k p) n -> p k n", p=128)),
                    writes=[("stage", pb)] + list(extra_w))
                P.dma("sp", "mb%d" % pb, lambda e: e.dma_start(out=modbr_, in_=modb_d[l:l + 1, j * 512:(j + 1) * 512]),
                      writes=[("modbr", pb)] + list(extra_w))
            if not (part & 2):
                return
            for k in range(8):
                P.pe(lambda e, k=k: e.matmul(bank(bk, 512, 1), lhsT=cact[:, k:k + 1], rhs=stage_[:, k, :],
                                             start=(k == 0), stop=(k == 7)),
                     reads=["cact", ("stage", pb)], writes=[("ps", bk)])
            P.dve(lambda e: e.tensor_tensor(out=mrow_, in0=bank(bk, 512, 1), in1=modbr_, op=ALU.add),
                  reads=[("ps", bk), ("modbr", pb)], writes=[("mrow", pb)])
            P.dma("sp", "ms%d" % pb, lambda e: e.dma_start(out=modscr[l:l + 1, j * 512:(j + 1) * 512], in_=mrow_),
                  reads=[("mrow", pb)], writes=[("modscr", l)])

        for i in range(16):
            P.dma("act", "x", lambda e, i=i: e.dma_start(out=x[:, i, :], in_=x_d[i * 128:(i + 1) * 128, :]),
                  writes=[("x", i)])
        P.dve(lambda e: e.memset(rb33, 1.0), writes=["rb33"])
        P.dma("sp", "c1", lambda e: e.dma_start(out=rb33[0:32, :], in_=relb_d), writes=["rb33"])
        P.dma("sp", "c1", lambda e: e.dma_start(out=btab, in_=btab_d.rearrange("d r j -> r d j")), writes=["btab"])
        jmat = ar.f32([128, 128])
        thk = [ar.f32([128, 8, 256]) for _ in range(3)]
        tfx = [ar.f32([128, 8, 256]) for _ in range(2)]
        P.dma("sp", "c1", lambda e: e.dma_start(out=jmat, in_=jmat_d), writes=["jmat"])
        for d in range(3):
            P.pe(lambda e, d=d: e.matmul(bank(6 + d % 2, 384, 8), lhsT=rb33[:, :], rhs=btab[:, d, :], start=True, stop=True),
                 reads=["rb33", "btab"], writes=[("ps", 6 + d % 2)])
            P.dve(lambda e, d=d: e.tensor_copy(out=grow[:, d, :], in_=bank(6 + d % 2, 384, 8)),
                  reads=[("ps", 6 + d % 2)], writes=["grow"])
        P.dma("act", "c2", lambda e: e.dma_start(out=gscr.rearrange("d h j -> h d j"), in_=grow),
              reads=["grow"], writes=["gscr"])
        for d in range(3):
            P.dma("act", "tk%d" % d, lambda e, d=d: e.dma_start(
                out=thk[d], in_=bass.AP(gscr_h, d * 8 * 384, [[1, 128], [384, 8], [1, 256]])),
                reads=["gscr"], writes=[("thk", d)])

        def texp_flip():
            nj = 0
            for d in range(3):
                pb = d % 2
                for j in range(4):
                    bk = 6 + nj % 2
                    nj += 1
                    P.pe(lambda e, d=d, j=j, bk=bk: e.matmul(bank(bk), lhsT=jmat, rhs=thk[d][:, 2 * j:2 * j + 2, :].rearrange("p a b -> p (a b)"),
                                                             start=True, stop=True),
                         reads=["jmat", ("thk", d)], writes=[("ps", bk)])
                    P.dve(lambda e, pb=pb, j=j, bk=bk: e.tensor_copy(out=tfx[pb][:, 2 * j:2 * j + 2, :].rearrange("p a b -> p (a b)"), in_=bank(bk)),
                          reads=[("ps", bk)], writes=[("tfx", pb)])
                P.dma("act", "tx%d" % pb, lambda e, d=d, pb=pb: e.dma_start(out=texp[d].rearrange("h p q -> p h q"), in_=tfx[pb]),
                      reads=[("tfx", pb)], writes=["texp"])

        nblk = 0
        for hh in range(2):
            P.dma("sp", "mb0", lambda e, hh=hh: e.dma_start(out=modbrR, in_=modb_d[0:1, hh * 3072:(hh + 1) * 3072]),
                  writes=["modbrR"])
            for k in range(8):
                sb_ = nblk % 3
                nblk += 1
                P.dma("pool", "mr%d" % sb_, lambda e, hh=hh, k=k, sb_=sb_: e.dma_start(
                    out=stageR[sb_], in_=modw_d[0, k * 128:(k + 1) * 128, hh * 3072:(hh + 1) * 3072]),
                    writes=[("stageR", sb_)])
                for j in range(6):
                    P.pe(lambda e, k=k, j=j, sb_=sb_: e.matmul(bank(j, 512, 1), lhsT=cact[:, k:k + 1], rhs=stageR[sb_][:, j * 512:(j + 1) * 512],
                                                               start=(k == 0), stop=(k == 7)),
                         reads=["cact", ("stageR", sb_)], writes=[("ps", j)])
            for j in range(6):
                P.dve(lambda e, j=j: e.tensor_tensor(out=mrowR[:, j * 512:(j + 1) * 512], in0=bank(j, 512, 1),
                                                     in1=modbrR[:, j * 512:(j + 1) * 512], op=ALU.add),
                      reads=[("ps", j), "modbrR"], writes=["mrowR"])
            P.dma("sp", "ms0", lambda e, hh=hh: e.dma_start(out=modscr[0:1, hh * 3072:(hh + 1) * 3072], in_=mrowR),
                  reads=["mrowR"], writes=[("modscr", 0)])
            if hh == 0:
                texp_flip()

        ssqN = statA[:, 16:32]

        def norm_phase(l, which, pre=None, post=None, have_ssq=False):
            P.barrier()
            ar.reset()
            if pre is not None:
                pre()
            gmod = ar.f32([128, 1024])
            ngb = ar.f32([128, 1024])
            shb = ar.f32([128, 1024])
            h32 = [ar.f32([128, 1024]) for _ in range(2)]
            junk = ar.bf16([128, 1024])
            ssq = ssqN
            rstd = ar.f32([128, 16])
            xh32 = [ar.f32([128, 8, 128]) for _ in range(2)] if which == 2 else None
            off_sh, off_sc = (0, 1024) if which == 1 else (3072, 4096)
            ng_d = n1g_d if which == 1 else n2g_d
            P.dma("sp", "n0", lambda e: e.dma_start(out=gmod, in_=modscr[l, off_sc:off_sc + 1024].partition_broadcast(128)),
                  writes=["gmod"])
            P.dma("sp", "n0", lambda e: e.dma_start(out=ngb, in_=ng_d[l, :].partition_broadcast(128)), writes=["ngb"])
            P.dma("sp", "n0", lambda e: e.dma_start(out=shb, in_=modscr[l, off_sh:off_sh + 1024].partition_broadcast(128)),
                  writes=["shb"])
            P.dve(lambda e: e.scalar_tensor_tensor(out=gmod, in0=gmod, scalar=1.0, in1=ngb, op0=ALU.add, op1=ALU.mult),
                  reads=["gmod", "ngb"], writes=["gmod"])
            if not have_ssq:
                for i in range(16):
                    P.act(lambda e, i=i: e.activation(out=junk, in_=x[:, i, :], func=AF.Square, accum_out=ssq[:, i:i + 1]),
                          reads=[("x", i)], writes=["junk", ("ssq", i)])
            P.act(lambda e: e.activation(out=rstd, in_=ssq, func=AF.Sqrt, bias=eps_t[:, 0:1], scale=1.0 / 1024),
                  reads=[("ssq", i) for i in range(16)] + ["eps"], writes=["rstd0"])
            P.dve(lambda e: e.reciprocal(out=rstd, in_=rstd), reads=["rstd0"], writes=["rstd"])
            def stage1(i):
                hb = h32[i % 2]
                pp = i % 2
                P.dve(lambda e: e.scalar_tensor_tensor(out=hb, in0=x[:, i, :], scalar=rstd[:, i:i + 1], in1=gmod,
                                                       op0=ALU.mult, op1=ALU.mult),
                      reads=[("x", i), "rstd", "gmod"], writes=[("h32", pp)])
                P.dve(lambda e: e.tensor_tensor(out=hb, in0=hb, in1=shb, op=ALU.add),
                      reads=[("h32", pp), "shb"], writes=[("h32", pp)])
                for c in range(8):
                    P.pe(lambda e, c=c: e.transpose(out=PS[:, pp * 1024 + c * 128:pp * 1024 + (c + 1) * 128],
                                                    in_=hb[:, c * 128:(c + 1) * 128], identity=identf[:]),
                         reads=[("h32", pp), "identf"], writes=[("ps", 2 * pp + c // 4)])

            def stage2(i):
                pp = i % 2
                if which == 1:
                    P.act(lambda e: e.activation(out=hT[:, :, i * 128:(i + 1) * 128],
                                                 in_=PS[:, pp * 1024:(pp + 1) * 1024].rearrange("p (c t) -> p c t", c=8),
                                                 func=AF.Copy),
                          reads=[("ps", 2 * pp), ("ps", 2 * pp + 1)], writes=[("hT", i)])
                if which == 2:
                    xb = xh32[pp]
                    P.dve(lambda e: e.tensor_copy(out=xb, in_=PS[:, pp * 1024:(pp + 1) * 1024].rearrange("p (c t) -> p c t", c=8)),
                          reads=[("ps", 2 * pp), ("ps", 2 * pp + 1)], writes=[("xh32", pp)])
                    P.act(lambda e: e.activation(out=hT[:, :, i * 128:(i + 1) * 128], in_=xb, func=AF.Copy),
                          reads=[("xh32", pp)], writes=[("hT", i)])
                    for c in range(8):
                        P.pe(lambda e, c=c: e.matmul(PS[:, 4 * 512 + i * 16:4 * 512 + (i + 1) * 16], lhsT=xb[:, c, :],
                                                     rhs=rw[:, c, :], start=(c == 0), stop=(c == 7)),
                             reads=[("xh32", pp), "rw"], writes=[("ps", 4)])

            for s in range(17):
                if s < 16:
                    stage1(s)
                if s >= 1:
                    stage2(s - 1)

        def norm_phase_post(post):
            if post is not None:
                post()

        def router_phase():
            T = lambda: ar.f32([128, 16, 16])
            L, E_, pr, sel, t1, t2, t3, selm = T(), T(), T(), T(), T(), T(), T(), T()
            mx = ar.f32([128, 16]); sm = ar.f32([128, 16])
            g4 = ar.f32([128, 16, 4]); p6 = [ar.f32([128, 16, 4]) for _ in range(6)]
            gmx = ar.f32([128, 16]); gm = ar.f32([128, 16, 4])
            m1 = ar.f32([128, 16]); m2 = ar.f32([128, 16])
            bc = lambda a, n: a.unsqueeze(2).to_broadcast([128, 16, n])
            v = lambda e: e
            P.dve(lambda e: e.tensor_copy(out=L, in_=PS[:, 2048:2048 + 256].rearrange("p (a b) -> p a b", a=16)),
                  reads=[("ps", 4)], writes=["rt"])
            seq = []
            seq.append(lambda e: e.tensor_reduce(out=mx, in_=L, axis=AX.X, op=ALU.max))
            seq.append(lambda e: e.tensor_tensor(out=t1, in0=L, in1=bc(mx, 16), op=ALU.subtract))
            for f in seq:
                P.dve(f, reads=["rt"], writes=["rt"])
            P.act(lambda e: e.activation(out=E_, in_=t1, func=AF.Exp), reads=["rt"], writes=["rt"])
            seq = []
            seq.append(lambda e: e.tensor_reduce(out=sm, in_=E_, axis=AX.X, op=ALU.add))
            seq.append(lambda e: e.reciprocal(out=sm, in_=sm))
            seq.append(lambda e: e.tensor_tensor(out=pr, in0=E_, in1=bc(sm, 16), op=ALU.mult))
            seq.append(lambda e: e.tensor_tensor(out=sel, in0=pr, in1=rbb[:], op=ALU.add))
            s4 = sel.rearrange("p a (g k) -> p a g k", k=4)
            pairs = [(0, 1), (0, 2), (0, 3), (1, 2), (1, 3), (2, 3)]
            for q, (a, b) in enumerate(pairs):
                seq.append(lambda e, q=q, a=a, b=b: e.tensor_tensor(out=p6[q], in0=s4[:, :, :, a], in1=s4[:, :, :, b], op=ALU.add))
            seq.append(lambda e: e.tensor_tensor(out=g4, in0=p6[0], in1=p6[1], op=ALU.max))
            for q in range(2, 6):
                seq.append(lambda e, q=q: e.tensor_tensor(out=g4, in0=g4, in1=p6[q], op=ALU.max))
            seq.append(lambda e: e.tensor_reduce(out=gmx, in_=g4, axis=AX.X, op=ALU.max))
            seq.append(lambda e: e.tensor_tensor(out=gm, in0=g4, in1=bc(gmx, 4), op=ALU.is_ge))
            gm16 = gm.unsqueeze(3).to_broadcast([128, 16, 4, 4])
            sm4 = selm.rearrange("p a (g k) -> p a g k", k=4)
            seq.append(lambda e: e.scalar_tensor_tensor(out=sm4, in0=s4, scalar=100.0, in1=gm16, op0=ALU.add, op1=ALU.mult))
            seq.append(lambda e: e.tensor_scalar(out=selm, in0=selm, scalar1=-100.0, scalar2=None, op0=ALU.add))
            seq.append(lambda e: e.tensor_reduce(out=m1, in_=selm, axis=AX.X, op=ALU.max))
            seq.append(lambda e: e.tensor_tensor(out=t2, in0=selm, in1=bc(m1, 16), op=ALU.is_ge))
            seq.append(lambda e: e.scalar_tensor_tensor(out=t3, in0=t2, scalar=-1000.0, in1=selm, op0=ALU.mult, op1=ALU.add))
            seq.append(lambda e: e.tensor_reduce(out=m2, in_=t3, axis=AX.X, op=ALU.max))
            seq.append(lambda e: e.tensor_tensor(out=t2, in0=selm, in1=bc(m2, 16), op=ALU.is_ge))
            seq.append(lambda e: e.tensor_tensor(out=t3, in0=t2, in1=pr, op=ALU.mult))
            seq.append(lambda e: e.tensor_reduce(out=sm, in_=t3, axis=AX.X, op=ALU.add))
            seq.append(lambda e: e.reciprocal(out=sm, in_=sm))
            seq.append(lambda e: e.tensor_tensor(out=gates[:], in0=t3, in1=bc(sm, 16), op=ALU.mult))
            for f in seq:
                P.dve(f, reads=["rt", "rbb"], writes=["rt", "gates"])

        def dump_and_finish():
            for i in range(16):
                P.dma("sp", "dbg", lambda e, i=i: e.dma_start(out=dbg_x[i * 128:(i + 1) * 128, :], in_=x[:, i, :]),
                      reads=[("x", i)])
            P.dma("sp", "dbg", lambda e: e.dma_start(out=dbg_hT, in_=hT[:]), reads=[("hT", i) for i in range(16)])
            P.dma("sp", "dbg", lambda e: e.dma_start(out=dbg_g, in_=gates[:].rearrange("p a b -> p (a b)")), reads=["gates"])
            P.emit(final_wait_streams=["dbg"])

        def layer(l):
            ar2.reset()
            vball = ar2.bf16([128, 16, 512])
            gub = ar2.bf16([128, 16, 512])
            w_uva = ar2.bf16([128, 8, 1024])
            w_oa = ar2.bf16([128, 4, 1024])
            WT = ar2.bf16([128, 8, 128])
            oaT = [ar2.bf16([128, 4, 128]) for _ in range(2)]
            g1b = ar2.f32([128, 1024])
            Wld = ar2.f32([128, 8, 128])
            lngb = ar2.f32([128, 512]); lnbb = ar2.f32([128, 512])
            gv = [ar2.f32([128, 512]) for _ in range(2)]
            vn = [ar2.f32([128, 512]) for _ in range(2)]
            oa = [ar2.f32([128, 512]) for _ in range(2)]
            junkA = ar2.bf16([128, 512])
            bsT = ar2.f32([8, 128])
            gabf = ar2.f32([128, 8])
            st6 = ar2.f32([128, 2, 6]); mv = ar2.f32([128, 2, 2]); vpe = ar2.f32([128, 2]); rsv = ar2.f32([128, 2])
            ssqa = ar2.f32([128, 16]); rsa = ar2.f32([128, 16])
            assert vball is not None

            def A_pre():
                for h2 in range(2):
                    P.dma("pool", "wA", lambda e, h2=h2: e.dma_start(
                        out=w_uva[:, :, h2 * 512:(h2 + 1) * 512],
                        in_=win_d[l, :, h2 * 512:(h2 + 1) * 512].rearrange("(k p) n -> p k n", p=128)), writes=[("w_uva", h2)])
                P.dma("pool", "wA", lambda e: e.dma_start(out=w_oa, in_=wout_d[l, 0:512, :].rearrange("(k p) n -> p k n", p=128)),
                      writes=["w_oa"])
                P.dma("sp", "a0", lambda e: e.dma_start(out=g1b, in_=modscr[l, 2048:3072].partition_broadcast(128)), writes=["g1b"])
                P.dma("sp", "a0", lambda e: e.dma_start(out=gabf, in_=gab_d[l]), writes=["gabf"])
                P.dma("sp", "a0", lambda e: e.dma_start(out=Wld, in_=ws_d[l].rearrange("g t s -> t g s")), writes=["Wld"])
                P.dma("sp", "a0", lambda e: e.dma_start(out=bsT, in_=bs_d[l]), writes=["bsT"])
                P.dma("sp", "a0", lambda e: e.dma_start(out=lngb, in_=lng_d[l, :].partition_broadcast(128)), writes=["lngb"])
                P.dma("sp", "a0", lambda e: e.dma_start(out=lnbb, in_=lnb_d[l, :].partition_broadcast(128)), writes=["lnbb"])

            def A_post():
                for c in range(4):
                    P.dve(lambda e, c=c: e.scalar_tensor_tensor(out=w_oa[:, c, :], in0=w_oa[:, c, :], scalar=gabf[:, c:c + 1], in1=g1b,
                                                                 op0=ALU.mult, op1=ALU.mult),
                          reads=["w_oa", "gabf", "g1b"], writes=["w_oa"])
                for g in range(8):
                    P.pe(lambda e, g=g: e.transpose(out=PS[:, 3072 + g * 128:3072 + (g + 1) * 128], in_=Wld[:, g, :], identity=identf[:]),
                         reads=["Wld", "identf"], writes=[("ps", 6 + g // 4)])
                for g in range(8):
                    P.dve(lambda e, g=g: e.tensor_tensor(out=WT[:, g, :], in0=PS[:, 3072 + g * 128:3072 + (g + 1) * 128], in1=trilm[:], op=ALU.mult),
                          reads=[("ps", 6 + g // 4), "trilm"], writes=["WT"])


            norm_phase(l, 1, pre=A_pre, have_ssq=(l > 0))
            assert ar.off <= 8 * 1024, ar.off
            A_post()
            if stop == ("N1", l):
                P.barrier(); dump_and_finish(); return True

            P.barrier()
            def A1(i):
                pp = i % 2
                for k in range(8):
                    P.pe(lambda e, k=k: e.matmul(bank(pp), lhsT=hT[:, k, i * 128:(i + 1) * 128], rhs=w_uva[:, k, 0:512],
                                                 start=(k == 0), stop=(k == 7)),
                         reads=[("hT", i), ("w_uva", 0)], writes=[("ps", pp)])
                for k in range(8):
                    P.pe(lambda e, k=k: e.matmul(bank(2 + pp), lhsT=hT[:, k, i * 128:(i + 1) * 128], rhs=w_uva[:, k, 512:1024],
                                                 start=(k == 0), stop=(k == 7)),
                         reads=[("hT", i), ("w_uva", 1)], writes=[("ps", 2 + pp)])
                P.act(lambda e: e.activation(out=gub[:, i, :], in_=bank(pp), func=AF.Gelu), reads=[("ps", pp)], writes=[("gub", i)])
                P.act(lambda e: e.activation(out=gv[pp], in_=bank(2 + pp), func=AF.Gelu), reads=[("ps", 2 + pp)], writes=[("gv", pp)])
                P.dve(lambda e: e.bn_stats(out=st6[:, pp, :], in_=gv[pp]), reads=[("gv", pp)], writes=[("st6", pp)])
                P.dve(lambda e: e.bn_aggr(out=mv[:, pp, :], in_=st6[:, pp, :]), reads=[("st6", pp)], writes=[("mv", pp)])
                P.dve(lambda e: e.tensor_scalar(out=vpe[:, pp:pp + 1], in0=mv[:, pp, 1:2], scalar1=EPS, scalar2=None, op0=ALU.add),
                      reads=[("mv", pp)], writes=[("vpe", pp)])
                P.pool(lambda e: e.tensor_tensor(out=rsv[:, pp:pp + 1], in0=vpe[:, pp:pp + 1], in1=nhalf[:, 0:1], op=ALU.pow),
                       reads=[("vpe", pp), "nhalf"], writes=[("rsv", pp)])

            def A1b(i):
                pp = i % 2
                P.dve(lambda e: e.tensor_scalar(out=vn[pp], in0=gv[pp], scalar1=mv[:, pp, 0:1], scalar2=rsv[:, pp:pp + 1],
                                                op0=ALU.subtract, op1=ALU.mult),
                      reads=[("gv", pp), ("mv", pp), ("rsv", pp)], writes=[("vn", pp)])
                P.dve(lambda e: e.tensor_tensor(out=vn[pp], in0=vn[pp], in1=lngb, op=ALU.mult),
                      reads=[("vn", pp), "lngb"], writes=[("vn", pp)])
                P.dve(lambda e: e.tensor_tensor(out=vball[:, i, :], in0=vn[pp], in1=lnbb, op=ALU.add),
                      reads=[("vn", pp), "lnbb"], writes=[("vball", i)])

            def A2(i):
                pp = i % 2
                P.pe(lambda e: e.matmul(bank(pp), lhsT=bsT[:, :], rhs=ind[:, :], start=True, stop=False),
                     reads=["bsT", "ind"], writes=[("ps", pp)])
                for g in range(8):
                    P.pe(lambda e, g=g: e.matmul(PS[:, pp * 512 + g * 64:pp * 512 + (g + 1) * 64], lhsT=WT[:, g, :],
                                                 rhs=vball[:, i, g * 64:(g + 1) * 64], start=False, stop=(g == 7)),
                         reads=["WT", ("vball", i)], writes=[("ps", pp)])
                P.dve(lambda e: e.tensor_tensor(out=oa[pp], in0=bank(pp), in1=gub[:, i, :], op=ALU.mult),
                      reads=[("ps", pp), ("gub", i)], writes=[("oa", pp)])
                P.act(lambda e: e.activation(out=junkA, in_=oa[pp], func=AF.Square, accum_out=ssqa[:, i:i + 1]),
                      reads=[("oa", pp)], writes=["junkA", ("ssqa", i)])
                P.dve(lambda e: e.tensor_scalar(out=rsa[:, i:i + 1], in0=ssqa[:, i:i + 1], scalar1=1.0 / 512, scalar2=EPS,
                                                op0=ALU.mult, op1=ALU.add),
                      reads=[("ssqa", i)], writes=[("rsa0", i)])
                P.pool(lambda e: e.tensor_tensor(out=rsa[:, i:i + 1], in0=rsa[:, i:i + 1], in1=nhalf[:, 0:1], op=ALU.pow),
                       reads=[("rsa0", i), "nhalf"], writes=[("rsa", i)])

            def A2b(i):
                pp = i % 2
                for c in range(4):
                    P.pe(lambda e, c=c: e.transpose(out=PS[:, (2 + pp) * 512 + c * 128:(2 + pp) * 512 + (c + 1) * 128],
                                                    in_=oa[pp][:, c * 128:(c + 1) * 128], identity=identf[:]),
                         reads=[("oa", pp), "identf"], writes=[("ps", 2 + pp)])
                P.act(lambda e: e.activation(out=oaT[pp], in_=bank(2 + pp).rearrange("p (c t) -> p c t", c=4), func=AF.Copy),
                      reads=[("ps", 2 + pp)], writes=[("oaT", pp)])

            def A3(i):
                pp = i % 2
                for hf in range(2):
                    bk = 4 + 2 * pp + hf
                    for c in range(4):
                        P.pe(lambda e, c=c, hf=hf, bk=bk: e.matmul(bank(bk), lhsT=oaT[pp][:, c, :], rhs=w_oa[:, c, hf * 512:(hf + 1) * 512],
                                                                   start=(c == 0), stop=(c == 3)),
                             reads=[("oaT", pp), "w_oa"], writes=[("ps", bk)])
                    P.dve(lambda e, hf=hf, bk=bk: e.scalar_tensor_tensor(out=x[:, i, hf * 512:(hf + 1) * 512], in0=bank(bk),
                                                                         scalar=rsa[:, i:i + 1], in1=x[:, i, hf * 512:(hf + 1) * 512],
                                                                         op0=ALU.mult, op1=ALU.add),
                          reads=[("ps", bk), ("rsa", i), ("x", i)], writes=[("x", i)])

            for i in range(17):
                if i < 16:
                    A1(i)
                if i >= 1:
                    A1b(i - 1)
            for s in range(18):
                if s < 16:
                    A2(s)
                if 0 <= s - 1 < 16:
                    A2b(s - 1)
                if 0 <= s - 2 < 16:
                    A3(s - 2)
            if stop == ("A", l):
                P.barrier(); dump_and_finish(); return True

            P.barrier()
            ar.reset()
            ost = [ar.bf16([128, 1024]) for _ in range(2)]
            wq = ar.bf16([128, 8, 128]); wk = ar.bf16([128, 8, 128]); wv = ar.bf16([128, 8, 128])
            QTd = [[ar.bf16([128, 2048]) for _ in range(3)] for _ in range(2)]
            KTd = [ar.bf16([128, 2048]) for _ in range(3)]
            VT = ar.bf16([128, 2048])
            Vb = [ar.bf16([128, 16, 2, 65]) for _ in range(3)]
            Pt = [ar.bf16([128, 512]) for _ in range(NSB)]
            obtok = ar.bf16([128, 16, 128])
            Tt = [[ar.f32([128, 512]) for _ in range(3)] for _ in range(2)]
            E32 = [ar.f32([128, 512]) for _ in range(NSB)]
            accS = [ar.f32([65, 512]) for _ in range(2)]
            rden = ar.f32([128, 4])
            ssqb = ar.f32([128, 16, 4]); rsb = statA[:, 0:16]
            junkB = ar.bf16([128, 128])
            b_end = ar.off
            for d in range(3):
                P.dve(lambda e, d=d: e.memset(Vb[d][:, :, :, 64:65], 1.0), writes=[("Vb", d)])
            for di in range(3):
                P.dve(lambda e, di=di: e.memset(QTd[0][di][64:128, :], 0.0), writes=["qz0"])
                P.dve(lambda e, di=di: e.memset(QTd[1][di][0:64, :], 0.0), writes=["qz1"])
            def B_proj(hp):
                for nm, wt, col in (("wq", wq, 1024), ("wk", wk, 1536), ("wv", wv, 2048)):
                    P.dma("pool", "wB", lambda e, wt=wt, col=col: e.dma_start(
                        out=wt, in_=win_d[l, :, col + hp * 128:col + (hp + 1) * 128].rearrange("(k p) n -> p k n", p=128)),
                        writes=[nm])
                nbk = 0
                for tb in range(4):
                    for nm, wt in (("wq", wq), ("wk", wk), ("wv", wv)):
                        bk = 6 + (nbk % 2)
                        nbk += 1
                        for k in range(8):
                            P.pe(lambda e, k=k, wt=wt, bk=bk, tb=tb: e.matmul(bank(bk), lhsT=wt[:, k, :], rhs=hT[:, k, tb * 512:(tb + 1) * 512],
                                                                              start=(k == 0), stop=(k == 7)),
                                 reads=[nm], writes=[("ps", bk)])
                        if nm == "wk":
                            P.act(lambda e, bk=bk, tb=tb: e.activation(out=KTd[0][:, tb * 512:(tb + 1) * 512], in_=bank(bk), func=AF.Copy),
                                  reads=[("ps", bk)], writes=[("wkT", tb)])
                        elif nm == "wv":
                            P.dve(lambda e, bk=bk, tb=tb: e.tensor_copy(out=VT[:, tb * 512:(tb + 1) * 512], in_=bank(bk)),
                                  reads=[("ps", bk)], writes=[("VT", tb)])
                        else:
                            for hh in range(2):
                                P.act(lambda e, bk=bk, tb=tb, hh=hh: e.activation(
                                    out=QTd[hh][0][64 * hh:64 * hh + 64, tb * 512:(tb + 1) * 512],
                                    in_=PS[64 * hh:64 * hh + 64, bk * 512:(bk + 1) * 512], func=AF.Copy, scale=0.125),
                                    reads=[("ps", bk)], writes=[("wqT", tb)])
                def deint(t, d):
                    return t.rearrange("p (r i) -> p r i", r=d), None
                for di in (1, 2):
                    d = CONFIGS[di][1]
                    P.pool(lambda e, di=di, d=d: e.tensor_copy(out=KTd[di].rearrange("p (r i) -> p r i", r=d),
                                                               in_=KTd[0].rearrange("p (i r) -> p r i", r=d)),
                           reads=[("wkT", j) for j in range(4)], writes=[("wkTd", di)])
                    P.pool(lambda e, di=di, d=d: e.tensor_copy(out=QTd[0][di][0:64, :].rearrange("p (r i) -> p r i", r=d),
                                                               in_=QTd[0][0][0:64, :].rearrange("p (i r) -> p r i", r=d)),
                           reads=[("wqT", j) for j in range(4)], writes=[("wqTd", 0, di)])
                for di in (1, 2):
                    d = CONFIGS[di][1]
                    P.pool(lambda e, di=di, d=d: e.tensor_copy(out=QTd[1][di][64:128, :].rearrange("p (r i) -> p r i", r=d),
                                                               in_=QTd[1][0][64:128, :].rearrange("p (i r) -> p r i", r=d)),
                           reads=[("wqT", j) for j in range(4)], writes=[("wqTd", 1, di)])
                for di, (win, d) in enumerate(CONFIGS):
                    nb = 16 // d
                    for q8 in range(2):
                        bk = 6 + (nbk % 2)
                        nbk += 1
                        pbf = bank(bk).bitcast(BF16)
                        for t8 in range(8):
                            tile = q8 * 8 + t8
                            r, m = tile // nb, tile % nb
                            t0 = r + d * 128 * m
                            P.pe(lambda e, t0=t0, d=d, pbf=pbf, t8=t8: e.transpose(
                                out=pbf[:, t8 * 128:(t8 + 1) * 128], in_=VT[:, t0:t0 + d * 127 + 1:d], identity=identb[:]),
                                reads=[("VT", j) for j in range(4)] + ["identb"], writes=[("ps", bk)])
                        P.act(lambda e, di=di, q8=q8, pbf=pbf: e.activation(
                            out=Vb[di][:, q8 * 8:(q8 + 1) * 8, :, 0:64],
                            in_=pbf.rearrange("p (a h e) -> p a h e", a=8, h=2), func=AF.Copy),
                            reads=[("ps", bk)], writes=[("Vb", di)])

            def B_head(hp, hl, base):
                h = 2 * hp + hl
                hpar = h % 2
                p0 = 64 * hl
                for di in range(3):
                    P.dma("sp", "tt%d" % hpar, lambda e, di=di: e.dma_start(
                        out=Tt[hpar][di].rearrange("p (a b) -> p a b", a=2),
                        in_=bass.AP(texp.tensor, (di * 8 + h) * 128 * 256, [[256, 128], [0, 2], [1, 256]])),
                        writes=[("Tt", hpar, di)])
                started = [False] * 4
                steps = []
                for di, (win, d) in enumerate(CONFIGS):
                    nb = 16 // d
                    if nb >= 2:
                        for r in range(d):
                            for m in range(0, nb, 2):
                                steps.append((di, d, nb, [(r, m, 256, 0), (r, m + 1, 256 if m + 2 < nb else 128, 256)]))
                    else:
                        for r in range(0, d, 2):
                            steps.append((di, d, nb, [(r, 0, 128, 0), (r + 1, 0, 128, 256)]))

                def region(ap512, subs):
                    if subs[0][2] == 256:
                        return ap512[:, 0:256 + subs[1][2]]
                    return ap512.rearrange("p (a b) -> p a b", a=2)[:, :, 0:128]

                def S_step(st, sidx):
                    di, d, nb, subs = st
                    sb_ = sidx % NSB
                    bk = 4 + sb_
                    for (r, m, width, coff) in subs:
                        bs0 = r * (2048 // d) + 128 * m
                        P.pe(lambda e, bs0=bs0, width=width, coff=coff: e.matmul(
                            PS[:, bk * 512 + coff:bk * 512 + coff + width], lhsT=KTd[di][:, bs0:bs0 + 128],
                            rhs=QTd[hl][di][:, bs0:bs0 + width], start=True, stop=True),
                            reads=[("wkT", j) for j in range(4)] + [("wqT", j) for j in range(4)] +
                            ([("wkTd", di), ("wqTd", hl, di)] if di > 0 else []), writes=[("ps", bk)])
                    P.dve(lambda e: e.tensor_tensor(out=region(E32[sb_], subs), in0=region(bank(bk), subs),
                                                    in1=region(Tt[hpar][di], subs), op=ALU.add),
                          reads=[("ps", bk), ("Tt", hpar, di)], writes=[("E32", sb_)])
                    P.act(lambda e: e.activation(out=region(Pt[sb_], subs), in_=region(E32[sb_], subs), func=AF.Exp),
                          reads=[("E32", sb_)], writes=[("Pt", sb_)])

                def PV_step(st, sidx):
                    di, d, nb, subs = st
                    sb_ = sidx % NSB
                    for (r, m, width, coff) in subs:
                        tile = r * nb + m
                        lhs = Vb[di][:, tile, hl, :]
                        for qb in ((m, m + 1) if m + 1 < nb else (m,)):
                            c0 = coff + (qb - m) * 128
                            if d < 16:
                                if d == 1:
                                    bk, cs, step = qb // 4, (qb % 4) * 128, 1
                                else:
                                    bk, cs, step = qb, r, 4
                                pieces = [(bk, cs, step, 128, c0)]
                            else:
                                pieces = [(b4, r, 16, 32, c0 + 32 * b4) for b4 in range(4)]
                            for (bk, cs, step, cnt, pc) in pieces:
                                st_flag = not started[bk]
                                started[bk] = True
                                P.pe(lambda e, bk=bk, cs=cs, step=step, cnt=cnt, pc=pc, st_flag=st_flag, lhs=lhs: e.matmul(
                                    PS[0:65, bk * 512 + cs:bk * 512 + cs + step * (cnt - 1) + 1:step], lhsT=lhs,
                                    rhs=Pt[sb_][:, pc:pc + cnt], start=st_flag, stop=False, skip_group_check=True),
                                    reads=[("Vb", di), ("Pt", sb_)], writes=[("ps", bk)])

                SK = NSB - 1
                for j in range(len(steps) + SK):
                    if j < len(steps):
                        S_step(steps[j], base + j)
                    if j - SK >= 0:
                        PV_step(steps[j - SK], base + j - SK)
                if stop == ("Bs", l):
                    return len(steps)
                for b4 in range(4):
                    ab = accS[b4 % 2]
                    P.act(lambda e, b4=b4, ab=ab: e.activation(out=ab, in_=bank(b4, 512, 65), func=AF.Copy),
                          reads=[("ps", b4)], writes=[("accS", b4 % 2)])
                    bk = 6 + (b4 % 2)
                    for j in range(4):
                        P.pe(lambda e, j=j, ab=ab, bk=bk: e.transpose(out=PS[:, bk * 512 + j * 65:bk * 512 + (j + 1) * 65],
                                                                      in_=ab[:, j * 128:(j + 1) * 128], identity=identf[0:65, 0:65]),
                             reads=[("accS", b4 % 2), "identf"], writes=[("ps", bk)])
                    pv = bank(bk, 260).rearrange("p (j e) -> p j e", j=4)
                    P.dve(lambda e, pv=pv: e.reciprocal(out=rden.unsqueeze(2), in_=pv[:, :, 64:65]),
                          reads=[("ps", bk)], writes=["rden"])
                    P.dve(lambda e, pv=pv, b4=b4: e.tensor_tensor(out=obtok[:, b4 * 4:(b4 + 1) * 4, p0:p0 + 64], in0=pv[:, :, 0:64],
                                                                  in1=rden.unsqueeze(2).to_broadcast([128, 4, 64]), op=ALU.mult),
                          reads=[("ps", bk), "rden"], writes=[("obtok", hl)])
                return len(steps)

            def B_pair_end(hp):
                for i in range(16):
                    P.act(lambda e, i=i: e.activation(out=junkB, in_=obtok[:, i, :], func=AF.Square, accum_out=ssqb[:, i, hp:hp + 1]),
                          reads=[("obtok", 0), ("obtok", 1)], writes=["junkB", ("ssqb", hp)])
                for i8 in range(2):
                    bk = 6 + i8
                    pbf = bank(bk).bitcast(BF16)
                    for j in range(8):
                        i = i8 * 8 + j
                        P.pe(lambda e, i=i, j=j, pbf=pbf: e.transpose(out=pbf[:, j * 128:(j + 1) * 128], in_=obtok[:, i, :], identity=identb[:]),
                             reads=[("obtok", 0), ("obtok", 1), "identb"], writes=[("ps", bk)])
                    P.act(lambda e, i8=i8, pbf=pbf: e.activation(out=ost[i8], in_=pbf, func=AF.Copy),
                          reads=[("ps", bk)], writes=[("ost", i8)])
                    P.dma("sp", "ob", lambda e, i8=i8: e.dma_start(out=obT_d[:, hp, i8 * 1024:(i8 + 1) * 1024], in_=ost[i8]),
                          reads=[("ost", i8)], writes=["obT_d"])

            sidx = 0
            for hp_ in range(4):
                B_proj(hp_)
                if stop == ("Bp", l):
                    P.barrier(); dump_and_finish(); return True
                for hl_ in range(2):
                    sidx += B_head(hp_, hl_, sidx)
                    if stop in (("Bs", l), ("Bh", l)):
                        P.barrier(); dump_and_finish(); return True
                B_pair_end(hp_)
                if stop == ("Be", l):
                    P.barrier(); dump_and_finish(); return True
            P.dve(lambda e: e.tensor_reduce(out=rsb, in_=ssqb, axis=AX.X, op=ALU.add), reads=[("ssqb", j) for j in range(4)], writes=["rsb0"])
            P.dve(lambda e: e.tensor_scalar(out=rsb, in0=rsb, scalar1=1.0 / 512, scalar2=EPS, op0=ALU.mult, op1=ALU.add),
                  reads=["rsb0"], writes=["rsb1"])
            P.pool(lambda e: e.tensor_tensor(out=rsb, in0=rsb, in1=nhalf[:, 0:1].to_broadcast([128, 16]), op=ALU.pow),
                   reads=["rsb1", "nhalf"], writes=["rsb"])
            if stop == ("B1", l):
                P.barrier()
                P.dma("sp", "dbg", lambda e: e.dma_start(out=dbg_obT, in_=obT_d))
                P.barrier(); dump_and_finish(); return True
            P.barrier()
            ar.reset()
            obT = ar.bf16([128, 4, 2048])
            P.dma("sp", "a0", lambda e: e.dma_start(out=obT, in_=obT_d), writes=["obT"])
            w_ob = ar.bf16([128, 4, 1024])
            junkb = ar.bf16([128, 1024])
            g1b2 = ar.f32([128, 1024])
            gabf2 = ar.f32([128, 8])
            P.dma("pool", "wA", lambda e: e.dma_start(out=w_ob, in_=wout_d[l, 512:1024, :].rearrange("(k p) n -> p k n", p=128)),
                  writes=["w_ob"])
            P.dma("sp", "a0", lambda e: e.dma_start(out=g1b2, in_=modscr[l, 2048:3072].partition_broadcast(128)), writes=["g1b2"])
            P.dma("sp", "a0", lambda e: e.dma_start(out=gabf2, in_=gab_d[l]), writes=["gabf2"])
            for c in range(4):
                P.dve(lambda e, c=c: e.scalar_tensor_tensor(out=w_ob[:, c, :], in0=w_ob[:, c, :], scalar=gabf2[:, 4 + c:5 + c], in1=g1b2,
                                                             op0=ALU.mult, op1=ALU.mult),
                      reads=["w_ob", "gabf2", "g1b2"], writes=["w_ob"])
            for i in range(16):
                for hf in range(2):
                    bk = 2 * (i % 2) + hf
                    for c in range(4):
                        P.pe(lambda e, c=c, hf=hf, bk=bk, i=i: e.matmul(bank(bk), lhsT=obT[:, c, i * 128:(i + 1) * 128],
                                                                        rhs=w_ob[:, c, hf * 512:(hf + 1) * 512], start=(c == 0), stop=(c == 3)),
                             reads=["w_ob", "obT"], writes=[("ps", bk)])
                    P.dve(lambda e, hf=hf, bk=bk, i=i: e.scalar_tensor_tensor(out=x[:, i, hf * 512:(hf + 1) * 512], in0=bank(bk),
                                                                              scalar=rsb[:, i:i + 1], in1=x[:, i, hf * 512:(hf + 1) * 512],
                                                                              op0=ALU.mult, op1=ALU.add),
                          reads=[("ps", bk), ("x", i)], writes=[("x", i)])
                P.act(lambda e, i=i: e.activation(out=junkb, in_=x[:, i, :], func=AF.Square, accum_out=ssqN[:, i:i + 1]),
                      reads=[("x", i)], writes=["junkb", ("ssq", i)])
            if stop == ("B", l):
                P.barrier(); dump_and_finish(); return True

            ar2.reset()
            sg = [ar2.bf16([128, 512]) for _ in range(2)]
            actT = [ar2.bf16([128, 4, 512]) for _ in range(2)]
            junkM = ar2.bf16([128, 1024])
            if l == 0:
                stageM = [ar2.bf16([128, 8, 512]) for _ in range(2)]
                modbrM = [ar2.f32([1, 512]) for _ in range(2)]
                mrowM = [ar2.f32([1, 512]) for _ in range(2)]
            assert ar2.off <= 11 * 1024, ar2.off
            ar2.off = 11 * 1024
            wgb = [ar2.bf16([128, 8, 512]) for _ in range(2)]
            wub = [ar2.bf16([128, 8, 512]) for _ in range(2)]
            wdb = [ar2.bf16([128, 4, 1024]) for _ in range(2)]
            g2b = ar2.f32([128, 1024])

            def load_expert(ex):
                pb = ex % 2
                for kk in range(2):
                    P.dma("pool", "wg%d" % pb, lambda e, kk=kk: e.dma_start(
                        out=wgb[pb][:, kk * 4:(kk + 1) * 4, :],
                        in_=wg_d[l, ex, kk * 512:(kk + 1) * 512, :].rearrange("(k p) n -> p k n", p=128)), writes=[("wg", pb)])
                    P.dma("pool", "wu%d" % pb, lambda e, kk=kk: e.dma_start(
                        out=wub[pb][:, kk * 4:(kk + 1) * 4, :],
                        in_=wu_d[l, ex, kk * 512:(kk + 1) * 512, :].rearrange("(k p) n -> p k n", p=128)), writes=[("wu", pb)])
                    P.dma("pool", "wd%d" % pb, lambda e, kk=kk: e.dma_start(
                        out=wdb[pb][:, kk * 2:(kk + 1) * 2, :],
                        in_=wd_d[l, ex, kk * 256:(kk + 1) * 256, :].rearrange("(k p) n -> p k n", p=128)), writes=[("wd", pb)])
                for c in range(4):
                    P.pool(lambda e, c=c: e.tensor_tensor(out=wdb[pb][:, c, :], in0=wdb[pb][:, c, :], in1=g2b, op=ALU.mult),
                           reads=[("wd", pb), "g2b"], writes=[("wd", pb)])

            def M_pre():
                P.dma("sp", "m0", lambda e: e.dma_start(out=g2b, in_=modscr[l, 5120:6144].partition_broadcast(128)), writes=["g2b"])
                load_expert(0)
                load_expert(1)

            norm_phase(l, 2, pre=M_pre, have_ssq=True)
            if stop == ("N2a", l):
                P.barrier(); dump_and_finish(); return True
            P.barrier()

            def GU(ex, tb, n):
                pb = ex % 2
                ab = n % 2
                for fc in range(4):
                    q = fc % 2
                    for k in range(8):
                        P.pe(lambda e, k=k, fc=fc, q=q: e.matmul(bank(q), lhsT=wgb[pb][:, k, fc * 128:(fc + 1) * 128],
                                                                 rhs=hT[:, k, tb * 512:(tb + 1) * 512], start=(k == 0), stop=(k == 7)),
                             reads=[("wg", pb)], writes=[("ps", q)])
                    for k in range(8):
                        P.pe(lambda e, k=k, fc=fc, q=q: e.matmul(bank(2 + q), lhsT=wub[pb][:, k, fc * 128:(fc + 1) * 128],
                                                                 rhs=hT[:, k, tb * 512:(tb + 1) * 512], start=(k == 0), stop=(k == 7)),
                             reads=[("wu", pb)], writes=[("ps", 2 + q)])
                    P.act(lambda e, q=q: e.activation(out=sg[q], in_=bank(q), func=AF.Silu), reads=[("ps", q)], writes=[("sg", q)])
                    P.dve(lambda e, q=q, fc=fc: e.tensor_tensor(out=actT[ab][:, fc, :], in0=bank(2 + q), in1=sg[q], op=ALU.mult),
                          reads=[("ps", 2 + q), ("sg", q)], writes=[("actT", ab)])

            def DN(ex, tb, n):
                pb = ex % 2
                ab = n % 2
                for tt in range(4):
                    i = tb * 4 + tt
                    for hf in range(2):
                        bk = 4 + ((tt * 2 + hf) % 4)
                        for fc in range(4):
                            P.pe(lambda e, fc=fc, hf=hf, tt=tt, bk=bk: e.matmul(bank(bk), lhsT=actT[ab][:, fc, tt * 128:(tt + 1) * 128],
                                                                                rhs=wdb[pb][:, fc, hf * 512:(hf + 1) * 512],
                                                                                start=(fc == 0), stop=(fc == 3)),
                                 reads=[("actT", ab), ("wd", pb)], writes=[("ps", bk)])
                        P.dve(lambda e, hf=hf, i=i, bk=bk: e.scalar_tensor_tensor(out=x[:, i, hf * 512:(hf + 1) * 512], in0=bank(bk),
                                                                                  scalar=gates[:, i, ex:ex + 1], in1=x[:, i, hf * 512:(hf + 1) * 512],
                                                                                  op0=ALU.mult, op1=ALU.add),
                              reads=[("ps", bk), "gates", ("x", i)], writes=[("x", i)])
                    if ex == 15:
                        P.act(lambda e, i=i: e.activation(out=junkM, in_=x[:, i, :], func=AF.Square, accum_out=ssqN[:, i:i + 1]),
                              reads=[("x", i)], writes=["junkM", ("ssq", i)])

            seqs = [(ex, tb) for ex in range(16) for tb in range(4)]
            for n in range(len(seqs) + 1):
                if n < len(seqs):
                    GU(seqs[n][0], seqs[n][1], n)
                if n == 0:
                    router_phase()
                    assert ar.off <= 11 * 1024, ar.off
                    if stop == ("N2", l):
                        P.barrier(); dump_and_finish(); return True
                if n >= 1:
                    ex, tb = seqs[n - 1]
                    DN(ex, tb, n - 1)
                    if tb == 3 and ex + 2 < 16:
                        load_expert(ex + 2)
                    if tb == 3 and l == 0:
                        if 1 <= ex <= 12:
                            j = ex - 1
                            mod_chunk(1, j, j % 2, stageM[j % 2], modbrM[j % 2], mrowM[j % 2], 7, part=2)
                        if ex < 12:
                            mod_chunk(1, ex, ex % 2, stageM[ex % 2], modbrM[ex % 2], mrowM[ex % 2], 7, part=1,
                                      extra_w=["rt"] if ex < 2 else ())
            if stop == ("M", l):
                P.barrier(); dump_and_finish(); return True

            return False

        for l_ in range(n_layers):
            if layer(l_):
                return nc

        P.barrier()
        ar.reset()
        fgb = ar.f32([128, 1024])
        ob = [ar.f32([128, 1024]) for _ in range(3)]
        junk = ar.bf16([128, 1024])
        ssq = ssqN; rstd = ar.f32([128, 16])
        P.dma("sp", "n0", lambda e: e.dma_start(out=fgb, in_=fg_d[0, :].partition_broadcast(128)), writes=["fgb"])
        P.act(lambda e: e.activation(out=rstd, in_=ssq, func=AF.Sqrt, bias=eps_t[:, 0:1], scale=1.0 / 1024),
              reads=[("ssq", i) for i in range(16)] + ["eps"], writes=["rstd0"])
        P.dve(lambda e: e.reciprocal(out=rstd, in_=rstd), reads=["rstd0"], writes=["rstd"])
        for i in range(16):
            o = ob[i % 3]
            P.dve(lambda e, i=i, o=o: e.scalar_tensor_tensor(out=o, in0=x[:, i, :], scalar=rstd[:, i:i + 1], in1=fgb,
                                                             op0=ALU.mult, op1=ALU.mult),
                  reads=[("x", i), "rstd", "fgb"], writes=[("ob", i % 3)])
            P.dma("sp", "out", lambda e, i=i, o=o: e.dma_start(out=out_d[i * 128:(i + 1) * 128, :], in_=o),
                  reads=[("ob", i % 3)], writes=[("out", i)])
        if dbg:
            P.barrier(); dump_and_finish(); return nc
        P.emit(final_wait_streams=["out"])
    return nc


def t5_bucket_np(dist):
    dist = np.maximum(dist, 0)
    ratio = np.log(np.maximum(dist, 1) / 16) / np.log(2048 / 16)
    large = 16 + np.floor(ratio * 16).astype(np.int64)
    large = np.minimum(large, 31)
    return np.where(dist < 16, dist, large).astype(np.int32)


def host_consts():
    btab = np.zeros((3, 33, 384), np.float32)
    for di, (win, d) in enumerate(CONFIGS):
        for j in range(384):
            rel = j - 127
            if 0 <= rel <= 128:
                btab[di, int(t5_bucket_np(np.array(rel * d))), j] = 1.0
            else:
                btab[di, 32, j] = NEG
    ind = np.zeros((8, 512), np.float32)
    for g in range(8):
        ind[g, g * 64:(g + 1) * 64] = 1.0
    trilm = np.triu(np.ones((128, 128), np.float32))
    return dict(identf=np.eye(128, dtype=np.float32), btab=btab, ind=ind, trilm=trilm,
                jmat=np.ascontiguousarray(np.eye(128, dtype=np.float32)[::-1]))


def make_in_maps(inputs, cores):
    f = lambda a: np.ascontiguousarray(np.asarray(a, dtype=np.float32))
    shared = dict(
        rel_bias=f(inputs["rel_bias"]), router_w=f(inputs["router_w"]), router_b=f(inputs["router_b"]).reshape(1, 16),
        mod_w=f(inputs["mod_w"]), mod_b=f(inputs["mod_b"]), norm1_g=f(inputs["norm1_g"]), w_in=f(inputs["w_in"]),
        gmlp_ln_g=f(inputs["gmlp_ln_g"]), gmlp_ln_b=f(inputs["gmlp_ln_b"]), gmlp_ws=f(inputs["gmlp_ws"]),
        gmlp_bs=f(inputs["gmlp_bs"]), w_out=f(inputs["w_out"]), norm2_g=f(inputs["norm2_g"]),
        moe_w_gate=f(inputs["moe_w_gate"]), moe_w_up=f(inputs["moe_w_up"]), moe_w_down=f(inputs["moe_w_down"]),
        final_g=f(inputs["final_g"]).reshape(1, 1024),
    )
    gab = np.concatenate([f(inputs["out_norm_a_g"]), f(inputs["out_norm_b_g"])], axis=1)
    shared["gab"] = np.ascontiguousarray(gab.reshape(2, 8, 128).transpose(0, 2, 1))
    shared.update(host_consts())
    x = f(inputs["x"]); c = f(inputs["c"])
    maps = []
    for b in cores:
        m = dict(shared)
        m["x"] = np.ascontiguousarray(x[b])
        m["cT"] = np.ascontiguousarray(c[b].reshape(8, 128).T)
        maps.append(m)
    return maps


def kernel(**inputs):
    nc = build()
    maps = make_in_maps(inputs, list(range(8)))
    res = run_bass_kernel_spmd(nc, maps, core_ids=list(range(8)))
    return np.stack([np.asarray(r["out"], dtype=np.float32) for r in res.results], axis=0)
```
